# Optimizing a Trainium2 kernel written in Bass

```python
import jax, jax.numpy as jnp
from jax import lax
import numpy as np

D_MODEL = 1024
BATCH = 8
SEQ = 4096
DEPTH = 2

GRID_W = 64
CTX_LEN = 256
MIX_WIDTH = D_MODEL
FNET_WIDTH = MIX_WIDTH // 2
FNET_GROUPS = 4
FNET_GROUP_DIM = FNET_WIDTH // FNET_GROUPS
GLA_WIDTH = MIX_WIDTH - FNET_WIDTH
GLA_HEADS = 4
GLA_DV = GLA_WIDTH // GLA_HEADS
GLA_DK = GLA_DV // 2
GLA_QK = GLA_HEADS * GLA_DK
GATE_RANK = 16
GATE_NORMALIZER = 16.0
GLA_CHUNK = 64
D_FF = 2816
N_EXPERTS = 8
TOP_K = 2
EXPERT_FF = 3584
N_DENSE = (DEPTH + 1) // 2
N_MOE = DEPTH // 2
EPS = 1e-6
SPLIT_POINTS = (FNET_WIDTH,
                FNET_WIDTH + GLA_QK,
                FNET_WIDTH + 2 * GLA_QK,
                FNET_WIDTH + 2 * GLA_QK + GLA_WIDTH,
                FNET_WIDTH + 2 * GLA_QK + 2 * GLA_WIDTH,
                FNET_WIDTH + 2 * GLA_QK + 2 * GLA_WIDTH + GATE_RANK)
IN_WIDTH = FNET_WIDTH + 2 * GLA_QK + 2 * GLA_WIDTH + 2 * GATE_RANK

kernel_name = 'hybrid_fnet_gla_moe_dit'


def rmsnorm(x, w):
    xf = x.astype(jnp.float32)
    y = xf * lax.rsqrt(jnp.mean(xf * xf, axis=-1, keepdims=True) + EPS)
    return y.astype(x.dtype) * w


def modulate(h, shift, scale):
    return h * (1 + scale) + shift


def fourier_mix(u):
    B, L, _ = u.shape
    ug = u.reshape(B, L, FNET_GROUPS, FNET_GROUP_DIM).astype(jnp.float32)
    y = jnp.fft.fftn(ug, axes=(1, 3), norm='ortho').real
    return y.reshape(B, L, FNET_WIDTH).astype(u.dtype)


def gla_chunked(q, k, v, log_a, s0):
    B, L, H, DK = q.shape
    DV = v.shape[-1]
    n = L // GLA_CHUNK

    def to_chunks(t):
        return jnp.moveaxis(t.astype(jnp.float32).reshape(B, n, GLA_CHUNK, H, t.shape[-1]), 1, 0)

    qs, ks, vs, gs = to_chunks(q), to_chunks(k), to_chunks(v), to_chunks(log_a)
    causal = jnp.tril(jnp.ones((GLA_CHUNK, GLA_CHUNK), dtype=bool))[None, :, :, None, None]

    def step(S, inp):
        qc, kc, vc, gc = inp
        b = jnp.cumsum(gc, axis=1)
        o_inter = jnp.einsum('bthk,bhkv->bthv', qc * jnp.exp(b), S)
        diff = b[:, :, None] - b[:, None, :]
        decay = jnp.where(causal, jnp.exp(jnp.where(causal, diff, 0.0)), 0.0)
        scores = jnp.einsum('bthk,bshk,btshk->bhts', qc, kc, decay)
        o_intra = jnp.einsum('bhts,bshv->bthv', scores, vc)
        b_last = b[:, -1]
        S_new = jnp.exp(b_last)[..., None] * S + jnp.einsum(
            'bshk,bshv->bhkv', kc * jnp.exp(b_last[:, None] - b), vc)
        return S_new, o_inter + o_intra

    s_fin, o = lax.scan(step, s0.astype(jnp.float32), (qs, ks, vs, gs))
    o = jnp.moveaxis(o, 0, 1).reshape(B, L, H, DV)
    return o, s_fin


def split_projection(p, w_gate_f, b_gate_f, w_gate_b, b_gate_b):
    B, L, _ = p.shape
    u, q, k, v, g, lf, lb = jnp.split(p, SPLIT_POINTS, axis=-1)
    q = q.reshape(B, L, GLA_HEADS, GLA_DK) * (GLA_DK ** -0.5)
    k = k.reshape(B, L, GLA_HEADS, GLA_DK)
    v = v.reshape(B, L, GLA_HEADS, GLA_DV)
    log_f = jax.nn.log_sigmoid((lf @ w_gate_f + b_gate_f).astype(jnp.float32)) / GATE_NORMALIZER
    log_b = jax.nn.log_sigmoid((lb @ w_gate_b + b_gate_b).astype(jnp.float32)) / GATE_NORMALIZER
    log_f = log_f.reshape(B, L, GLA_HEADS, GLA_DK)
    log_b = log_b.reshape(B, L, GLA_HEADS, GLA_DK)
    return u, q, k, v, g, log_f, log_b


def gla_output(o, g, gla_norm_w):
    B, L = o.shape[:2]
    on = o * lax.rsqrt(jnp.mean(o * o, axis=-1, keepdims=True) + EPS)
    on = on.astype(g.dtype) * gla_norm_w
    return on.reshape(B, L, GLA_WIDTH) * jax.nn.silu(g)


def token_mixer(hx, hc, w_in, w_gate_f, b_gate_f, w_gate_b, b_gate_b, gla_norm_w, w_out, ctx_out):
    ux, qx, kx, vx, gx, lfx, lbx = split_projection(hx @ w_in, w_gate_f, b_gate_f, w_gate_b, b_gate_b)
    uc, qc, kc, vc, gc, lfc, lbc = split_projection(hc @ w_in, w_gate_f, b_gate_f, w_gate_b, b_gate_b)
    flip = lambda t: jnp.flip(t, axis=1)
    s0 = jnp.zeros((hx.shape[0], GLA_HEADS, GLA_DK, GLA_DV), jnp.float32)
    oc_f, sc_f = gla_chunked(qc, kc, vc, lfc, s0)
    ox_f, _ = gla_chunked(qx, kx, vx, lfx, sc_f)
    oc_b, sc_b = gla_chunked(flip(qc), flip(kc), flip(vc), flip(lbc), s0)
    ox_b, _ = gla_chunked(flip(qx), flip(kx), flip(vx), flip(lbx), sc_b)
    yx = gla_output(ox_f + flip(ox_b), gx, gla_norm_w)
    out_x = jnp.concatenate([fourier_mix(ux), yx], axis=-1) @ w_out
    if ctx_out:
        yc = gla_output(oc_f + flip(oc_b), gc, gla_norm_w)
        out_c = jnp.concatenate([fourier_mix(uc), yc], axis=-1) @ w_out
    else:
        out_c = None
    return out_x, out_c


def swiglu(h, w1, w3, w2):
    return (jax.nn.silu(h @ w1) * (h @ w3)) @ w2


def moe_swiglu(h, router_w, w1, w3, w2):
    B, L, D = h.shape
    t = h.reshape(B * L, D)
    logits = (t @ router_w).astype(jnp.float32)
    top_v, top_i = lax.top_k(logits, TOP_K)
    gates = jax.nn.softmax(top_v, axis=-1)
    combine = jnp.einsum('tk,tke->te', gates, jax.nn.one_hot(top_i, N_EXPERTS, dtype=jnp.float32))
    combine = combine.astype(t.dtype)
    out = jnp.zeros_like(t)
    for e in range(N_EXPERTS):
        out = out + combine[:, e:e + 1] * swiglu(t, w1[e], w3[e], w2[e])
    return out.reshape(B, L, D)


def channel_mixer(h, l, ffn_w1, ffn_w3, ffn_w2, router_w, moe_w1, moe_w3, moe_w2):
    i = l // 2
    if l % 2 == 0:
        return swiglu(h, ffn_w1[i], ffn_w3[i], ffn_w2[i])
    return moe_swiglu(h, router_w[i], moe_w1[i], moe_w3[i], moe_w2[i])


def setup_inputs(seed: int = 0) -> dict:
    key = jax.random.key(seed)
    ks = jax.random.split(key, 26)
    nrm = lambda k, shape, s: jax.random.normal(k, shape, jnp.float32) * s
    D = D_MODEL
    return {
        'x': nrm(ks[0], (BATCH, SEQ, D), 1.0),
        'c': nrm(ks[1], (BATCH, D), 1.0),
        'ctx': nrm(ks[2], (BATCH, CTX_LEN, D), 1.0),
        'c_ctx': nrm(ks[3], (D,), 1.0),
        'w_mod': nrm(ks[4], (DEPTH, D, 6 * D), 0.5 * D ** -0.5),
        'b_mod': nrm(ks[5], (DEPTH, 6 * D), 0.02),
        'norm_mix_w': 1.0 + nrm(ks[6], (DEPTH, D), 0.02),
        'norm_ffn_w': 1.0 + nrm(ks[7], (DEPTH, D), 0.02),
        'w_in': nrm(ks[8], (DEPTH, D, IN_WIDTH), D ** -0.5),
        'w_gate_f': nrm(ks[9], (DEPTH, GATE_RANK, GLA_QK), GATE_RANK ** -0.5),
        'b_gate_f': nrm(ks[10], (DEPTH, GLA_QK), 0.5),
        'w_gate_b': nrm(ks[11], (DEPTH, GATE_RANK, GLA_QK), GATE_RANK ** -0.5),
        'b_gate_b': nrm(ks[12], (DEPTH, GLA_QK), 0.5),
        'gla_norm_w': 1.0 + nrm(ks[13], (DEPTH, GLA_DV), 0.02),
        'w_out': nrm(ks[14], (DEPTH, MIX_WIDTH, D), MIX_WIDTH ** -0.5),
        'ffn_w1': nrm(ks[15], (N_DENSE, D, D_FF), D ** -0.5),
        'ffn_w3': nrm(ks[16], (N_DENSE, D, D_FF), D ** -0.5),
        'ffn_w2': nrm(ks[17], (N_DENSE, D_FF, D), D_FF ** -0.5),
        'router_w': nrm(ks[18], (N_MOE, D, N_EXPERTS), D ** -0.5),
        'moe_w1': nrm(ks[19], (N_MOE, N_EXPERTS, D, EXPERT_FF), D ** -0.5),
        'moe_w3': nrm(ks[20], (N_MOE, N_EXPERTS, D, EXPERT_FF), D ** -0.5),
        'moe_w2': nrm(ks[21], (N_MOE, N_EXPERTS, EXPERT_FF, D), EXPERT_FF ** -0.5),
        'final_norm_w': 1.0 + nrm(ks[22], (D,), 0.02),
    }


def reference(x, c, ctx, c_ctx, w_mod, b_mod, norm_mix_w, norm_ffn_w, w_in,
              w_gate_f, b_gate_f, w_gate_b, b_gate_b, gla_norm_w, w_out,
              ffn_w1, ffn_w3, ffn_w2, router_w, moe_w1, moe_w3, moe_w2, final_norm_w):
    silu_c = jax.nn.silu(c)
    silu_cc = jax.nn.silu(c_ctx)
    for l in range(DEPTH):
        last = l == DEPTH - 1
        mod_x = (silu_c @ w_mod[l] + b_mod[l])[:, None, :]
        mod_c = (silu_cc @ w_mod[l] + b_mod[l])[None, None, :]
        sh1x, sc1x, g1x, sh2x, sc2x, g2x = jnp.split(mod_x, 6, axis=-1)
        sh1c, sc1c, g1c, sh2c, sc2c, g2c = jnp.split(mod_c, 6, axis=-1)
        hx = modulate(rmsnorm(x, norm_mix_w[l]), sh1x, sc1x)
        hc = modulate(rmsnorm(ctx, norm_mix_w[l]), sh1c, sc1c)
        mx, mc = token_mixer(hx, hc, w_in[l], w_gate_f[l], b_gate_f[l], w_gate_b[l], b_gate_b[l],
                             gla_norm_w[l], w_out[l], not last)
        x = x + g1x * mx
        hx = modulate(rmsnorm(x, norm_ffn_w[l]), sh2x, sc2x)
        x = x + g2x * channel_mixer(hx, l, ffn_w1, ffn_w3, ffn_w2, router_w, moe_w1, moe_w3, moe_w2)
        if not last:
            ctx = ctx + g1c * mc
            hc = modulate(rmsnorm(ctx, norm_ffn_w[l]), sh2c, sc2c)
            ctx = ctx + g2c * channel_mixer(hc, l, ffn_w1, ffn_w3, ffn_w2, router_w, moe_w1, moe_w3, moe_w2)
    return rmsnorm(x, final_norm_w)
```

```python
import contextlib
import os
import numpy as np
import ml_dtypes
import concourse.bass as bass
import concourse.mybir as mybir
from concourse.bass_utils import run_bass_kernel_spmd

F32 = mybir.dt.float32
BF16 = mybir.dt.bfloat16
AF = mybir.ActivationFunctionType
ALU = mybir.AluOpType

D = 1024
KD = 8
SEQ = 4096
CTX = 256
NTOK = SEQ + CTX
DEPTH = 2
IN_W = 2080
DFF = 2816
EFF = 3584
NEXP = 8
EPS = 1e-6
TILES = [(0, 256)] + [(256 + 512 * i, 512) for i in range(8)]
NCH = NTOK // 128
TS = 512
NSLOT = (2 * SEQ + NEXP * (TS - 1)) // TS
I32 = mybir.dt.int32
CAP = SEQ


class T:
    __slots__ = ("w", "r")

    def __init__(self):
        self.w = {}
        self.r = {}


def TL(n):
    return [T() for _ in range(n)]


class Sched:
    ENG = ("pe", "act", "dve", "pool", "sp")

    def __init__(self, nc, stack, ring=20):
        self.nc = nc
        self.streams = {e: [] for e in self.ENG}
        self.semobj = {}
        for e in ("pe", "act", "dve", "pool"):
            self.semobj[e] = stack.enter_context(nc.semaphore("s_" + e))
        self.cnt = {e: 0 for e in ("pe", "act", "dve", "pool")}
        self.seen = {e: {} for e in self.ENG}
        self.ring = {}
        self.ring_n = {}
        self.ring_pos = {}
        for q, n in (("sp", ring), ("pool", ring)):
            keys = []
            for i in range(n):
                k = f"d_{q}{i}"
                self.semobj[k] = stack.enter_context(nc.semaphore(k))
                keys.append(k)
            self.ring[q] = keys
            self.ring_n[q] = {k: 0 for k in keys}
            self.ring_pos[q] = 0

    def _deps(self, reads, writes):
        deps = {}
        for t in reads:
            for k, v in t.w.items():
                if deps.get(k, 0) < v:
                    deps[k] = v
        for t in writes:
            for k, v in t.w.items():
                if deps.get(k, 0) < v:
                    deps[k] = v
            for k, v in t.r.items():
                if deps.get(k, 0) < v:
                    deps[k] = v
        return deps

    def _waits(self, eng, deps, skip_own=None):
        waits = []
        seen = self.seen[eng]
        for k, v in deps.items():
            if k == skip_own:
                continue
            if seen.get(k, 0) < v:
                seen[k] = v
                waits.append((self.semobj[k], v))
        return waits

    def _mark(self, reads, writes, key, val):
        for t in reads:
            if t.r.get(key, 0) < val:
                t.r[key] = val
        for t in writes:
            t.w = {key: val}
            t.r = {}

    def op(self, eng, fn, reads=(), writes=()):
        deps = self._deps(reads, writes)
        waits = self._waits(eng, deps, skip_own=("pe" if eng == "pe" else None))
        self.cnt[eng] += 1
        val = self.cnt[eng]
        so = self.semobj[eng]

        def emit(e, waits=waits, fn=fn, so=so):
            for s, v in waits:
                e.wait_ge(s, v)
            fn(e).then_inc(so, 1)
        self.streams[eng].append(emit)
        self._mark(reads, writes, eng, val)

    def dma(self, q, out, in_, reads=(), writes=(), **kw):
        deps = self._deps(reads, writes)
        keys = self.ring[q]
        key = keys[self.ring_pos[q] % len(keys)]
        self.ring_pos[q] += 1
        prev = self.ring_n[q][key]
        if prev > 0 and deps.get(key, 0) < prev * 16:
            deps[key] = prev * 16
        waits = self._waits(q, deps)
        self.ring_n[q][key] = prev + 1
        val = (prev + 1) * 16
        so = self.semobj[key]

        def emit(e, waits=waits, so=so, out=out, in_=in_, kw=kw):
            for s, v in waits:
                e.wait_ge(s, v)
            e.dma_start(out=out, in_=in_, **kw).then_inc(so, 16)
        self.streams[q].append(emit)
        self._mark(reads, writes, key, val)

    def dma_custom(self, q, fn, reads=(), writes=()):
        deps = self._deps(reads, writes)
        keys = self.ring[q]
        key = keys[self.ring_pos[q] % len(keys)]
        self.ring_pos[q] += 1
        prev = self.ring_n[q][key]
        if prev > 0 and deps.get(key, 0) < prev * 16:
            deps[key] = prev * 16
        waits = self._waits(q, deps)
        self.ring_n[q][key] = prev + 1
        val = (prev + 1) * 16
        so = self.semobj[key]

        def emit(e, waits=waits, so=so, fn=fn):
            for s, v in waits:
                e.wait_ge(s, v)
            fn(e).then_inc(so, 16)
        self.streams[q].append(emit)
        self._mark(reads, writes, key, val)

    def barrier(self):
        allv = {}
        for e in ("pe", "act", "dve", "pool"):
            if self.cnt[e] > 0:
                allv[e] = self.cnt[e]
        for q in self.ring:
            for k, n in self.ring_n[q].items():
                if n > 0:
                    allv[k] = n * 16
        for eng in self.ENG:
            waits = self._waits(eng, dict(allv))

            def emit(e, waits=waits):
                for s, v in waits:
                    e.wait_ge(s, v)
            self.streams[eng].append(emit)

    def emit_all(self):
        with self.nc.Block() as block:
            @block.tensor
            def _(e):
                for f in self.streams["pe"]:
                    f(e)

            @block.scalar
            def _(e):
                for f in self.streams["act"]:
                    f(e)

            @block.vector
            def _(e):
                for f in self.streams["dve"]:
                    f(e)

            @block.gpsimd
            def _(e):
                for f in self.streams["pool"]:
                    f(e)

            @block.sync
            def _(e):
                for f in self.streams["sp"]:
                    f(e)


class Builder:
    def __init__(self, dbg=None, stop_after=None):
        self.dbg = dbg or set()
        self.stop_after = stop_after
        self.nc = bass.Bass("TRN2", target_bir_lowering=False)
        self.uid = 0
        self._bregs = {}

    def breg(self, e, bound):
        if bound not in self._bregs:
            self._bregs[bound] = e.to_reg(bound)
        return self._bregs[bound]

    def sb(self, st, shape, dt, name=None):
        self.uid += 1
        return st.enter_context(self.nc.sbuf_tensor(f"{name or 't'}_{self.uid}", list(shape), dt))

    def din(self, name, shape, dt=F32):
        return self.nc.dram_tensor(name, list(shape), dt, kind="ExternalInput").ap()

    def dscr(self, name, shape, dt):
        kind = "ExternalOutput" if name in self.dbg else "Internal"
        return self.nc.dram_tensor(name, list(shape), dt, kind=kind).ap()

    def psn(self):
        i = self.ps_i % 8
        self.ps_i += 1
        return self.ps[i], self.Tps[i]

    def mm(self, out, lhsT, rhs, start, stop, r, w):
        self.S.op("pe", lambda e: e.matmul(out, lhsT, rhs, start=start, stop=stop, skip_group_check=True), reads=r, writes=w)

    def act(self, out, in_, func, r, w, bias=None, scale=None):
        kw = {}
        if bias is not None:
            kw["bias"] = bias
        if scale is not None:
            kw["scale"] = scale
        self.S.op("act", lambda e: e.activation(out=out, in_=in_, func=func, **kw), reads=r, writes=w)

    def tt(self, eng, out, in0, in1, op, r, w):
        self.S.op(eng, lambda e: e.tensor_tensor(out=out, in0=in0, in1=in1, op=op), reads=r, writes=w)

    def ts(self, eng, out, in0, s1, s2, op0, op1, r, w):
        if s2 is None:
            self.S.op(eng, lambda e: e.tensor_scalar(out=out, in0=in0, scalar1=s1, scalar2=None, op0=op0), reads=r, writes=w)
        else:
            self.S.op(eng, lambda e: e.tensor_scalar(out=out, in0=in0, scalar1=s1, scalar2=s2, op0=op0, op1=op1), reads=r, writes=w)

    def stt(self, out, in0, scalar, in1, op0, op1, r, w):
        self.S.op("dve", lambda e: e.scalar_tensor_tensor(out=out, in0=in0, scalar=scalar, in1=in1, op0=op0, op1=op1), reads=r, writes=w)

    def cp(self, eng, out, in_, r, w):
        if eng == "act":
            self.S.op("act", lambda e: e.activation(out=out, in_=in_, func=AF.Copy), reads=r, writes=w)
        else:
            self.S.op(eng, lambda e: e.tensor_copy(out=out, in_=in_), reads=r, writes=w)

    def recip(self, out, in_, r, w):
        self.S.op("dve", lambda e: e.reciprocal(out=out, in_=in_), reads=r, writes=w)

    def memset(self, eng, ap, val, w):
        self.S.op(eng, lambda e: e.memset(ap, val), writes=w)

    def rstd_from_ps(self, rstd, Trs, ps, Tp, n, inv_n):
        self.act(rstd[:, :n], ps[:, :n], AF.Sqrt, [Tp, self.Tc], [Trs], bias=self.eps_c[:, 0:1], scale=inv_n)
        self.recip(rstd[:, :n], rstd[:, :n], [Trs], [Trs])

    def build(self):
        nc = self.nc
        I = {}
        I["xt"] = self.din("xt", [D, NTOK])
        I["cvec"] = self.din("cvec", [128, 16])
        for l in range(DEPTH):
            I[f"w_mod{l}"] = self.din(f"w_mod{l}", [D, 6 * D])
            I[f"b_mod{l}"] = self.din(f"b_mod{l}", [128, 48])
            I[f"nmw{l}"] = self.din(f"nmw{l}", [128, 8])
            I[f"nfw{l}"] = self.din(f"nfw{l}", [128, 8])
            I[f"w_in{l}"] = self.din(f"w_in{l}", [D, IN_W])
            I[f"wg{l}"] = self.din(f"wg{l}", [16, 512])
            I[f"bg{l}"] = self.din(f"bg{l}", [1, 512])
            I[f"gnw{l}"] = self.din(f"gnw{l}", [128, 1])
            I[f"w_out{l}"] = self.din(f"w_out{l}", [D, D])
        I["ffn_w1"] = self.din("ffn_w1", [D, DFF])
        I["ffn_w3"] = self.din("ffn_w3", [D, DFF])
        I["ffn_w2"] = self.din("ffn_w2", [DFF, D])
        I["router_w"] = self.din("router_w", [D, NEXP])
        I["mw1"] = self.din("mw1", [NEXP * 7 * 128, 8 * 512])
        I["mw3"] = self.din("mw3", [NEXP * 7 * 128, 8 * 512])
        I["mw2"] = self.din("mw2", [NEXP * 7 * 128, 4 * D])
        I["fnw"] = self.din("fnw", [128, 8])
        I["dftc"] = self.din("dftc", [SEQ, SEQ], BF16)
        I["dfts"] = self.din("dfts", [SEQ, SEQ], BF16)
        I["dftc256"] = self.din("dftc256", [CTX, CTX], BF16)
        I["dfts256"] = self.din("dfts256", [CTX, CTX], BF16)
        I["cs128"] = self.din("cs128", [128, 256], BF16)
        I["tri"] = self.din("tri", [128, 512])
        I["mask"] = self.din("mask", [128, 1024], BF16)
        I["ident"] = self.din("ident", [128, 128])
        I["sel"] = self.din("sel", [8, 1024])
        I["su"] = self.din("su", [128, 128])
        I["identb"] = self.din("identb", [128, 128], BF16)
        I["slotstart"] = self.din("slotstart", [128, NSLOT])
        I["gp7"] = self.din("gp7", [128, 7])
        I["ecap"] = self.din("ecap", [128, 8])
        I["jsh"] = self.din("jsh", [128, 256], BF16)
        I["alt"] = self.din("alt", [1, SEQ], BF16)
        self.I = I
        out = nc.dram_tensor("out", [D, SEQ], F32, kind="ExternalOutput").ap()
        self.out = out
        self.XS = self.dscr("XS", [D, NTOK], F32)
        self.U_tm = self.dscr("U_tm", [NTOK, 512], BF16)
        self.QKT = self.dscr("QKT", [512, NTOK], F32)
        self.KV_tm = self.dscr("KV_tm", [NTOK, 768], BF16)
        self.SGT = self.dscr("SGT", [512, NTOK], BF16)
        self.LG = self.dscr("LG", [NTOK, 512], F32)
        self.YT = self.dscr("YT", [D, NTOK], BF16)
        self.OB = self.dscr("OB", [512, NTOK], F32)
        self.XG = self.dscr("XG", [NEXP * CAP, D], BF16)
        self.YS = self.dscr("YS", [NSLOT * TS, D], F32)

        with contextlib.ExitStack() as st:
            self.S = S = Sched(nc, st)
            self.ps = [st.enter_context(nc.psum_tensor(f"ps{i}", [128, 512], F32)) for i in range(8)]
            self.Tps = TL(8)
            self.ps_i = 0
            self.Tc = Tc = T()
            self.ones_bf = self.sb(st, [128, 128], BF16, "ones")
            self.ones_f = self.sb(st, [1, 128], F32, "onesf")
            self.eps_c = self.sb(st, [128, 1], F32, "eps")
            self.ident = self.sb(st, [128, 128], F32, "ident")
            self.modt = [self.sb(st, [128, 96], F32, f"modt{l}") for l in range(DEPTH)]
            self.A1 = [self.sb(st, [128, 16], F32, f"A1{l}") for l in range(DEPTH)]
            self.A2 = [self.sb(st, [128, 16], F32, f"A2{l}") for l in range(DEPTH)]
            self.Tmod = T()
            self.memset("pool", self.ones_bf[:], 1.0, [Tc])
            self.memset("pool", self.ones_f[:], 1.0, [Tc])
            self.memset("pool", self.eps_c[:], EPS, [Tc])
            S.dma("sp", self.ident[:], I["ident"][:, :], writes=[Tc])
            self.mod_init(st)
            with contextlib.ExitStack() as stm:
                wm0 = [self.sb(stm, [128, 8 * 1024], BF16, "wm") for _ in range(2)]
                Twm0 = TL(2)
                for m in range(2):
                    self.mod_block(0, m, wm0[m], Twm0[m], self.Tmod)
            S.barrier()
            pending = [(0, m) for m in range(2, 6)] + [(1, m) for m in range(6)]
            self.Tmod2 = T()
            for l in range(DEPTH):
                last = l == DEPTH - 1
                xsrc = I["xt"] if l == 0 else self.XS
                self.phase_proj(l, xsrc, pending if l == 0 else [])
                S.barrier()
                if self.stop_after == f"proj{l}":
                    break
                self.phase_gla(l)
                S.barrier()
                if self.stop_after == f"gla{l}":
                    break
                self.phase_fourier(l, do_ctx=not last)
                S.barrier()
                if self.stop_after == f"four{l}":
                    break
                self.phase_outproj(l, xsrc, do_ctx=not last)
                S.barrier()
                if self.stop_after == f"outp{l}":
                    break
                if l % 2 == 1:
                    self.phase_moe(l)
                else:
                    self.phase_ffn(l, last)
                S.barrier()
                if self.stop_after == f"ffn{l}":
                    break
            S.barrier()
            S.emit_all()
        return nc

    def mod_init(self, st):
        S, I = self.S, self.I
        cv = self.sb(st, [128, 16], F32)
        self.m_cs = self.sb(st, [128, 16], BF16)
        self.m_bm = [self.sb(st, [128, 48], F32) for _ in range(DEPTH)]
        self.m_nw = {}
        self.m_tmp = self.sb(st, [128, 16], F32)
        self.Tmc, self.Tmtmp = T(), T()
        Tcv = T()
        S.dma("sp", cv[:], I["cvec"][:, :], writes=[Tcv])
        self.act(self.m_cs[:], cv[:], AF.Silu, [Tcv], [self.Tmc])
        for l in range(DEPTH):
            S.dma("sp", self.m_bm[l][:], I[f"b_mod{l}"][:, :], writes=[self.Tmc])
            for nm in (f"nmw{l}", f"nfw{l}"):
                self.m_nw[nm] = self.sb(st, [128, 8], F32)
                S.dma("sp", self.m_nw[nm][:], I[nm][:, :], writes=[self.Tmc])

    def mod_block(self, l, m, wm, Twm, Tm):
        self.mod_load(l, m, wm, Twm)
        self.mod_compute(l, m, wm, Twm, Tm)

    def mod_load(self, l, m, wm, Twm):
        S, I = self.S, self.I
        for hh in range(2):
            S.dma("pool", wm[:, hh * 4096:(hh + 1) * 4096].rearrange("p (k n) -> p k n", k=4),
                  I[f"w_mod{l}"][hh * 512:(hh + 1) * 512, m * 1024:(m + 1) * 1024].rearrange("(k p) n -> p k n", p=128),
                  writes=[Twm])

    def mod_compute(self, l, m, wm, Twm, Tm):
        S, I = self.S, self.I
        p, Tp = self.psn()
        for j in range(8):
            for k in range(8):
                self.mm(p[:, j:j + 9:8], wm[:, k * 1024 + j * 128:k * 1024 + (j + 1) * 128],
                        self.m_cs[:, k:k + 9:8], j == 0 and k == 0, k == 7, [Twm, self.Tmc], [Tp])
        for a in range(2):
            self.tt("dve", self.modt[l][:, a * 48 + m * 8:a * 48 + (m + 1) * 8], p[:, a * 8:(a + 1) * 8],
                    self.m_bm[l][:, m * 8:(m + 1) * 8], ALU.add, [Tp, self.Tmc], [Tm])
        if m in (1, 4):
            nm, dst, off = (f"nmw{l}", self.A1[l], 8) if m == 1 else (f"nfw{l}", self.A2[l], 32)
            tmp = self.m_tmp
            for a in range(2):
                self.ts("dve", tmp[:, a * 8:(a + 1) * 8], self.modt[l][:, a * 48 + off:a * 48 + off + 8], 1.0, None,
                        ALU.add, None, [Tm], [self.Tmtmp])
                self.tt("dve", dst[:, a * 8:(a + 1) * 8], tmp[:, a * 8:(a + 1) * 8], self.m_nw[nm][:], ALU.mult,
                        [self.Tmtmp, self.Tmc], [Tm])

    def norm_mod(self, xt, Txt, n, A, Bv, a, hT, ThT, sq, Tsq, rstd, Trs, tmpb, Ttmp, hF=None, ThF=None, xstride=None):
        xs = xstride or n
        for k in range(8):
            self.act(sq[:, k * n:(k + 1) * n], xt[:, k * xs:k * xs + n], AF.Square, [Txt], [Tsq[k]])
        p, Tp = self.psn()
        for k in range(8):
            self.mm(p[:, :n], self.ones_bf[:], sq[:, k * n:(k + 1) * n], k == 0, k == 7, [Tsq[k], self.Tc], [Tp])
        self.rstd_from_ps(rstd, Trs, p, Tp, n, 1.0 / D)
        for k in range(8):
            tb = k % len(tmpb)
            self.stt(tmpb[tb][:, :n], xt[:, k * xs:k * xs + n], A[:, a * 8 + k:a * 8 + k + 1], rstd[:, :n],
                     ALU.mult, ALU.mult, [Txt, Trs, self.Tmod], [Ttmp[tb]])
            self.act(hT[:, k * n:(k + 1) * n], tmpb[tb][:, :n], AF.Identity, [Ttmp[tb], self.Tmod], [ThT[k]],
                     bias=Bv[:, a * 48 + k:a * 48 + k + 1])
            if hF is not None:
                self.ts("pool", hF[:, k * n:(k + 1) * n], tmpb[tb][:, :n], Bv[:, a * 48 + k:a * 48 + k + 1], None,
                        ALU.add, None, [Ttmp[tb], self.Tmod], [ThF[k]])

    def phase_proj(self, l, xsrc, pending=()):
        S, I = self.S, self.I
        pending = list(pending)
        with contextlib.ExitStack() as st:
            if pending:
                wmp = [self.sb(st, [128, 8 * 1024], BF16, "wmp") for _ in range(2)]
                Twmp = TL(2)
                njob = 0
            win = self.sb(st, [128, 8 * IN_W], BF16, "win")
            Twin2 = [TL(2) for _ in range(8)]
            for hf, (c0, c1) in enumerate(((0, 1040), (1040, 2080))):
                for k in range(8):
                    S.dma("pool", win[:, k * IN_W + c0:k * IN_W + c1], I[f"w_in{l}"][k * 128:(k + 1) * 128, c0:c1], writes=[Twin2[k][hf]])

            def Tw(k, col, w):
                if col + w <= 1040:
                    return [Twin2[k][0]]
                if col >= 1040:
                    return [Twin2[k][1]]
                return Twin2[k]
            if pending:
                self.mod_load(*pending[0], wmp[0], Twmp[0])
                njob = 1
            wg = self.sb(st, [16, 512], F32)
            bg = self.sb(st, [1, 512], F32)
            Twg = T()
            S.dma("sp", wg[:], I[f"wg{l}"][:, :], writes=[Twg])
            S.dma("sp", bg[:], I[f"bg{l}"][:, :], writes=[Twg])
            xt = [self.sb(st, [128, 8 * 512], F32, "xt") for _ in range(2)]
            Txt = TL(2)
            sq = self.sb(st, [128, 8 * 512], BF16, "sq")
            Tsq = TL(8)
            rstd = self.sb(st, [128, 512], F32)
            Trs = T()
            tmpb = [self.sb(st, [128, 512], F32) for _ in range(3)]
            Ttmp = TL(3)
            hT = [self.sb(st, [128, 8 * 512], BF16, "hT") for _ in range(2)]
            ThT = [TL(8) for _ in range(2)]
            u_st = [self.sb(st, [128, 4 * 512], BF16) for _ in range(2)]
            Tu = TL(2)
            qk_st = [self.sb(st, [128, 4 * 512], F32) for _ in range(2)]
            Tqk = TL(2)
            kv_st = [self.sb(st, [128, 4 * 768], BF16) for _ in range(2)]
            Tkv = TL(2)
            sg_st = [self.sb(st, [128, 4 * 512], BF16) for _ in range(2)]
            Tsg = TL(2)
            lfb = [self.sb(st, [16, 2 * 512], F32) for _ in range(2)]
            Tlfb = TL(2)
            lg_st = [self.sb(st, [128, 4 * 512], F32) for _ in range(2)]
            Tlg = TL(2)
            etmp = [self.sb(st, [128, 512], F32) for _ in range(2)]
            Tet = TL(2)
            for ti, (c0, n) in enumerate(TILES):
                a = 1 if ti == 0 else 0
                b = ti % 2
                nb = n // 128
                S.dma("sp", xt[b][:, :8 * n].rearrange("p (k n) -> p k n", k=8),
                      xsrc[:, c0:c0 + n].rearrange("(k p) n -> p k n", p=128), writes=[Txt[b]])
                self.norm_mod(xt[b], Txt[b], n, self.A1[l], self.modt[l], a, hT[b], ThT[b], sq, Tsq, rstd, Trs, tmpb, Ttmp)
                h = hT[b]
                Th = ThT[b]
                for tb in range(nb):
                    p, Tp = self.psn()
                    for k in range(8):
                        self.mm(p[:, :512], h[:, k * n + tb * 128:k * n + (tb + 1) * 128], win[:, k * IN_W:k * IN_W + 512],
                                k == 0, k == 7, [Th[k]] + Tw(k, 0, 512), [Tp])
                    self.cp("dve" if tb % 2 == 0 else "act", u_st[b][:, tb * 512:(tb + 1) * 512], p[:, :512], [Tp], [Tu[b]])
                S.dma("pool", self.U_tm[c0:c0 + n, :].rearrange("(t p) n -> p t n", p=128),
                      u_st[b][:, :nb * 512].rearrange("p (t n) -> p t n", t=nb), reads=[Tu[b]])
                for j in range(4):
                    p, Tp = self.psn()
                    col = 512 + j * 128
                    for k in range(8):
                        self.mm(p[:, :n], win[:, k * IN_W + col:k * IN_W + col + 128], h[:, k * n:(k + 1) * n],
                                k == 0, k == 7, [Th[k]] + Tw(k, col, 128), [Tp])
                    self.cp("act" if j % 2 == 0 else "dve", qk_st[b][:, j * n:(j + 1) * n], p[:, :n], [Tp], [Tqk[b]])
                S.dma("pool", self.QKT[:, c0:c0 + n].rearrange("(j p) n -> p j n", p=128),
                      qk_st[b][:, :4 * n].rearrange("p (j n) -> p j n", j=4), reads=[Tqk[b]])
                for tb in range(nb):
                    p, Tp = self.psn()
                    for k in range(8):
                        self.mm(p[:, :256], h[:, k * n + tb * 128:k * n + (tb + 1) * 128], win[:, k * IN_W + 768:k * IN_W + 1024],
                                k == 0, k == 7, [Th[k]] + Tw(k, 768, 256), [Tp])
                    self.cp("dve", kv_st[b][:, tb * 768:tb * 768 + 256], p[:, :256], [Tp], [Tkv[b]])
                    p, Tp = self.psn()
                    for k in range(8):
                        self.mm(p[:, :512], h[:, k * n + tb * 128:k * n + (tb + 1) * 128], win[:, k * IN_W + 1024:k * IN_W + 1536],
                                k == 0, k == 7, [Th[k]] + Tw(k, 1024, 512), [Tp])
                    self.cp("act", kv_st[b][:, tb * 768 + 256:(tb + 1) * 768], p[:, :512], [Tp], [Tkv[b]])
                S.dma("pool", self.KV_tm[c0:c0 + n, :].rearrange("(t p) n -> p t n", p=128),
                      kv_st[b][:, :nb * 768].rearrange("p (t n) -> p t n", t=nb), reads=[Tkv[b]])
                for j in range(4):
                    p, Tp = self.psn()
                    col = 1536 + j * 128
                    for k in range(8):
                        self.mm(p[:, :n], win[:, k * IN_W + col:k * IN_W + col + 128], h[:, k * n:(k + 1) * n],
                                k == 0, k == 7, [Th[k]] + Tw(k, col, 128), [Tp])
                    self.act(sg_st[b][:, j * n:(j + 1) * n], p[:, :n], AF.Silu, [Tp], [Tsg[b]])
                S.dma("pool", self.SGT[:, c0:c0 + n].rearrange("(j p) n -> p j n", p=128),
                      sg_st[b][:, :4 * n].rearrange("p (j n) -> p j n", j=4), reads=[Tsg[b]])
                for dd in range(2):
                    p, Tp = self.psn()
                    col = 2048 + dd * 16
                    for k in range(8):
                        self.mm(p[0:16, :n], win[:, k * IN_W + col:k * IN_W + col + 16], h[:, k * n:(k + 1) * n],
                                k == 0, k == 7, [Th[k]] + Tw(k, col, 16), [Tp])
                    self.cp("dve", lfb[b][:, dd * 512:dd * 512 + n], p[0:16, :n], [Tp], [Tlfb[b]])
                for tb in range(nb):
                    for dd in range(2):
                        p, Tp = self.psn()
                        self.mm(p[:, :256], lfb[b][:, dd * 512 + tb * 128:dd * 512 + (tb + 1) * 128], wg[:, dd * 256:(dd + 1) * 256],
                                True, False, [Tlfb[b], Twg], [Tp])
                        self.mm(p[:, :256], self.ones_f[:, :], bg[:, dd * 256:(dd + 1) * 256], False, True, [self.Tc, Twg], [Tp])
                        e = (tb * 2 + dd) % 2
                        self.act(etmp[e][:, :256], p[:, :256], AF.Exp, [Tp], [Tet[e]], scale=-1.0)
                        self.act(lg_st[b][:, tb * 512 + dd * 256:tb * 512 + (dd + 1) * 256], etmp[e][:, :256], AF.Ln,
                                 [Tet[e]], [Tlg[b]], bias=1.0)
                S.dma("pool", self.LG[c0:c0 + n, :].rearrange("(t p) n -> p t n", p=128),
                      lg_st[b][:, :nb * 512].rearrange("p (t n) -> p t n", t=nb), reads=[Tlg[b]])
                if pending:
                    if njob > 0:
                        self.mod_compute(*pending[njob - 1], wmp[(njob - 1) % 2], Twmp[(njob - 1) % 2], self.Tmod2)
                    if njob < len(pending):
                        self.mod_load(*pending[njob], wmp[njob % 2], Twmp[njob % 2])
                        njob += 1
            while pending and njob <= len(pending):
                self.mod_compute(*pending[njob - 1], wmp[(njob - 1) % 2], Twmp[(njob - 1) % 2], self.Tmod2)
                if njob < len(pending):
                    self.mod_load(*pending[njob], wmp[njob % 2], Twmp[njob % 2])
                njob += 1

    def phase_gla(self, l):
        S, I = self.S, self.I
        with contextlib.ExitStack() as st:
            tri = self.sb(st, [128, 512], F32, "tri")
            mask = self.sb(st, [128, 1024], BF16, "mask")
            gnw = self.sb(st, [128, 1], F32)
            Tk = T()
            S.dma("sp", tri[:], I["tri"][:, :], writes=[Tk])
            S.dma("sp", mask[:], I["mask"][:, :], writes=[Tk])
            S.dma("sp", gnw[:], I[f"gnw{l}"][:, :], writes=[Tk])
            obuf = self.sb(st, [128, 4 * NTOK], F32, "obuf")
            Tob = TL(NCH)
            S32 = [self.sb(st, [128, 512], F32) for _ in range(2)]
            Sbf = [self.sb(st, [128, 512], BF16) for _ in range(2)]
            TS32 = TL(2)
            TSbf = TL(2)
            for d in range(2):
                self.memset("pool", S32[d][:], 0.0, [TS32[d]])
                self.memset("pool", Sbf[d][:], 0.0, [TSbf[d]])
            NB = 2
            mk2 = lambda W, dt: [self.sb(st, [128, 2 * W], dt) for _ in range(NB)]
            V = lambda t, d, W: t[:, d * W:(d + 1) * W]
            V3 = lambda t: t[:, :].rearrange("p (d w) -> p d w", d=2)
            qk_in, kv_in, lg_in = mk2(512, F32), mk2(768, BF16), mk2(256, F32)
            Tin = [TL(NB) for _ in range(2)]
            Eq, Ek, Er = mk2(256, F32), mk2(256, F32), mk2(256, F32)
            TE = [TL(NB) for _ in range(2)]
            qtl, ktl, kht, scm = mk2(512, BF16), mk2(256, BF16), mk2(256, BF16), mk2(512, BF16)
            for bb in range(NB):
                self.memset("pool", qtl[bb][:, :], 0.0, [])
            Tq = [TL(NB) for _ in range(2)]
            Tkh = [TL(NB) for _ in range(2)]
            Tsc = [TL(NB) for _ in range(2)]
            order = [list(range(NCH)), [1, 0] + list(range(NCH - 1, 1, -1))]
            step_of = [{c: i for i, c in enumerate(order[d])} for d in range(2)]
            for i in range(NCH):
                b = i % NB
                cs = [order[0][i], order[1][i]]
                psA, psB, psC, psD = [None] * 2, [None] * 2, [None] * 2, [None] * 2
                for d in range(2):
                    c0 = cs[d] * 128
                    S.dma("sp", V(qk_in[b], d, 512).rearrange("p (j n) -> p j n", j=4),
                          self.QKT[:, c0:c0 + 128].rearrange("(j p) n -> p j n", p=128), writes=[Tin[d][b]])
                    S.dma("sp", V(kv_in[b], d, 768), self.KV_tm[c0:c0 + 128, :], writes=[Tin[d][b]])
                    S.dma("sp", V(lg_in[b], d, 256), self.LG[c0:c0 + 128, d * 256:(d + 1) * 256], writes=[Tin[d][b]])
                for d in range(2):
                    psA[d] = self.psn()
                    p, Tp = psA[d]
                    lg = V(lg_in[b], d, 256)
                    self.mm(p[:, 0:128], lg[:, 0:128], tri[:, (2 * d) * 128:(2 * d + 1) * 128], True, True, [Tin[d][b], Tk], [Tp])
                    self.mm(p[:, 128:256], lg[:, 128:256], tri[:, (2 * d) * 128:(2 * d + 1) * 128], True, True, [Tin[d][b], Tk], [Tp])
                    self.mm(p[:, 256:512], tri[:, (2 * d + 1) * 128:(2 * d + 2) * 128], lg[:, 0:256], True, True, [Tin[d][b], Tk], [Tp])
                for d in range(2):
                    p, Tp = psA[d]
                    self.act(V(Eq[b], d, 256), p[:, 0:256], AF.Exp, [Tp], [TE[d][b]])
                    self.act(V(Ek[b], d, 256), p[:, 0:256], AF.Exp, [Tp], [TE[d][b]], scale=-1.0)
                    self.act(V(Er[b], d, 256), p[:, 256:512], AF.Exp, [Tp], [TE[d][b]])
                rd = [Tin[0][b], Tin[1][b], TE[0][b], TE[1][b]]
                self.stt(V3(qtl[b])[0:64, :, 0:256], V3(qk_in[b])[0:64, :, 0:256], 0.125, V3(Eq[b])[0:64, :, :], ALU.mult, ALU.mult,
                         rd, [Tq[0][b], Tq[1][b]])
                self.stt(V3(qtl[b])[64:128, :, 256:512], V3(qk_in[b])[64:128, :, 0:256], 0.125, V3(Eq[b])[64:128, :, :], ALU.mult, ALU.mult,
                         rd, [Tq[0][b], Tq[1][b]])
                self.tt("dve", V3(ktl[b])[:, :, :], V3(qk_in[b])[:, :, 256:512], V3(Ek[b])[:, :, :], ALU.mult, rd, [Tq[0][b], Tq[1][b]])
                self.tt("dve", V3(kht[b])[:, :, :], V3(kv_in[b])[:, :, 0:256], V3(Er[b])[:, :, :], ALU.mult, rd, [Tkh[0][b], Tkh[1][b]])
                for d in range(2):
                    psB[d] = self.psn()
                    p, Tp = psB[d]
                    for h in range(4):
                        half = h // 2
                        qo = (h % 2) * 256 + half * 128
                        self.mm(p[:, h * 128:(h + 1) * 128], V(ktl[b], d, 256)[:, half * 128:(half + 1) * 128],
                                V(qtl[b], d, 512)[:, qo:qo + 128], True, True, [Tq[d][b]], [Tp])
                    self.tt("dve", V(scm[b], d, 512), p[:, :], mask[:, d * 512:(d + 1) * 512], ALU.mult, [Tp, Tk], [Tsc[d][b]])
                for d in range(2):
                    psC[d] = self.psn()
                    p, Tp = psC[d]
                    c = cs[d]
                    for h in range(4):
                        half = h // 2
                        self.mm(p[:, h * 128:(h + 1) * 128], V(kv_in[b], d, 768)[:, 256 + h * 128:256 + (h + 1) * 128],
                                V(scm[b], d, 512)[:, h * 128:(h + 1) * 128], True, False, [Tin[d][b], Tsc[d][b]], [Tp])
                        qo = (h % 2) * 256 + half * 128
                        self.mm(p[:, h * 128:(h + 1) * 128], Sbf[d][:, h * 128:(h + 1) * 128],
                                V(qtl[b], d, 512)[:, qo:qo + 128], False, True, [TSbf[d], Tq[d][b]], [Tp])
                    first = step_of[d][c] < step_of[1 - d][c]
                    oview = obuf[:, :].rearrange("p (h n) -> p h n", h=4)[:, :, c * 128:(c + 1) * 128]
                    pview = p[:, :].rearrange("p (h n) -> p h n", h=4)
                    if first:
                        self.cp("act", oview, pview, [Tp], [Tob[c]])
                    else:
                        self.tt("dve", oview, oview, pview, ALU.add, [Tp], [Tob[c]])
                for d in range(2):
                    psD[d] = self.psn()
                    p, Tp = psD[d]
                    tl = 127 if d == 0 else 0
                    for half in range(2):
                        self.mm(p[:, half * 256:(half + 1) * 256], V(kht[b], d, 256)[:, half * 128:(half + 1) * 128],
                                V(kv_in[b], d, 768)[:, 256 + half * 256:256 + (half + 1) * 256], True, True, [Tkh[d][b], Tin[d][b]], [Tp])
                    for half in range(2):
                        self.stt(S32[d][:, half * 256:(half + 1) * 256], S32[d][:, half * 256:(half + 1) * 256],
                                 V(Eq[b], d, 256)[:, half * 128 + tl:half * 128 + tl + 1],
                                 p[:, half * 256:(half + 1) * 256], ALU.mult, ALU.add, [Tp, TE[d][b]], [TS32[d]])
                    self.cp("act", Sbf[d][:, :], S32[d][:, :], [TS32[d]], [TSbf[d]])
            sgt = [self.sb(st, [128, 4 * 512], BF16) for _ in range(2)]
            Tsg = TL(2)
            sq = self.sb(st, [128, 4 * 512], BF16)
            Tsq = TL(4)
            rs = [self.sb(st, [128, 512], F32) for _ in range(2)]
            Trs = TL(2)
            t1 = [self.sb(st, [128, 512], F32) for _ in range(2)]
            Tt1 = TL(2)
            yg = [self.sb(st, [128, 4 * 512], BF16) for _ in range(2)]
            Tyg = TL(2)
            if "OB" in self.dbg:
                S.dma("pool", self.OB[:, :].rearrange("(h p) n -> p h n", p=128),
                      obuf[:, :].rearrange("p (h n) -> p h n", h=4), reads=Tob)
            for ti, (c0, n) in enumerate(TILES):
                b = ti % 2
                Tobs = [Tob[c] for c in range(c0 // 128, (c0 + n) // 128)]
                S.dma("sp", sgt[b][:, :4 * n].rearrange("p (j n) -> p j n", j=4),
                      self.SGT[:, c0:c0 + n].rearrange("(j p) n -> p j n", p=128), writes=[Tsg[b]])
                for h in range(4):
                    self.act(sq[:, h * n:(h + 1) * n], obuf[:, h * NTOK + c0:h * NTOK + c0 + n], AF.Square, Tobs, [Tsq[h]])
                for h in range(4):
                    p, Tp = self.psn()
                    self.mm(p[:, :n], self.ones_bf[:], sq[:, h * n:(h + 1) * n], True, True, [Tsq[h], self.Tc], [Tp])
                    e = h % 2
                    self.rstd_from_ps(rs[e], Trs[e], p, Tp, n, 1.0 / 128)
                    self.stt(t1[e][:, :n], obuf[:, h * NTOK + c0:h * NTOK + c0 + n], gnw[:, 0:1], rs[e][:, :n], ALU.mult, ALU.mult,
                             Tobs + [Trs[e], Tk], [Tt1[e]])
                    self.tt("pool", yg[b][:, h * n:(h + 1) * n], t1[e][:, :n], sgt[b][:, h * n:(h + 1) * n], ALU.mult,
                            [Tt1[e], Tsg[b]], [Tyg[b]])
                S.dma("pool", self.YT[512:1024, c0:c0 + n].rearrange("(j p) n -> p j n", p=128),
                      yg[b][:, :4 * n].rearrange("p (j n) -> p j n", j=4), reads=[Tyg[b]])

    def phase_fourier(self, l, do_ctx):
        S, I = self.S, self.I
        with contextlib.ExitStack() as st:
            U = self.sb(st, [128, NCH * 512], BF16, "U")
            TU = T()
            for g0 in range(0, NCH, 17):
                S.dma("sp", U[:, g0 * 512:(g0 + 17) * 512].rearrange("p (t n) -> p t n", t=17),
                      self.U_tm[g0 * 128:(g0 + 17) * 128, :].rearrange("(t p) n -> p t n", p=128), writes=[TU])
            cs128 = self.sb(st, [128, 256], BF16)
            jsh = self.sb(st, [128, 256], BF16)
            alt = self.sb(st, [1, SEQ], BF16)
            Tcs = T()
            S.dma("sp", cs128[:], I["cs128"][:, :], writes=[Tcs])
            S.dma("sp", jsh[:], I["jsh"][:, :], writes=[Tcs])
            S.dma("sp", alt[:], I["alt"][:, :], writes=[Tcs])
            NW = 256
            HC = 16
            cl = [self.sb(st, [128, HC * NW], BF16, "cl") for _ in range(2)]
            sl = [self.sb(st, [128, HC * NW], BF16, "sl") for _ in range(2)]
            Tcl = TL(2)
            z = self.sb(st, [128, 8 * NW], BF16)
            Tz = TL(4)
            yf = [self.sb(st, [128, 4 * NW], BF16) for _ in range(2)]
            Tyf = TL(2)
            UE = self.sb(st, [128, HC * 512], BF16, "UE")
            UO = self.sb(st, [128, HC * 512], BF16, "UO")
            TUE = TL(HC)
            for tc in range(HC):
                p, Tp = self.psn()
                self.mm(p[:, :512], jsh[:, 0:128], U[:, (2 + 31 - tc) * 512:(2 + 32 - tc) * 512], True, tc == 0, [TU, Tcs], [Tp])
                if tc > 0:
                    self.mm(p[:, :512], jsh[:, 128:256], U[:, (2 + 32 - tc) * 512:(2 + 33 - tc) * 512], False, True, [TU, Tcs], [Tp])
                self.tt("dve", UE[:, tc * 512:(tc + 1) * 512], U[:, (2 + tc) * 512:(3 + tc) * 512], p[:, :512], ALU.add, [TU, Tp], [TUE[tc]])
                self.tt("dve", UO[:, tc * 512:(tc + 1) * 512], U[:, (2 + tc) * 512:(3 + tc) * 512], p[:, :512], ALU.subtract, [TU, Tp], [TUE[tc]])
            jobs = []
            if do_ctx:
                jobs.append(("c", 0))
            for nt in range(SEQ // NW):
                jobs.append(("x", nt))
            for ji, (kind, nt) in enumerate(jobs):
                b = ji % 2
                banks = [self.psn() for _ in range(4)]
                if kind == "c":
                    col0 = 0
                    S.dma("sp", cl[b][:, :2 * NW].rearrange("p (t n) -> p t n", t=2),
                          I["dftc256"][:, :].rearrange("(t p) n -> p t n", p=128), writes=[Tcl[b]])
                    S.dma("sp", sl[b][:, :2 * NW].rearrange("p (t n) -> p t n", t=2),
                          I["dfts256"][:, :].rearrange("(t p) n -> p t n", p=128), writes=[Tcl[b]])
                    for tc in range(2):
                        for g in range(4):
                            p, Tp = banks[g]
                            lhs = U[:, tc * 512 + g * 128:tc * 512 + (g + 1) * 128]
                            self.mm(p[:, 0:NW], lhs, cl[b][:, tc * NW:(tc + 1) * NW], tc == 0, tc == 1, [TU, Tcl[b]], [Tp])
                            self.mm(p[:, NW:2 * NW], lhs, sl[b][:, tc * NW:(tc + 1) * NW], False, tc == 1, [TU, Tcl[b]], [Tp])
                else:
                    col0 = CTX + nt * NW
                    S.dma("sp", cl[b][:, :].rearrange("p (t n) -> p t n", t=HC),
                          I["dftc"][0:HC * 128, nt * NW:(nt + 1) * NW].rearrange("(t p) n -> p t n", p=128), writes=[Tcl[b]])
                    S.dma("sp", sl[b][:, :].rearrange("p (t n) -> p t n", t=HC),
                          I["dfts"][0:HC * 128, nt * NW:(nt + 1) * NW].rearrange("(t p) n -> p t n", p=128), writes=[Tcl[b]])
                    for tc in range(HC):
                        for g in range(4):
                            p, Tp = banks[g]
                            self.mm(p[:, 0:NW], UE[:, tc * 512 + g * 128:tc * 512 + (g + 1) * 128], cl[b][:, tc * NW:(tc + 1) * NW],
                                    tc == 0, False, [TUE[tc], Tcl[b]], [Tp])
                            self.mm(p[:, NW:2 * NW], UO[:, tc * 512 + g * 128:tc * 512 + (g + 1) * 128], sl[b][:, tc * NW:(tc + 1) * NW],
                                    False, tc == HC - 1, [TUE[tc], Tcl[b]], [Tp])
                    for g in range(4):
                        p, Tp = banks[g]
                        self.mm(p[:, 0:NW], U[0:1, (2 + HC) * 512 + g * 128:(2 + HC) * 512 + (g + 1) * 128], alt[0:1, nt * NW:(nt + 1) * NW],
                                False, True, [TU, Tcs], [Tp])
                for g in range(4):
                    p, Tp = banks[g]
                    self.cp("act" if g % 2 == 0 else "dve", z[:, g * 2 * NW:(g + 1) * 2 * NW], p[:, :2 * NW], [Tp], [Tz[g]])
                pa, Tpa = self.psn()
                pb, Tpb = self.psn()
                for g in range(4):
                    p, Tp = (pa, Tpa) if g < 2 else (pb, Tpb)
                    o = (g % 2) * NW
                    self.mm(p[:, o:o + NW], cs128[:, 0:128], z[:, g * 2 * NW:g * 2 * NW + NW], g % 2 == 0, False, [Tz[g], Tcs], [Tp])
                    self.mm(p[:, o:o + NW], cs128[:, 128:256], z[:, g * 2 * NW + NW:(g + 1) * 2 * NW], False, True, [Tz[g], Tcs], [Tp])
                self.cp("act", yf[b][:, 0:2 * NW], pa[:, :2 * NW], [Tpa], [Tyf[b]])
                self.cp("dve", yf[b][:, 2 * NW:4 * NW], pb[:, :2 * NW], [Tpb], [Tyf[b]])
                S.dma("pool", self.YT[0:512, col0:col0 + NW].rearrange("(j p) n -> p j n", p=128),
                      yf[b][:, :].rearrange("p (j n) -> p j n", j=4), reads=[Tyf[b]])

    def phase_outproj(self, l, xsrc, do_ctx):
        S, I = self.S, self.I
        with contextlib.ExitStack() as st:
            wo = self.sb(st, [128, 8 * D], BF16, "wo")
            Two = T()
            for hh in range(2):
                S.dma("pool", wo[:, hh * 4096:(hh + 1) * 4096].rearrange("p (k n) -> p k n", k=4),
                      I[f"w_out{l}"][hh * 512:(hh + 1) * 512, :].rearrange("(k p) n -> p k n", p=128), writes=[Two])
            yc = [self.sb(st, [128, 8 * 512], BF16) for _ in range(2)]
            Tyc = TL(2)
            xt = [self.sb(st, [128, 8 * 512], F32) for _ in range(2)]
            Txt = TL(2)
            tiles = TILES if do_ctx else TILES[1:]
            for ti, (c0, n) in enumerate(tiles):
                a = 1 if c0 == 0 else 0
                b = ti % 2
                S.dma("sp", yc[b][:, :8 * n].rearrange("p (k n) -> p k n", k=8),
                      self.YT[:, c0:c0 + n].rearrange("(k p) n -> p k n", p=128), writes=[Tyc[b]])
                S.dma("sp", xt[b][:, :8 * n].rearrange("p (k n) -> p k n", k=8),
                      xsrc[:, c0:c0 + n].rearrange("(k p) n -> p k n", p=128), writes=[Txt[b]])
                for i in range(8):
                    p, Tp = self.psn()
                    for k in range(8):
                        self.mm(p[:, :n], wo[:, k * D + i * 128:k * D + (i + 1) * 128], yc[b][:, k * n:(k + 1) * n],
                                k == 0, k == 7, [Two, Tyc[b]], [Tp])
                    self.stt(xt[b][:, i * n:(i + 1) * n], p[:, :n], self.modt[l][:, a * 48 + 16 + i:a * 48 + 17 + i],
                             xt[b][:, i * n:(i + 1) * n], ALU.mult, ALU.add, [Tp, self.Tmod], [Txt[b]])
                S.dma("pool", self.XS[:, c0:c0 + n].rearrange("(k p) n -> p k n", p=128),
                      xt[b][:, :8 * n].rearrange("p (k n) -> p k n", k=8), reads=[Txt[b]])


    def phase_moe(self, l):
        S, I = self.S, self.I
        NBLK = SEQ // 128
        a = 0
        NG = EFF // 512
        G = 4
        with contextlib.ExitStack() as st0:
            rw = self.sb(st0, [128, 8 * NEXP], F32)
            su = self.sb(st0, [128, 128], F32)
            ones128 = self.sb(st0, [128, 128], F32)
            identb = self.sb(st0, [128, 128], BF16)
            slotstart = self.sb(st0, [128, NSLOT], F32)
            gp7 = self.sb(st0, [128, NG], F32)
            fnw = self.sb(st0, [128, 8], F32)
            Tf = T()
            S.dma("sp", rw[:, :].rearrange("p (k e) -> p k e", k=8), I["router_w"].rearrange("(k p) e -> p k e", p=128), writes=[Tf])
            S.dma("sp", su[:], I["su"][:, :], writes=[Tf])
            S.dma("sp", identb[:], I["identb"][:, :], writes=[Tf])
            S.dma("sp", slotstart[:], I["slotstart"][:, :], writes=[Tf])
            S.dma("sp", gp7[:], I["gp7"][:, :], writes=[Tf])
            S.dma("sp", fnw[:], I["fnw"][:, :], writes=[Tf])
            self.memset("pool", ones128[:], 1.0, [Tf])
            IND = [self.sb(st0, [128, NBLK * 8], F32) for _ in range(2)]
            TOPA = self.sb(st0, [128, NBLK * 8], F32)
            R8A = self.sb(st0, [128, NBLK * 8], F32)
            BP = self.sb(st0, [128, (NBLK + 1) * 8], F32)
            ecap = self.sb(st0, [128, 8], F32)
            dsi = self.sb(st0, [128, 2 * NBLK], I32)
            Tds = TL(NBLK)
            TR8 = TL(NBLK)
            xidx = self.sb(st0, [128, 4 * NSLOT], I32)
            Txi = T()
            GK = [self.sb(st0, [128, NBLK], F32) for _ in range(2)]
            TI = TL(NBLK)
            TBP, TGK = T(), T()
            S.dma("sp", ecap[:], I["ecap"][:, :], writes=[Tf])
            S.dma("sp", BP[:, 0:8], I["ecap"][:, :], writes=[TBP])
            desti = self.sb(st0, [128, 2 * NBLK], I32)
            Tdd = T()
            widx = self.sb(st0, [128, NG * NSLOT], I32)
            Twi = T()
            TXG = T()
            with contextlib.ExitStack() as st:
                h2tm = self.sb(st, [128, NBLK * D], BF16, "h2tm")
                Th2tm = TL(NBLK)
                xt = [self.sb(st, [128, 8 * 512], F32, "xt") for _ in range(2)]
                Txt = TL(2)
                sq = self.sb(st, [128, 8 * 512], BF16)
                Tsq = TL(8)
                rstd = self.sb(st, [128, 512], F32)
                Trs = T()
                tmpb = [self.sb(st, [128, 512], F32) for _ in range(2)]
                Ttmp = TL(2)
                hFk = [self.sb(st, [128, 512], F32) for _ in range(2)]
                ThF = TL(2)
                h2T = [self.sb(st, [128, 8 * 512], BF16) for _ in range(2)]
                Th2T = [TL(8) for _ in range(2)]
                ind = [self.sb(st, [128, 8], F32) for _ in range(4)]
                Tind = TL(4)
                t8 = [[self.sb(st, [128, 8], F32) for _ in range(2)] for _ in range(4)]
                dsf = [self.sb(st, [128, 2], F32) for _ in range(4)]
                Tt8 = TL(4)
                for ti, (c0, n) in enumerate(TILES[1:]):
                    b = ti % 2
                    S.dma("sp", xt[b][:, :].rearrange("p (k n) -> p k n", k=8),
                          self.XS[:, c0:c0 + n].rearrange("(k p) n -> p k n", p=128), writes=[Txt[b]])
                    for k in range(8):
                        self.act(sq[:, k * n:(k + 1) * n], xt[b][:, k * n:(k + 1) * n], AF.Square, [Txt[b]], [Tsq[k]])
                    p, Tp = self.psn()
                    for k in range(8):
                        self.mm(p[:, :n], self.ones_bf[:], sq[:, k * n:(k + 1) * n], k == 0, k == 7, [Tsq[k], self.Tc], [Tp])
                    self.rstd_from_ps(rstd, Trs, p, Tp, n, 1.0 / D)
                    pl, Tpl = self.psn()
                    for k in range(8):
                        tb_ = k % 2
                        self.stt(tmpb[tb_][:, :n], xt[b][:, k * n:(k + 1) * n], self.A2[l][:, a * 8 + k:a * 8 + k + 1], rstd[:, :n],
                                 ALU.mult, ALU.mult, [Txt[b], Trs, self.Tmod], [Ttmp[tb_]])
                        self.act(h2T[b][:, k * n:(k + 1) * n], tmpb[tb_][:, :n], AF.Identity, [Ttmp[tb_], self.Tmod], [Th2T[b][k]],
                                 bias=self.modt[l][:, a * 48 + 24 + k:a * 48 + 25 + k])
                        self.act(hFk[tb_][:, :n], tmpb[tb_][:, :n], AF.Identity, [Ttmp[tb_], self.Tmod], [ThF[tb_]],
                                 bias=self.modt[l][:, a * 48 + 24 + k:a * 48 + 25 + k])
                        for tb in range(4):
                            self.mm(pl[:, tb * 8:(tb + 1) * 8], hFk[tb_][:, tb * 128:(tb + 1) * 128], rw[:, k * 8:(k + 1) * 8],
                                    k == 0 and tb == 0, k == 7, [ThF[tb_], Tf], [Tpl])
                    for tb in range(4):
                        blk = ti * 4 + tb
                        pt, Tpt = self.psn()
                        ptb = pt[:, :].bitcast(BF16)
                        for k in range(8):
                            S.op("pe", lambda e, ptb=ptb, k=k, src=h2T[b][:, k * n + tb * 128:k * n + (tb + 1) * 128]:
                                 e.transpose(ptb[:, k * 128:(k + 1) * 128], src, identb[:, :]), reads=[Th2T[b][k], Tf], writes=[Tpt])
                        self.cp("act", h2tm[:, blk * D:(blk + 1) * D], ptb[:, 0:D], [Tpt], [Th2tm[blk]])
                        plb = pl[:, tb * 8:(tb + 1) * 8]
                        top8 = TOPA[:, blk * 8:(blk + 1) * 8]
                        i1 = IND[0][:, blk * 8:(blk + 1) * 8]
                        i2 = IND[1][:, blk * 8:(blk + 1) * 8]
                        S.op("dve", lambda e, top8=top8, plb=plb: e.max(out=top8, in_=plb), reads=[Tpl], writes=[TI[blk]])
                        self.ts("dve", i1, plb, top8[:, 0:1], None, ALU.is_equal, None, [Tpl, TI[blk]], [TI[blk]])
                        self.ts("dve", i2, plb, top8[:, 1:2], None, ALU.is_equal, None, [Tpl, TI[blk]], [TI[blk]])
                        self.tt("dve", ind[tb][:, :], i1, i2, ALU.add, [TI[blk]], [Tind[tb]])
                        pp, Tpp = self.psn()
                        self.mm(pp[:, 0:8], su[:, :], ind[tb][:, :], True, True, [Tind[tb], Tf], [Tpp])
                        self.mm(pp[:, 8:16], ones128[:, :], ind[tb][:, :], False, True, [Tind[tb], Tf], [Tpp])
                        r8 = R8A[:, blk * 8:(blk + 1) * 8]
                        self.tt("dve", r8, BP[:, blk * 8:(blk + 1) * 8], pp[:, 0:8], ALU.add, [Tpp, TBP], [TR8[blk]])
                        self.tt("dve", BP[:, (blk + 1) * 8:(blk + 2) * 8], BP[:, blk * 8:(blk + 1) * 8], pp[:, 8:16], ALU.add, [Tpp, TBP], [TBP])
                        for kk in range(2):
                            self.tt("dve", t8[tb][kk][:, :], IND[kk][:, blk * 8:(blk + 1) * 8], r8, ALU.mult, [TI[blk], TR8[blk]], [Tt8[tb]])
                            S.op("dve", lambda e, tb=tb, kk=kk: e.reduce_sum(out=dsf[tb][:, kk:kk + 1], in_=t8[tb][kk][:, :], axis=mybir.AxisListType.X),
                                 reads=[Tt8[tb]], writes=[Tt8[tb]])
                        self.cp("dve", dsi[:, blk * 2:blk * 2 + 2], dsf[tb][:, 0:2], [Tt8[tb]], [Tds[blk]])
                        for kk in range(2):
                            def sc1(e, q=blk * 2 + kk, blk=blk):
                                return e.indirect_dma_start(out=self.XG[:, :], out_offset=bass.IndirectOffsetOnAxis(ap=dsi[:, q:q + 1], axis=0),
                                                            in_=h2tm[:, blk * D:(blk + 1) * D], in_offset=None,
                                                            bounds_check=self.breg(e, NEXP * CAP - 1), oob_is_err=False)
                            S.dma_custom("pool", sc1, reads=[Tds[blk], Th2tm[blk]], writes=[TXG])
                run = self.sb(st, [128, 8], F32)
                self.tt("dve", run[:, :], BP[:, NBLK * 8:(NBLK + 1) * 8], ecap[:, :], ALU.subtract, [TBP, Tf], [TBP])
                gd = self.sb(st, [128, NBLK], F32)
                Tg = T()
                self.tt("dve", gd[:, :], TOPA[:, 1:NBLK * 8:8], TOPA[:, 0:NBLK * 8:8], ALU.subtract, TI, [Tg])
                self.act(gd[:, :], gd[:, :], AF.Exp, [Tg], [Tg])
                self.ts("dve", gd[:, :], gd[:, :], 1.0, None, ALU.add, None, [Tg], [Tg])
                self.recip(GK[0][:, :], gd[:, :], [Tg], [TGK])
                self.ts("dve", GK[1][:, :], GK[0][:, :], -1.0, 1.0, ALU.mult, ALU.add, [TGK], [TGK])
                destf = self.sb(st, [128, 2 * NBLK], F32)
                Tm = [self.sb(st, [128, NBLK * 8], F32) for _ in range(2)]
                r32 = [self.sb(st, [128, NBLK], F32) for _ in range(2)]
                Tdf = TL(2)
                for kk in range(2):
                    dk = destf[:, kk * NBLK:(kk + 1) * NBLK]
                    self.tt("dve", Tm[kk][:, :], IND[kk][:, :], R8A[:, :], ALU.mult, TI + TR8, [Tdf[kk]])
                    S.op("dve", lambda e, kk=kk, dk=dk: e.reduce_sum(out=dk, in_=Tm[kk][:, :].rearrange("p (b e) -> p b e", e=8),
                                                                      axis=mybir.AxisListType.X), reads=[Tdf[kk]], writes=[Tdf[kk]])
                pad = self.sb(st, [128, 8], F32)
                base = self.sb(st, [128, 8], F32)
                ends = self.sb(st, [128, 8], F32)
                esf = self.sb(st, [128, NSLOT], F32)
                est = [self.sb(st, [128, NSLOT], F32) for _ in range(2)]
                widf = self.sb(st, [128, NG * NSLOT], F32)
                Tb = T()
                for e_ in range(8):
                    self.ts("dve", est[e_ % 2][:, :], slotstart[:, :], run[:, e_:e_ + 1], None, ALU.is_lt, None, [TBP, Tf, Tb], [Tb])
                    S.op("dve", lambda e, e_=e_: e.reduce_sum(out=pad[:, e_:e_ + 1], in_=est[e_ % 2][:, :], axis=mybir.AxisListType.X),
                         reads=[Tb], writes=[Tb])
                self.ts("dve", pad[:, :], pad[:, :], float(TS), None, ALU.mult, None, [Tb], [Tb])
                self.memset("dve", base[:, 0:1], 0.0, [Tb])
                for e_ in range(1, 8):
                    self.tt("dve", base[:, e_:e_ + 1], base[:, e_ - 1:e_], pad[:, e_ - 1:e_], ALU.add, [Tb], [Tb])
                self.tt("dve", ends[:, :], base[:, :], pad[:, :], ALU.add, [Tb], [Tb])
                bos = self.sb(st, [128, NSLOT], F32)
                bm = self.sb(st, [128, 8], F32)
                self.tt("dve", bm[:, :], base[:, :], ecap[:, :], ALU.subtract, [Tb, Tf], [Tb])
                self.memset("dve", esf[:, :], 0.0, [Tb])
                self.memset("dve", bos[:, :], 0.0, [Tb])
                for e_ in range(8):
                    self.ts("dve", est[e_ % 2][:, :], slotstart[:, :], ends[:, e_:e_ + 1], None, ALU.is_ge, None, [Tb, Tf], [Tb])
                    self.tt("dve", esf[:, :], esf[:, :], est[e_ % 2][:, :], ALU.add, [Tb], [Tb])
                    self.stt(bos[:, :], est[e_ % 2][:, :], pad[:, e_:e_ + 1], bos[:, :], ALU.mult, ALU.add, [Tb], [Tb])
                self.ts("dve", esf[:, :], esf[:, :], 7.0, None, ALU.min, None, [Tb], [Tb])
                for g in range(NG):
                    self.ts("dve", widf[:, g * NSLOT:(g + 1) * NSLOT], esf[:, :], float(NG * 128), gp7[:, g:g + 1], ALU.mult, ALU.add,
                            [Tb, Tf], [Tb])
                self.cp("dve", widx[:, :], widf[:, :], [Tb], [Twi])
                xoff = self.sb(st, [128, NSLOT], F32)
                xidf = self.sb(st, [128, 4 * NSLOT], F32)
                self.stt(xoff[:, :], esf[:, :], float(CAP), slotstart[:, :], ALU.mult, ALU.add, [Tb, Tf], [Tb])
                self.tt("dve", xoff[:, :], xoff[:, :], bos[:, :], ALU.subtract, [Tb], [Tb])
                for t_ in range(4):
                    self.ts("dve", xidf[:, t_ * NSLOT:(t_ + 1) * NSLOT], xoff[:, :], gp7[:, t_:t_ + 1], None, ALU.add, None, [Tb, Tf], [Tb])
                self.cp("dve", xidx[:, :], xidf[:, :], [Tb], [Txi])
                for e_ in range(8):
                    for kk in range(2):
                        dk = destf[:, kk * NBLK:(kk + 1) * NBLK]
                        self.stt(dk, IND[kk][:, e_:NBLK * 8:8], bm[:, e_:e_ + 1], dk, ALU.mult, ALU.add, TI + [Tb, Tdf[kk]], [Tdf[kk]])
                self.cp("dve", desti[:, :], destf[:, :], Tdf, [Tdd])
            S.barrier()
            with contextlib.ExitStack() as st:
                NWB = 3
                w13 = [self.sb(st, [128, 2 * 8 * 512], BF16, "w13") for _ in range(NWB)]
                w2 = [self.sb(st, [128, G * D], BF16, "w2") for _ in range(NWB)]
                Tw = TL(NWB)
                xg_tm = [self.sb(st, [128, 4 * D], BF16, "xgtm") for _ in range(2)]
                Txg = TL(2)
                xgT = [self.sb(st, [128, 8 * TS], BF16, "xgT") for _ in range(2)]
                TxgT = TL(2)
                actb = [self.sb(st, [128, G * TS], BF16, "actb") for _ in range(2)]
                Tab = [TL(G) for _ in range(2)]
                sil = [self.sb(st, [128, TS], F32) for _ in range(2)]
                Tsil = TL(2)
                acc = [self.sb(st, [128, 4 * D], F32, "acctm") for _ in range(2)]
                Tacc = [TL(4) for _ in range(2)]
                jobs = [(s_, gi) for s_ in range(NSLOT) for gi in range(NG)]
                NROW = NEXP * NG * 128

                def issue_w(ji):
                    s_, gi = jobs[ji]
                    wb = ji % NWB
                    ixap = widx[:, gi * NSLOT + s_:gi * NSLOT + s_ + 1]
                    for (wname, dst) in (("mw1", w13[wb][:, 0:4096]), ("mw3", w13[wb][:, 4096:8192]), ("mw2", w2[wb][:, :])):
                        def f(e, wname=wname, dst=dst, ixap=ixap):
                            return e.indirect_dma_start(out=dst, out_offset=None, in_=I[wname][:, :],
                                                        in_offset=bass.IndirectOffsetOnAxis(ap=ixap, axis=0),
                                                        bounds_check=self.breg(e, NROW - 1), oob_is_err=False)
                        S.dma_custom("pool", f, reads=[Twi], writes=[Tw[wb]])

                def issue_x(s_):
                    xb = s_ % 2
                    for t_ in range(4):
                        def gx(e, s_=s_, t_=t_, dst=xg_tm[xb][:, t_ * D:(t_ + 1) * D]):
                            return e.indirect_dma_start(out=dst, out_offset=None, in_=self.XG[:, :],
                                                        in_offset=bass.IndirectOffsetOnAxis(ap=xidx[:, t_ * NSLOT + s_:t_ * NSLOT + s_ + 1], axis=0),
                                                        bounds_check=self.breg(e, NEXP * CAP - 1), oob_is_err=False)
                        S.dma_custom("pool", gx, reads=[Txi, TXG], writes=[Txg[xb]])

                issue_w(0)
                issue_w(1)
                issue_x(0)
                TY = T()
                for ji, (s_, gi) in enumerate(jobs):
                    wb = ji % NWB
                    xb = s_ % 2
                    if ji + 2 < len(jobs):
                        issue_w(ji + 2)
                    if gi == 0:
                        if s_ + 1 < NSLOT:
                            issue_x(s_ + 1)
                        for tb in range(4):
                            pt, Tpt = self.psn()
                            ptb = pt[:, :].bitcast(BF16)
                            for k in range(8):
                                S.op("pe", lambda e, ptb=ptb, k=k, src=xg_tm[xb][:, tb * D + k * 128:tb * D + (k + 1) * 128]:
                                     e.transpose(ptb[:, k * 128:(k + 1) * 128], src, identb[:, :]), reads=[Txg[xb], Tf], writes=[Tpt])
                            self.cp("act" if tb % 2 == 0 else "dve",
                                    xgT[xb][:, :].rearrange("p (k n) -> p k n", k=8)[:, :, tb * 128:(tb + 1) * 128],
                                    ptb[:, 0:D].rearrange("p (k n) -> p k n", k=8), [Tpt], [TxgT[xb]])
                    ab = ji % 2
                    for j in range(G):
                        pa, Tpa = self.psn()
                        pb, Tpb = self.psn()
                        for k in range(8):
                            self.mm(pa[:, :TS], w13[wb][:, k * 512 + j * 128:k * 512 + (j + 1) * 128], xgT[xb][:, k * TS:(k + 1) * TS],
                                    k == 0, k == 7, [Tw[wb], TxgT[xb]], [Tpa])
                        for k in range(8):
                            self.mm(pb[:, :TS], w13[wb][:, 4096 + k * 512 + j * 128:4096 + k * 512 + (j + 1) * 128], xgT[xb][:, k * TS:(k + 1) * TS],
                                    k == 0, k == 7, [Tw[wb], TxgT[xb]], [Tpb])
                        sb_ = j % 2
                        self.act(sil[sb_][:, :], pa[:, :TS], AF.Silu, [Tpa], [Tsil[sb_]])
                        self.tt("dve", actb[ab][:, j * TS:(j + 1) * TS], sil[sb_][:, :], pb[:, :TS], ALU.mult, [Tpb, Tsil[sb_]], [Tab[ab][j]])
                    for tb in range(4):
                        for hf in range(2):
                            po, Tpo = self.psn()
                            for j in range(G):
                                self.mm(po[:, :512], actb[ab][:, j * TS + tb * 128:j * TS + (tb + 1) * 128],
                                        w2[wb][:, j * D + hf * 512:j * D + (hf + 1) * 512], j == 0, j == G - 1, [Tw[wb], Tab[ab][j]], [Tpo])
                            dst = acc[xb][:, tb * D + hf * 512:tb * D + (hf + 1) * 512]
                            if gi == 0:
                                self.cp("act", dst, po[:, :512], [Tpo], [Tacc[xb][tb]])
                            else:
                                self.tt("dve", dst, dst, po[:, :512], ALU.add, [Tpo], [Tacc[xb][tb]])
                    if gi == NG - 1:
                        S.dma("sp", self.YS[s_ * TS:(s_ + 1) * TS, :].rearrange("(t p) n -> p t n", p=128),
                              acc[xb][:, :].rearrange("p (t n) -> p t n", t=4), reads=Tacc[xb], writes=[TY])
            S.barrier()
            with contextlib.ExitStack() as st:
                ym = [self.sb(st, [128, 4 * 2 * D], F32, "ym") for _ in range(2)]
                Tym = [TL(4) for _ in range(2)]
                ysum = [self.sb(st, [128, 4 * D], F32, "ysum") for _ in range(2)]
                Tys = [TL(4) for _ in range(2)]
                xt = [self.sb(st, [128, 8 * 512], F32, "xt") for _ in range(2)]
                Txt = [TL(8) for _ in range(2)]
                sq = self.sb(st, [128, 8 * 512], BF16)
                Tsq = TL(8)
                rstd = self.sb(st, [128, 512], F32)
                Trs = T()
                for ti, (c0, n) in enumerate(TILES[1:]):
                    b = ti % 2
                    t0 = c0 - CTX
                    for k in range(8):
                        S.dma("sp", xt[b][:, k * n:(k + 1) * n], self.XS[k * 128:(k + 1) * 128, c0:c0 + n], writes=[Txt[b][k]])
                    for tb in range(4):
                        blk = ti * 4 + tb
                        for kk in range(2):
                            def gy(e, q=kk * NBLK + blk, dst=ym[b][:, (tb * 2 + kk) * D:(tb * 2 + kk + 1) * D]):
                                return e.indirect_dma_start(out=dst, out_offset=None, in_=self.YS[:, :],
                                                            in_offset=bass.IndirectOffsetOnAxis(ap=desti[:, q:q + 1], axis=0),
                                                            bounds_check=self.breg(e, NSLOT * TS - 1), oob_is_err=False)
                            S.dma_custom("pool", gy, reads=[Tdd, TY], writes=[Tym[b][tb]])
                        self.ts("dve", ysum[b][:, tb * D:(tb + 1) * D], ym[b][:, tb * 2 * D:tb * 2 * D + D], GK[0][:, blk:blk + 1], None,
                                ALU.mult, None, [Tym[b][tb], TGK], [Tys[b][tb]])
                        self.stt(ysum[b][:, tb * D:(tb + 1) * D], ym[b][:, tb * 2 * D + D:(tb + 1) * 2 * D], GK[1][:, blk:blk + 1],
                                 ysum[b][:, tb * D:(tb + 1) * D], ALU.mult, ALU.add, [Tym[b][tb], TGK], [Tys[b][tb]])
                    for k in range(8):
                        p, Tp = self.psn()
                        for tb in range(4):
                            S.op("pe", lambda e, p=p, tb=tb, src=ysum[b][:, tb * D + k * 128:tb * D + (k + 1) * 128]:
                                 e.transpose(p[:, tb * 128:(tb + 1) * 128], src, self.ident[:, :]), reads=[Tys[b][tb], self.Tc], writes=[Tp])
                        self.stt(xt[b][:, k * n:(k + 1) * n], p[:, :n], self.modt[l][:, a * 48 + 40 + k:a * 48 + 41 + k],
                                 xt[b][:, k * n:(k + 1) * n], ALU.mult, ALU.add, [Tp, self.Tmod], [Txt[b][k]])
                    for k in range(8):
                        self.act(sq[:, k * n:(k + 1) * n], xt[b][:, k * n:(k + 1) * n], AF.Square, [Txt[b][k]], [Tsq[k]])
                    p, Tp = self.psn()
                    for k in range(8):
                        self.mm(p[:, :n], self.ones_bf[:], sq[:, k * n:(k + 1) * n], k == 0, k == 7, [Tsq[k], self.Tc], [Tp])
                    self.rstd_from_ps(rstd, Trs, p, Tp, n, 1.0 / D)
                    for k in range(8):
                        self.stt(xt[b][:, k * n:(k + 1) * n], xt[b][:, k * n:(k + 1) * n], fnw[:, k:k + 1], rstd[:, :n],
                                 ALU.mult, ALU.mult, [Trs, Tf], [Txt[b][k]])
                        S.dma("sp", self.out[k * 128:(k + 1) * 128, t0:t0 + n], xt[b][:, k * n:(k + 1) * n], reads=[Txt[b][k]])

    def phase_ffn(self, l, last):
        S, I = self.S, self.I
        moe = (l % 2 == 1)
        G = 2 if moe else 4
        if moe:
            experts = list(range(NEXP))
            nf = EFF // 128
            W1 = lambda e: I["moe_w1"][e]
            W3 = lambda e: I["moe_w3"][e]
            W2 = lambda e: I["moe_w2"][e]
        else:
            experts = [0]
            nf = DFF // 128
            W1 = lambda e: I["ffn_w1"]
            W3 = lambda e: I["ffn_w3"]
            W2 = lambda e: I["ffn_w2"]
        groups = [(g0, min(G, nf - g0)) for g0 in range(0, nf, G)]
        if last:
            passes = [TILES[1:5], TILES[5:9]]
        else:
            passes = [TILES[0:5], TILES[5:9]]
        TB = 2304
        with contextlib.ExitStack() as st:
            acc = self.sb(st, [128, 8 * TB], F32, "acc")
            h2 = self.sb(st, [128, 8 * TB], BF16, "h2")
            w13 = [self.sb(st, [128, 2 * 8 * G * 128], BF16, "w13") for _ in range(2)]
            w2 = [self.sb(st, [128, G * D], BF16, "w2") for _ in range(2)]
            Tw = TL(2)
            actb = [self.sb(st, [128, G * 512], BF16, "actb") for _ in range(2)]
            Tab = [TL(G) for _ in range(2)]
            sil = [self.sb(st, [128, 512], F32) for _ in range(2)]
            Tsil = TL(2)
            sq = self.sb(st, [128, 8 * 512], BF16)
            Tsq = TL(8)
            rstd = self.sb(st, [128, 512], F32)
            Trs = T()
            tmpb = [self.sb(st, [128, 512], F32) for _ in range(2)]
            Ttmp = TL(2)
            fnw = self.sb(st, [128, 8], F32)
            Tf = T()
            S.dma("sp", fnw[:], I["fnw"][:, :], writes=[Tf])
            if moe:
                rw = self.sb(st, [128, 8 * NEXP], F32)
                sel = self.sb(st, [8, 1024], F32)
                S.dma("sp", rw[:, :].rearrange("p (k e) -> p k e", k=8), I["router_w"].rearrange("(k p) e -> p k e", p=128), writes=[Tf])
                S.dma("sp", sel[:], I["sel"][:, :], writes=[Tf])
                hF = self.sb(st, [128, 8 * 512], F32, "hF")
                ThF = TL(8)
                lgt = self.sb(st, [128, 8], F32)
                top = self.sb(st, [128, 8], F32)
                gat = self.sb(st, [128, 4], F32)
                cmb = self.sb(st, [128, 8], F32)
                cm2 = self.sb(st, [128, 8], F32)
                Trt = T()
                combT = self.sb(st, [8, TB], F32, "combT")
                TcT = TL(5)
                cbc = [self.sb(st, [128, 512], F32) for _ in range(2)]
                Tcb = TL(2)
            wcount = 0
            for tiles in passes:
                offs = []
                o = 0
                for (c0, n) in tiles:
                    offs.append(o)
                    o += n
                TBn = o
                Tacc = [TL(8) for _ in tiles]
                Th2 = [TL(8) for _ in tiles]
                for si, (c0, n) in enumerate(tiles):
                    a = 1 if c0 == 0 else 0
                    o = offs[si]
                    for k in range(8):
                        S.dma("sp", acc[:, k * TB + o:k * TB + o + n], self.XS[k * 128:(k + 1) * 128, c0:c0 + n], writes=[Tacc[si][k]])
                    for k in range(8):
                        self.act(sq[:, k * n:(k + 1) * n], acc[:, k * TB + o:k * TB + o + n], AF.Square, [Tacc[si][k]], [Tsq[k]])
                    p, Tp = self.psn()
                    for k in range(8):
                        self.mm(p[:, :n], self.ones_bf[:], sq[:, k * n:(k + 1) * n], k == 0, k == 7, [Tsq[k], self.Tc], [Tp])
                    self.rstd_from_ps(rstd, Trs, p, Tp, n, 1.0 / D)
                    for k in range(8):
                        tb = k % 2
                        self.stt(tmpb[tb][:, :n], acc[:, k * TB + o:k * TB + o + n], self.A2[l][:, a * 8 + k:a * 8 + k + 1], rstd[:, :n],
                                 ALU.mult, ALU.mult, [Tacc[si][k], Trs, self.Tmod], [Ttmp[tb]])
                        self.act(h2[:, k * TB + o:k * TB + o + n], tmpb[tb][:, :n], AF.Identity, [Ttmp[tb], self.Tmod], [Th2[si][k]],
                                 bias=self.modt[l][:, a * 48 + 24 + k:a * 48 + 25 + k])
                        if moe:
                            self.ts("pool", hF[:, k * n:(k + 1) * n], tmpb[tb][:, :n], self.modt[l][:, a * 48 + 24 + k:a * 48 + 25 + k], None,
                                    ALU.add, None, [Ttmp[tb], self.Tmod], [ThF[k]])
                    if moe:
                        for tb in range(n // 128):
                            p, Tp = self.psn()
                            for k in range(8):
                                self.mm(p[:, 0:8], hF[:, k * n + tb * 128:k * n + (tb + 1) * 128], rw[:, k * 8:(k + 1) * 8],
                                        k == 0, k == 7, [ThF[k], Tf], [Tp])
                            self.cp("dve", lgt[:, :], p[:, 0:8], [Tp], [Trt])
                            S.op("dve", lambda e: e.max(out=top[:, :], in_=lgt[:, :]), reads=[Trt], writes=[Trt])
                            self.tt("dve", gat[:, 0:1], top[:, 1:2], top[:, 0:1], ALU.subtract, [Trt], [Trt])
                            self.act(gat[:, 1:2], gat[:, 0:1], AF.Exp, [Trt], [Trt])
                            self.ts("dve", gat[:, 1:2], gat[:, 1:2], 1.0, None, ALU.add, None, [Trt], [Trt])
                            self.recip(gat[:, 2:3], gat[:, 1:2], [Trt], [Trt])
                            self.ts("dve", gat[:, 3:4], gat[:, 2:3], -1.0, 1.0, ALU.mult, ALU.add, [Trt], [Trt])
                            self.ts("dve", cmb[:, :], lgt[:, :], top[:, 0:1], gat[:, 2:3], ALU.is_equal, ALU.mult, [Trt], [Trt])
                            self.ts("dve", cm2[:, :], lgt[:, :], top[:, 1:2], gat[:, 3:4], ALU.is_equal, ALU.mult, [Trt], [Trt])
                            self.tt("dve", cmb[:, :], cmb[:, :], cm2[:, :], ALU.add, [Trt], [Trt])
                            p2, Tp2 = self.psn()
                            S.op("pe", lambda e, p2=p2: e.transpose(p2[0:8, 0:128], cmb[:, :], self.ident[:, :]), reads=[Trt, self.Tc], writes=[Tp2])
                            self.cp("act", combT[:, o + tb * 128:o + (tb + 1) * 128], p2[0:8, 0:128], [Tp2], [TcT[si]])
                for e in experts:
                    for (g0, gn) in groups:
                        wb = wcount % 2
                        wcount += 1
                        for (ww, off) in ((W1(e), 0), (W3(e), 8 * G * 128)):
                            for hh in range(2):
                                S.dma("pool", w13[wb][:, off + hh * 4 * G * 128:off + hh * 4 * G * 128 + 4 * gn * 128].rearrange("p (k n) -> p k n", k=4),
                                      ww[hh * 512:(hh + 1) * 512, g0 * 128:(g0 + gn) * 128].rearrange("(k p) n -> p k n", p=128), writes=[Tw[wb]])
                        S.dma("pool", w2[wb][:, :gn * D].rearrange("p (j n) -> p j n", j=gn),
                              W2(e)[g0 * 128:(g0 + gn) * 128, :].rearrange("(j p) n -> p j n", p=128), writes=[Tw[wb]])
                        for si, (c0, n) in enumerate(tiles):
                            a = 1 if c0 == 0 else 0
                            o = offs[si]
                            ab = si % 2
                            if moe:
                                cb = si % 2
                                pc, Tpc = self.psn()
                                self.mm(pc[:, :n], sel[:, e * 128:(e + 1) * 128], combT[:, o:o + n], True, True, [Tf, TcT[si]], [Tpc])
                                self.cp("act", cbc[cb][:, :n], pc[:, :n], [Tpc], [Tcb[cb]])
                            for j in range(gn):
                                pa, Tpa = self.psn()
                                pb, Tpb = self.psn()
                                wrow = (lambda k, off: w13[wb][:, off + (k // 4) * 4 * G * 128 + (k % 4) * gn * 128 + j * 128:
                                                               off + (k // 4) * 4 * G * 128 + (k % 4) * gn * 128 + (j + 1) * 128])
                                for k in range(8):
                                    self.mm(pa[:, :n], wrow(k, 0), h2[:, k * TB + o:k * TB + o + n], k == 0, k == 7, [Tw[wb], Th2[si][k]], [Tpa])
                                for k in range(8):
                                    self.mm(pb[:, :n], wrow(k, 8 * G * 128), h2[:, k * TB + o:k * TB + o + n], k == 0, k == 7, [Tw[wb], Th2[si][k]], [Tpb])
                                sb_ = j % 2
                                self.act(sil[sb_][:, :n], pa[:, :n], AF.Silu, [Tpa], [Tsil[sb_]])
                                if moe:
                                    self.tt("dve", sil[sb_][:, :n], sil[sb_][:, :n], pb[:, :n], ALU.mult, [Tpb, Tsil[sb_]], [Tsil[sb_]])
                                    self.tt("pool", actb[ab][:, j * 512:j * 512 + n], sil[sb_][:, :n], cbc[cb][:, :n], ALU.mult,
                                            [Tsil[sb_], Tcb[cb]], [Tab[ab][j]])
                                else:
                                    self.tt("dve", actb[ab][:, j * 512:j * 512 + n], sil[sb_][:, :n], pb[:, :n], ALU.mult,
                                            [Tpb, Tsil[sb_]], [Tab[ab][j]])
                            for i in range(8):
                                po, Tpo = self.psn()
                                for j in range(gn):
                                    self.mm(po[:, :n], w2[wb][:, j * D + i * 128:j * D + (i + 1) * 128], actb[ab][:, j * 512:j * 512 + n],
                                            j == 0, j == gn - 1, [Tw[wb], Tab[ab][j]], [Tpo])
                                self.stt(acc[:, i * TB + o:i * TB + o + n], po[:, :n], self.modt[l][:, a * 48 + 40 + i:a * 48 + 41 + i],
                                         acc[:, i * TB + o:i * TB + o + n], ALU.mult, ALU.add, [Tpo, self.Tmod], [Tacc[si][i]])
                for si, (c0, n) in enumerate(tiles):
                    o = offs[si]
                    if not last:
                        for k in range(8):
                            S.dma("pool", self.XS[k * 128:(k + 1) * 128, c0:c0 + n], acc[:, k * TB + o:k * TB + o + n], reads=[Tacc[si][k]])
                    else:
                        for k in range(8):
                            self.act(sq[:, k * n:(k + 1) * n], acc[:, k * TB + o:k * TB + o + n], AF.Square, [Tacc[si][k]], [Tsq[k]])
                        p, Tp = self.psn()
                        for k in range(8):
                            self.mm(p[:, :n], self.ones_bf[:], sq[:, k * n:(k + 1) * n], k == 0, k == 7, [Tsq[k], self.Tc], [Tp])
                        self.rstd_from_ps(rstd, Trs, p, Tp, n, 1.0 / D)
                        for k in range(8):
                            self.stt(acc[:, k * TB + o:k * TB + o + n], acc[:, k * TB + o:k * TB + o + n], fnw[:, k:k + 1], rstd[:, :n],
                                     ALU.mult, ALU.mult, [Trs, Tf], [Tacc[si][k]])
                            S.dma("pool", self.out[k * 128:(k + 1) * 128, c0 - CTX:c0 - CTX + n], acc[:, k * TB + o:k * TB + o + n],
                                  reads=[Tacc[si][k]])
                S.barrier()


_CONST = {}


def _consts():
    if _CONST:
        return _CONST
    bf = ml_dtypes.bfloat16
    t = np.arange(SEQ, dtype=np.int64)
    ang = 2.0 * np.pi * ((t[:, None] * t[None, :]) % SEQ).astype(np.float64) / SEQ
    _CONST["dftc"] = (np.cos(ang) / 64.0).astype(np.float32).astype(bf)
    _CONST["dfts"] = (np.sin(ang) / 64.0).astype(np.float32).astype(bf)
    t = np.arange(CTX, dtype=np.int64)
    ang = 2.0 * np.pi * ((t[:, None] * t[None, :]) % CTX).astype(np.float64) / CTX
    _CONST["dftc256"] = (np.cos(ang) / 16.0).astype(np.float32).astype(bf)
    _CONST["dfts256"] = (np.sin(ang) / 16.0).astype(np.float32).astype(bf)
    t = np.arange(128, dtype=np.int64)
    ang = 2.0 * np.pi * ((t[:, None] * t[None, :]) % 128).astype(np.float64) / 128
    s = 1.0 / np.sqrt(128.0)
    _CONST["cs128"] = np.concatenate([np.cos(ang) * s, -np.sin(ang) * s], axis=1).astype(np.float32).astype(bf)
    s_, t_ = np.meshgrid(np.arange(128), np.arange(128), indexing="ij")
    sc = -1.0 / 16.0
    tri = np.concatenate([(s_ <= t_), (s_ > t_), (s_ >= t_), (s_ < t_)], axis=1).astype(np.float32) * sc
    _CONST["tri"] = np.ascontiguousarray(tri)
    mf = (s_ <= t_).astype(np.float32)
    mb = (s_ >= t_).astype(np.float32)
    _CONST["mask"] = np.concatenate([mf] * 4 + [mb] * 4, axis=1).astype(bf)
    _CONST["ident"] = np.eye(128, dtype=np.float32)
    sel = np.zeros((8, 1024), np.float32)
    for e in range(8):
        sel[e, e * 128:(e + 1) * 128] = 1.0
    _CONST["sel"] = sel
    _CONST["su"] = (s_ < t_).astype(np.float32)
    _CONST["identb"] = np.eye(128, dtype=np.float32).astype(bf)
    _CONST["slotstart"] = np.ascontiguousarray(np.broadcast_to((np.arange(NSLOT, dtype=np.float32) * TS)[None, :], (128, NSLOT)))
    jsh = np.zeros((128, 256), np.float32)
    for p_ in range(1, 128):
        jsh[128 - p_, p_] = 1.0
    jsh[0, 128] = 1.0
    _CONST["jsh"] = jsh.astype(bf)
    _CONST["alt"] = (((-1.0) ** np.arange(SEQ)) / 64.0).astype(np.float32).astype(bf)[None, :]
    _CONST["ecap"] = np.ascontiguousarray(np.broadcast_to((np.arange(8, dtype=np.float32) * CAP)[None, :], (128, 8)))
    _CONST["gp7"] = np.ascontiguousarray((np.arange(7)[None, :] * 128 + np.arange(128)[:, None]).astype(np.float32))
    return _CONST


def _pcol(v, nchunk):
    return np.ascontiguousarray(np.asarray(v, np.float32).reshape(nchunk, 128).T)


def make_in_maps(inputs, cores):
    C = _consts()
    f = lambda a: np.ascontiguousarray(np.asarray(a, np.float32))
    shared = dict(C)
    for l in range(DEPTH):
        shared[f"w_mod{l}"] = f(inputs["w_mod"][l])
        shared[f"b_mod{l}"] = _pcol(inputs["b_mod"][l], 48)
        shared[f"nmw{l}"] = _pcol(inputs["norm_mix_w"][l], 8)
        shared[f"nfw{l}"] = _pcol(inputs["norm_ffn_w"][l], 8)
        shared[f"w_in{l}"] = f(inputs["w_in"][l])
        shared[f"wg{l}"] = np.ascontiguousarray(np.concatenate([f(inputs["w_gate_f"][l]), f(inputs["w_gate_b"][l])], axis=1))
        shared[f"bg{l}"] = np.ascontiguousarray(np.concatenate([f(inputs["b_gate_f"][l]), f(inputs["b_gate_b"][l])])[None, :])
        shared[f"gnw{l}"] = f(inputs["gla_norm_w"][l]).reshape(128, 1)
        shared[f"w_out{l}"] = f(inputs["w_out"][l])
    shared["ffn_w1"] = f(inputs["ffn_w1"][0])
    shared["ffn_w3"] = f(inputs["ffn_w3"][0])
    shared["ffn_w2"] = f(inputs["ffn_w2"][0])
    shared["router_w"] = f(inputs["router_w"][0])
    for nm, src in (("mw1", "moe_w1"), ("mw3", "moe_w3")):
        w = np.asarray(inputs[src][0], np.float32).reshape(NEXP, 8, 128, 7, 512)
        shared[nm] = np.ascontiguousarray(w.transpose(0, 3, 2, 1, 4)).reshape(NEXP * 7 * 128, 8 * 512)
    w = np.asarray(inputs["moe_w2"][0], np.float32).reshape(NEXP, 7, 4, 128, D)
    shared["mw2"] = np.ascontiguousarray(w.transpose(0, 1, 3, 2, 4)).reshape(NEXP * 7 * 128, 4 * D)
    shared["fnw"] = _pcol(inputs["final_norm_w"], 8)
    x = np.asarray(inputs["x"], np.float32)
    ctx = np.asarray(inputs["ctx"], np.float32)
    c = np.asarray(inputs["c"], np.float32)
    cc = np.asarray(inputs["c_ctx"], np.float32)
    maps = []
    for b in cores:
        m = dict(shared)
        m["xt"] = np.ascontiguousarray(np.concatenate([ctx[b], x[b]], axis=0).T)
        m["cvec"] = np.ascontiguousarray(np.concatenate([_pcol(c[b], 8), _pcol(cc, 8)], axis=1))
        maps.append(m)
    return maps


_NC = {}


def kernel(**inputs):
    if "nc" not in _NC:
        _NC["nc"] = Builder().build()
    nc = _NC["nc"]
    maps = make_in_maps(inputs, list(range(8)))
    res = run_bass_kernel_spmd(nc, maps, core_ids=list(range(8)))
    out = np.stack([np.ascontiguousarray(r["out"].T) for r in res.results], axis=0)
    return out.astype(np.float32)
```

```python
import contextlib
import os
import numpy as np
import ml_dtypes
import concourse.bass as bass
import concourse.mybir as mybir
from concourse.bass_utils import run_bass_kernel_spmd

F32 = mybir.dt.float32
BF16 = mybir.dt.bfloat16
AF = mybir.ActivationFunctionType
ALU = mybir.AluOpType

D = 1024
KD = 8
SEQ = 4096
CTX = 256
NTOK = SEQ + CTX
DEPTH = 2
IN_W = 2080
DFF = 2816
EFF = 3584
NEXP = 8
EPS = 1e-6
TILES = [(0, 256)] + [(256 + 512 * i, 512) for i in range(8)]
NCH = NTOK // 128
TS = 512
NSLOT = (2 * SEQ + NEXP * (TS - 1)) // TS
I32 = mybir.dt.int32
CAP = SEQ


class T:
    __slots__ = ("w", "r")

    def __init__(self):
        self.w = {}
        self.r = {}


def TL(n):
    return [T() for _ in range(n)]


class Sched:
    ENG = ("pe", "act", "dve", "pool", "sp")

    def __init__(self, nc, stack, ring=20):
        self.nc = nc
        self.streams = {e: [] for e in self.ENG}
        self.semobj = {}
        for e in ("pe", "act", "dve", "pool"):
            self.semobj[e] = stack.enter_context(nc.semaphore("s_" + e))
        self.cnt = {e: 0 for e in ("pe", "act", "dve", "pool")}
        self.seen = {e: {} for e in self.ENG}
        self.ring = {}
        self.ring_n = {}
        self.ring_pos = {}
        for q, n in (("sp", ring), ("pool", ring)):
            keys = []
            for i in range(n):
                k = f"d_{q}{i}"
                self.semobj[k] = stack.enter_context(nc.semaphore(k))
                keys.append(k)
            self.ring[q] = keys
            self.ring_n[q] = {k: 0 for k in keys}
            self.ring_pos[q] = 0

    def _deps(self, reads, writes):
        deps = {}
        for t in reads:
            for k, v in t.w.items():
                if deps.get(k, 0) < v:
                    deps[k] = v
        for t in writes:
            for k, v in t.w.items():
                if deps.get(k, 0) < v:
                    deps[k] = v
            for k, v in t.r.items():
                if deps.get(k, 0) < v:
                    deps[k] = v
        return deps

    def _waits(self, eng, deps, skip_own=None):
        waits = []
        seen = self.seen[eng]
        for k, v in deps.items():
            if k == skip_own:
                continue
            if seen.get(k, 0) < v:
                seen[k] = v
                waits.append((self.semobj[k], v))
        return waits

    def _mark(self, reads, writes, key, val):
        for t in reads:
            if t.r.get(key, 0) < val:
                t.r[key] = val
        for t in writes:
            t.w = {key: val}
            t.r = {}

    def op(self, eng, fn, reads=(), writes=()):
        deps = self._deps(reads, writes)
        waits = self._waits(eng, deps, skip_own=("pe" if eng == "pe" else None))
        self.cnt[eng] += 1
        val = self.cnt[eng]
        so = self.semobj[eng]

        def emit(e, waits=waits, fn=fn, so=so):
            for s, v in waits:
                e.wait_ge(s, v)
            fn(e).then_inc(so, 1)
        self.streams[eng].append(emit)
        self._mark(reads, writes, eng, val)

    def dma(self, q, out, in_, reads=(), writes=(), **kw):
        deps = self._deps(reads, writes)
        keys = self.ring[q]
        key = keys[self.ring_pos[q] % len(keys)]
        self.ring_pos[q] += 1
        prev = self.ring_n[q][key]
        if prev > 0 and deps.get(key, 0) < prev * 16:
            deps[key] = prev * 16
        waits = self._waits(q, deps)
        self.ring_n[q][key] = prev + 1
        val = (prev + 1) * 16
        so = self.semobj[key]

        def emit(e, waits=waits, so=so, out=out, in_=in_, kw=kw):
            for s, v in waits:
                e.wait_ge(s, v)
            e.dma_start(out=out, in_=in_, **kw).then_inc(so, 16)
        self.streams[q].append(emit)
        self._mark(reads, writes, key, val)

    def dma_custom(self, q, fn, reads=(), writes=()):
        deps = self._deps(reads, writes)
        keys = self.ring[q]
        key = keys[self.ring_pos[q] % len(keys)]
        self.ring_pos[q] += 1
        prev = self.ring_n[q][key]
        if prev > 0 and deps.get(key, 0) < prev * 16:
            deps[key] = prev * 16
        waits = self._waits(q, deps)
        self.ring_n[q][key] = prev + 1
        val = (prev + 1) * 16
        so = self.semobj[key]

        def emit(e, waits=waits, so=so, fn=fn):
            for s, v in waits:
                e.wait_ge(s, v)
            fn(e).then_inc(so, 16)
        self.streams[q].append(emit)
        self._mark(reads, writes, key, val)

    def barrier(self):
        allv = {}
        for e in ("pe", "act", "dve", "pool"):
            if self.cnt[e] > 0:
                allv[e] = self.cnt[e]
        for q in self.ring:
            for k, n in self.ring_n[q].items():
                if n > 0:
                    allv[k] = n * 16
        for eng in self.ENG:
            waits = self._waits(eng, dict(allv))

            def emit(e, waits=waits):
                for s, v in waits:
                    e.wait_ge(s, v)
            self.streams[eng].append(emit)

    def emit_all(self):
        with self.nc.Block() as block:
            @block.tensor
            def _(e):
                for f in self.streams["pe"]:
                    f(e)

            @block.scalar
            def _(e):
                for f in self.streams["act"]:
                    f(e)

            @block.vector
            def _(e):
                for f in self.streams["dve"]:
                    f(e)

            @block.gpsimd
            def _(e):
                for f in self.streams["pool"]:
                    f(e)

            @block.sync
            def _(e):
                for f in self.streams["sp"]:
                    f(e)


class Builder:
    def __init__(self, dbg=None, stop_after=None):
        self.dbg = dbg or set()
        self.stop_after = stop_after
        self.nc = bass.Bass("TRN2", target_bir_lowering=False)
        self.uid = 0
        self._bregs = {}

    def breg(self, e, bound):
        if bound not in self._bregs:
            self._bregs[bound] = e.to_reg(bound)
        return self._bregs[bound]

    def sb(self, st, shape, dt, name=None):
        self.uid += 1
        return st.enter_context(self.nc.sbuf_tensor(f"{name or 't'}_{self.uid}", list(shape), dt))

    def din(self, name, shape, dt=F32):
        return self.nc.dram_tensor(name, list(shape), dt, kind="ExternalInput").ap()

    def dscr(self, name, shape, dt):
        kind = "ExternalOutput" if name in self.dbg else "Internal"
        return self.nc.dram_tensor(name, list(shape), dt, kind=kind).ap()

    def psn(self):
        i = self.ps_i % 8
        self.ps_i += 1
        return self.ps[i], self.Tps[i]

    def mm(self, out, lhsT, rhs, start, stop, r, w):
        self.S.op("pe", lambda e: e.matmul(out, lhsT, rhs, start=start, stop=stop, skip_group_check=True), reads=r, writes=w)

    def act(self, out, in_, func, r, w, bias=None, scale=None):
        kw = {}
        if bias is not None:
            kw["bias"] = bias
        if scale is not None:
            kw["scale"] = scale
        self.S.op("act", lambda e: e.activation(out=out, in_=in_, func=func, **kw), reads=r, writes=w)

    def tt(self, eng, out, in0, in1, op, r, w):
        self.S.op(eng, lambda e: e.tensor_tensor(out=out, in0=in0, in1=in1, op=op), reads=r, writes=w)

    def ts(self, eng, out, in0, s1, s2, op0, op1, r, w):
        if s2 is None:
            self.S.op(eng, lambda e: e.tensor_scalar(out=out, in0=in0, scalar1=s1, scalar2=None, op0=op0), reads=r, writes=w)
        else:
            self.S.op(eng, lambda e: e.tensor_scalar(out=out, in0=in0, scalar1=s1, scalar2=s2, op0=op0, op1=op1), reads=r, writes=w)

    def stt(self, out, in0, scalar, in1, op0, op1, r, w):
        self.S.op("dve", lambda e: e.scalar_tensor_tensor(out=out, in0=in0, scalar=scalar, in1=in1, op0=op0, op1=op1), reads=r, writes=w)

    def cp(self, eng, out, in_, r, w):
        if eng == "act":
            self.S.op("act", lambda e: e.activation(out=out, in_=in_, func=AF.Copy), reads=r, writes=w)
        else:
            self.S.op(eng, lambda e: e.tensor_copy(out=out, in_=in_), reads=r, writes=w)

    def recip(self, out, in_, r, w):
        self.S.op("dve", lambda e: e.reciprocal(out=out, in_=in_), reads=r, writes=w)

    def memset(self, eng, ap, val, w):
        self.S.op(eng, lambda e: e.memset(ap, val), writes=w)

    def rstd_from_ps(self, rstd, Trs, ps, Tp, n, inv_n):
        self.act(rstd[:, :n], ps[:, :n], AF.Sqrt, [Tp, self.Tc], [Trs], bias=self.eps_c[:, 0:1], scale=inv_n)
        self.recip(rstd[:, :n], rstd[:, :n], [Trs], [Trs])

    def build(self):
        nc = self.nc
        I = {}
        I["xt"] = self.din("xt", [D, NTOK])
        I["cvec"] = self.din("cvec", [128, 16])
        for l in range(DEPTH):
            I[f"w_mod{l}"] = self.din(f"w_mod{l}", [D, 6 * D])
            I[f"b_mod{l}"] = self.din(f"b_mod{l}", [128, 48])
            I[f"nmw{l}"] = self.din(f"nmw{l}", [128, 8])
            I[f"nfw{l}"] = self.din(f"nfw{l}", [128, 8])
            I[f"w_in{l}"] = self.din(f"w_in{l}", [D, IN_W])
            I[f"wg{l}"] = self.din(f"wg{l}", [16, 512])
            I[f"bg{l}"] = self.din(f"bg{l}", [1, 512])
            I[f"gnw{l}"] = self.din(f"gnw{l}", [128, 1])
            I[f"w_out{l}"] = self.din(f"w_out{l}", [D, D])
        I["ffn_w1"] = self.din("ffn_w1", [D, DFF])
        I["ffn_w3"] = self.din("ffn_w3", [D, DFF])
        I["ffn_w2"] = self.din("ffn_w2", [DFF, D])
        I["router_w"] = self.din("router_w", [D, NEXP])
        I["mw1"] = self.din("mw1", [NEXP * 7 * 128, 8 * 512])
        I["mw3"] = self.din("mw3", [NEXP * 7 * 128, 8 * 512])
        I["mw2"] = self.din("mw2", [NEXP * 7 * 128, 4 * D])
        I["fnw"] = self.din("fnw", [128, 8])
        I["dftc"] = self.din("dftc", [SEQ, SEQ], BF16)
        I["dfts"] = self.din("dfts", [SEQ, SEQ], BF16)
        I["dftc256"] = self.din("dftc256", [CTX, CTX], BF16)
        I["dfts256"] = self.din("dfts256", [CTX, CTX], BF16)
        I["cs128"] = self.din("cs128", [128, 256], BF16)
        I["tri"] = self.din("tri", [128, 512])
        I["mask"] = self.din("mask", [128, 1024], BF16)
        I["ident"] = self.din("ident", [128, 128])
        I["sel"] = self.din("sel", [8, 1024])
        I["su"] = self.din("su", [128, 128])
        I["identb"] = self.din("identb", [128, 128], BF16)
        I["slotstart"] = self.din("slotstart", [128, NSLOT])
        I["gp7"] = self.din("gp7", [128, 7])
        I["ecap"] = self.din("ecap", [128, 8])
        I["jsh"] = self.din("jsh", [128, 256], BF16)
        I["alt"] = self.din("alt", [1, SEQ], BF16)
        self.I = I
        out = nc.dram_tensor("out", [D, SEQ], F32, kind="ExternalOutput").ap()
        self.out = out
        self.XS = self.dscr("XS", [D, NTOK], F32)
        self.U_tm = self.dscr("U_tm", [NTOK, 512], BF16)
        self.QKT = self.dscr("QKT", [512, NTOK], F32)
        self.KV_tm = self.dscr("KV_tm", [NTOK, 768], BF16)
        self.SGT = self.dscr("SGT", [512, NTOK], BF16)
        self.LG = self.dscr("LG", [NTOK, 512], F32)
        self.YT = self.dscr("YT", [D, NTOK], BF16)
        self.OB = self.dscr("OB", [512, NTOK], F32)
        self.XG = self.dscr("XG", [NEXP * CAP, D], BF16)
        self.YS = self.dscr("YS", [NSLOT * TS, D], F32)

        with contextlib.ExitStack() as st:
            self.S = S = Sched(nc, st)
            self.ps = [st.enter_context(nc.psum_tensor(f"ps{i}", [128, 512], F32)) for i in range(8)]
            self.Tps = TL(8)
            self.ps_i = 0
            self.Tc = Tc = T()
            self.ones_bf = self.sb(st, [128, 128], BF16, "ones")
            self.ones_f = self.sb(st, [1, 128], F32, "onesf")
            self.eps_c = self.sb(st, [128, 1], F32, "eps")
            self.ident = self.sb(st, [128, 128], F32, "ident")
            self.modt = [self.sb(st, [128, 96], F32, f"modt{l}") for l in range(DEPTH)]
            self.A1 = [self.sb(st, [128, 16], F32, f"A1{l}") for l in range(DEPTH)]
            self.A2 = [self.sb(st, [128, 16], F32, f"A2{l}") for l in range(DEPTH)]
            self.Tmod = T()
            self.memset("pool", self.ones_bf[:], 1.0, [Tc])
            self.memset("pool", self.ones_f[:], 1.0, [Tc])
            self.memset("pool", self.eps_c[:], EPS, [Tc])
            S.dma("sp", self.ident[:], I["ident"][:, :], writes=[Tc])
            self.mod_init(st)
            with contextlib.ExitStack() as stm:
                wm0 = [self.sb(stm, [128, 8 * 1024], BF16, "wm") for _ in range(2)]
                Twm0 = TL(2)
                for m in range(2):
                    self.mod_block(0, m, wm0[m], Twm0[m], self.Tmod)
            S.barrier()
            pending = [(0, m) for m in range(2, 6)] + [(1, m) for m in range(6)]
            self.Tmod2 = T()
            for l in range(DEPTH):
                last = l == DEPTH - 1
                xsrc = I["xt"] if l == 0 else self.XS
                self.phase_proj(l, xsrc, pending if l == 0 else [])
                S.barrier()
                if self.stop_after == f"proj{l}":
                    break
                self.phase_gla(l)
                S.barrier()
                if self.stop_after == f"gla{l}":
                    break
                self.phase_fourier(l, do_ctx=not last)
                S.barrier()
                if self.stop_after == f"four{l}":
                    break
                self.phase_outproj(l, xsrc, do_ctx=not last)
                S.barrier()
                if self.stop_after == f"outp{l}":
                    break
                if l % 2 == 1:
                    self.phase_moe(l)
                else:
                    self.phase_ffn(l, last)
                S.barrier()
                if self.stop_after == f"ffn{l}":
                    break
            S.barrier()
            S.emit_all()
        return nc

    def mod_init(self, st):
        S, I = self.S, self.I
        cv = self.sb(st, [128, 16], F32)
        self.m_cs = self.sb(st, [128, 16], BF16)
        self.m_bm = [self.sb(st, [128, 48], F32) for _ in range(DEPTH)]
        self.m_nw = {}
        self.m_tmp = self.sb(st, [128, 16], F32)
        self.Tmc, self.Tmtmp = T(), T()
        Tcv = T()
        S.dma("sp", cv[:], I["cvec"][:, :], writes=[Tcv])
        self.act(self.m_cs[:], cv[:], AF.Silu, [Tcv], [self.Tmc])
        for l in range(DEPTH):
            S.dma("sp", self.m_bm[l][:], I[f"b_mod{l}"][:, :], writes=[self.Tmc])
            for nm in (f"nmw{l}", f"nfw{l}"):
                self.m_nw[nm] = self.sb(st, [128, 8], F32)
                S.dma("sp", self.m_nw[nm][:], I[nm][:, :], writes=[self.Tmc])

    def mod_block(self, l, m, wm, Twm, Tm):
        self.mod_load(l, m, wm, Twm)
        self.mod_compute(l, m, wm, Twm, Tm)

    def mod_load(self, l, m, wm, Twm):
        S, I = self.S, self.I
        for hh in range(2):
            S.dma("pool", wm[:, hh * 4096:(hh + 1) * 4096].rearrange("p (k n) -> p k n", k=4),
                  I[f"w_mod{l}"][hh * 512:(hh + 1) * 512, m * 1024:(m + 1) * 1024].rearrange("(k p) n -> p k n", p=128),
                  writes=[Twm])

    def mod_compute(self, l, m, wm, Twm, Tm):
        S, I = self.S, self.I
        p, Tp = self.psn()
        for j in range(8):
            for k in range(8):
                self.mm(p[:, j:j + 9:8], wm[:, k * 1024 + j * 128:k * 1024 + (j + 1) * 128],
                        self.m_cs[:, k:k + 9:8], j == 0 and k == 0, k == 7, [Twm, self.Tmc], [Tp])
        for a in range(2):
            self.tt("dve", self.modt[l][:, a * 48 + m * 8:a * 48 + (m + 1) * 8], p[:, a * 8:(a + 1) * 8],
                    self.m_bm[l][:, m * 8:(m + 1) * 8], ALU.add, [Tp, self.Tmc], [Tm])
        if m in (1, 4):
            nm, dst, off = (f"nmw{l}", self.A1[l], 8) if m == 1 else (f"nfw{l}", self.A2[l], 32)
            tmp = self.m_tmp
            for a in range(2):
                self.ts("dve", tmp[:, a * 8:(a + 1) * 8], self.modt[l][:, a * 48 + off:a * 48 + off + 8], 1.0, None,
                        ALU.add, None, [Tm], [self.Tmtmp])
                self.tt("dve", dst[:, a * 8:(a + 1) * 8], tmp[:, a * 8:(a + 1) * 8], self.m_nw[nm][:], ALU.mult,
                        [self.Tmtmp, self.Tmc], [Tm])

    def norm_mod(self, xt, Txt, n, A, Bv, a, hT, ThT, sq, Tsq, rstd, Trs, tmpb, Ttmp, hF=None, ThF=None, xstride=None):
        xs = xstride or n
        for k in range(8):
            self.act(sq[:, k * n:(k + 1) * n], xt[:, k * xs:k * xs + n], AF.Square, [Txt], [Tsq[k]])
        p, Tp = self.psn()
        for k in range(8):
            self.mm(p[:, :n], self.ones_bf[:], sq[:, k * n:(k + 1) * n], k == 0, k == 7, [Tsq[k], self.Tc], [Tp])
        self.rstd_from_ps(rstd, Trs, p, Tp, n, 1.0 / D)
        for k in range(8):
            tb = k % len(tmpb)
            self.stt(tmpb[tb][:, :n], xt[:, k * xs:k * xs + n], A[:, a * 8 + k:a * 8 + k + 1], rstd[:, :n],
                     ALU.mult, ALU.mult, [Txt, Trs, self.Tmod], [Ttmp[tb]])
            self.act(hT[:, k * n:(k + 1) * n], tmpb[tb][:, :n], AF.Identity, [Ttmp[tb], self.Tmod], [ThT[k]],
                     bias=Bv[:, a * 48 + k:a * 48 + k + 1])
            if hF is not None:
                self.ts("pool", hF[:, k * n:(k + 1) * n], tmpb[tb][:, :n], Bv[:, a * 48 + k:a * 48 + k + 1], None,
                        ALU.add, None, [Ttmp[tb], self.Tmod], [ThF[k]])

    def phase_proj(self, l, xsrc, pending=()):
        S, I = self.S, self.I
        pending = list(pending)
        with contextlib.ExitStack() as st:
            if pending:
                wmp = [self.sb(st, [128, 8 * 1024], BF16, "wmp") for _ in range(2)]
                Twmp = TL(2)
                njob = 0
            win = self.sb(st, [128, 8 * IN_W], BF16, "win")
            Twin2 = [TL(2) for _ in range(8)]
            for hf, (c0, c1) in enumerate(((0, 1040), (1040, 2080))):
                for k in range(8):
                    S.dma("pool", win[:, k * IN_W + c0:k * IN_W + c1], I[f"w_in{l}"][k * 128:(k + 1) * 128, c0:c1], writes=[Twin2[k][hf]])

            def Tw(k, col, w):
                if col + w <= 1040:
                    return [Twin2[k][0]]
                if col >= 1040:
                    return [Twin2[k][1]]
                return Twin2[k]
            if pending:
                self.mod_load(*pending[0], wmp[0], Twmp[0])
                njob = 1
            wg = self.sb(st, [16, 512], F32)
            bg = self.sb(st, [1, 512], F32)
            Twg = T()
            S.dma("sp", wg[:], I[f"wg{l}"][:, :], writes=[Twg])
            S.dma("sp", bg[:], I[f"bg{l}"][:, :], writes=[Twg])
            xt = [self.sb(st, [128, 8 * 512], F32, "xt") for _ in range(2)]
            Txt = TL(2)
            sq = self.sb(st, [128, 8 * 512], BF16, "sq")
            Tsq = TL(8)
            rstd = self.sb(st, [128, 512], F32)
            Trs = T()
            tmpb = [self.sb(st, [128, 512], F32) for _ in range(3)]
            Ttmp = TL(3)
            hT = [self.sb(st, [128, 8 * 512], BF16, "hT") for _ in range(2)]
            ThT = [TL(8) for _ in range(2)]
            u_st = [self.sb(st, [128, 4 * 512], BF16) for _ in range(2)]
            Tu = TL(2)
            qk_st = [self.sb(st, [128, 4 * 512], F32) for _ in range(2)]
            Tqk = TL(2)
            kv_st = [self.sb(st, [128, 4 * 768], BF16) for _ in range(2)]
            Tkv = TL(2)
            sg_st = [self.sb(st, [128, 4 * 512], BF16) for _ in range(2)]
            Tsg = TL(2)
            lfb = [self.sb(st, [16, 2 * 512], F32) for _ in range(2)]
            Tlfb = TL(2)
            lg_st = [self.sb(st, [128, 4 * 512], F32) for _ in range(2)]
            Tlg = TL(2)
            etmp = [self.sb(st, [128, 512], F32) for _ in range(2)]
            Tet = TL(2)
            for ti, (c0, n) in enumerate(TILES):
                a = 1 if ti == 0 else 0
                b = ti % 2
                nb = n // 128
                S.dma("sp", xt[b][:, :8 * n].rearrange("p (k n) -> p k n", k=8),
                      xsrc[:, c0:c0 + n].rearrange("(k p) n -> p k n", p=128), writes=[Txt[b]])
                self.norm_mod(xt[b], Txt[b], n, self.A1[l], self.modt[l], a, hT[b], ThT[b], sq, Tsq, rstd, Trs, tmpb, Ttmp)
                h = hT[b]
                Th = ThT[b]
                for tb in range(nb):
                    p, Tp = self.psn()
                    for k in range(8):
                        self.mm(p[:, :512], h[:, k * n + tb * 128:k * n + (tb + 1) * 128], win[:, k * IN_W:k * IN_W + 512],
                                k == 0, k == 7, [Th[k]] + Tw(k, 0, 512), [Tp])
                    self.cp("dve" if tb % 2 == 0 else "act", u_st[b][:, tb * 512:(tb + 1) * 512], p[:, :512], [Tp], [Tu[b]])
                S.dma("pool", self.U_tm[c0:c0 + n, :].rearrange("(t p) n -> p t n", p=128),
                      u_st[b][:, :nb * 512].rearrange("p (t n) -> p t n", t=nb), reads=[Tu[b]])
                for j in range(4):
                    p, Tp = self.psn()
                    col = 512 + j * 128
                    for k in range(8):
                        self.mm(p[:, :n], win[:, k * IN_W + col:k * IN_W + col + 128], h[:, k * n:(k + 1) * n],
                                k == 0, k == 7, [Th[k]] + Tw(k, col, 128), [Tp])
                    self.cp("act" if j % 2 == 0 else "dve", qk_st[b][:, j * n:(j + 1) * n], p[:, :n], [Tp], [Tqk[b]])
                S.dma("pool", self.QKT[:, c0:c0 + n].rearrange("(j p) n -> p j n", p=128),
                      qk_st[b][:, :4 * n].rearrange("p (j n) -> p j n", j=4), reads=[Tqk[b]])
                for tb in range(nb):
                    p, Tp = self.psn()
                    for k in range(8):
                        self.mm(p[:, :256], h[:, k * n + tb * 128:k * n + (tb + 1) * 128], win[:, k * IN_W + 768:k * IN_W + 1024],
                                k == 0, k == 7, [Th[k]] + Tw(k, 768, 256), [Tp])
                    self.cp("dve", kv_st[b][:, tb * 768:tb * 768 + 256], p[:, :256], [Tp], [Tkv[b]])
                    p, Tp = self.psn()
                    for k in range(8):
                        self.mm(p[:, :512], h[:, k * n + tb * 128:k * n + (tb + 1) * 128], win[:, k * IN_W + 1024:k * IN_W + 1536],
                                k == 0, k == 7, [Th[k]] + Tw(k, 1024, 512), [Tp])
                    self.cp("act", kv_st[b][:, tb * 768 + 256:(tb + 1) * 768], p[:, :512], [Tp], [Tkv[b]])
                S.dma("pool", self.KV_tm[c0:c0 + n, :].rearrange("(t p) n -> p t n", p=128),
                      kv_st[b][:, :nb * 768].rearrange("p (t n) -> p t n", t=nb), reads=[Tkv[b]])
                for j in range(4):
                    p, Tp = self.psn()
                    col = 1536 + j * 128
                    for k in range(8):
                        self.mm(p[:, :n], win[:, k * IN_W + col:k * IN_W + col + 128], h[:, k * n:(k + 1) * n],
                                k == 0, k == 7, [Th[k]] + Tw(k, col, 128), [Tp])
                    self.act(sg_st[b][:, j * n:(j + 1) * n], p[:, :n], AF.Silu, [Tp], [Tsg[b]])
                S.dma("pool", self.SGT[:, c0:c0 + n].rearrange("(j p) n -> p j n", p=128),
                      sg_st[b][:, :4 * n].rearrange("p (j n) -> p j n", j=4), reads=[Tsg[b]])
                for dd in range(2):
                    p, Tp = self.psn()
                    col = 2048 + dd * 16
                    for k in range(8):
                        self.mm(p[0:16, :n], win[:, k * IN_W + col:k * IN_W + col + 16], h[:, k * n:(k + 1) * n],
                                k == 0, k == 7, [Th[k]] + Tw(k, col, 16), [Tp])
                    self.cp("dve", lfb[b][:, dd * 512:dd * 512 + n], p[0:16, :n], [Tp], [Tlfb[b]])
                for tb in range(nb):
                    for dd in range(2):
                        p, Tp = self.psn()
                        self.mm(p[:, :256], lfb[b][:, dd * 512 + tb * 128:dd * 512 + (tb + 1) * 128], wg[:, dd * 256:(dd + 1) * 256],
                                True, False, [Tlfb[b], Twg], [Tp])
                        self.mm(p[:, :256], self.ones_f[:, :], bg[:, dd * 256:(dd + 1) * 256], False, True, [self.Tc, Twg], [Tp])
                        e = (tb * 2 + dd) % 2
                        self.act(etmp[e][:, :256], p[:, :256], AF.Exp, [Tp], [Tet[e]], scale=-1.0)
                        self.act(lg_st[b][:, tb * 512 + dd * 256:tb * 512 + (dd + 1) * 256], etmp[e][:, :256], AF.Ln,
                                 [Tet[e]], [Tlg[b]], bias=1.0)
                S.dma("pool", self.LG[c0:c0 + n, :].rearrange("(t p) n -> p t n", p=128),
                      lg_st[b][:, :nb * 512].rearrange("p (t n) -> p t n", t=nb), reads=[Tlg[b]])
                if pending:
                    if njob > 0:
                        self.mod_compute(*pending[njob - 1], wmp[(njob - 1) % 2], Twmp[(njob - 1) % 2], self.Tmod2)
                    if njob < len(pending):
                        self.mod_load(*pending[njob], wmp[njob % 2], Twmp[njob % 2])
                        njob += 1
            while pending and njob <= len(pending):
                self.mod_compute(*pending[njob - 1], wmp[(njob - 1) % 2], Twmp[(njob - 1) % 2], self.Tmod2)
                if njob < len(pending):
                    self.mod_load(*pending[njob], wmp[njob % 2], Twmp[njob % 2])
                njob += 1

    def phase_gla(self, l):
        S, I = self.S, self.I
        with contextlib.ExitStack() as st:
            tri = self.sb(st, [128, 512], F32, "tri")
            mask = self.sb(st, [128, 1024], BF16, "mask")
            gnw = self.sb(st, [128, 1], F32)
            Tk = T()
            S.dma("sp", tri[:], I["tri"][:, :], writes=[Tk])
            S.dma("sp", mask[:], I["mask"][:, :], writes=[Tk])
            S.dma("sp", gnw[:], I[f"gnw{l}"][:, :], writes=[Tk])
            obuf = self.sb(st, [128, 4 * NTOK], F32, "obuf")
            Tob = TL(NCH)
            S32 = [self.sb(st, [128, 512], F32) for _ in range(2)]
            Sbf = [self.sb(st, [128, 512], BF16) for _ in range(2)]
            TS32 = TL(2)
            TSbf = TL(2)
            for d in range(2):
                self.memset("pool", S32[d][:], 0.0, [TS32[d]])
                self.memset("pool", Sbf[d][:], 0.0, [TSbf[d]])
            NB = 2
            mk2 = lambda W, dt: [self.sb(st, [128, 2 * W], dt) for _ in range(NB)]
            V = lambda t, d, W: t[:, d * W:(d + 1) * W]
            V3 = lambda t: t[:, :].rearrange("p (d w) -> p d w", d=2)
            qk_in, kv_in, lg_in = mk2(512, F32), mk2(768, BF16), mk2(256, F32)
            Tqi, Tki, Tli = [TL(NB) for _ in range(2)], [TL(NB) for _ in range(2)], [TL(NB) for _ in range(2)]
            Eq, Ek, Er = mk2(256, F32), mk2(256, F32), mk2(256, F32)
            TE = [TL(NB) for _ in range(2)]
            qtl, ktl, kht, scm = mk2(512, BF16), mk2(256, BF16), mk2(256, BF16), mk2(512, BF16)
            for bb in range(NB):
                self.memset("pool", qtl[bb][:, :], 0.0, [])
            Tq = [TL(NB) for _ in range(2)]
            Tkh = [TL(NB) for _ in range(2)]
            Tsc = [TL(NB) for _ in range(2)]
            order = [list(range(NCH)), [1, 0] + list(range(NCH - 1, 1, -1))]
            step_of = [{c: i for i, c in enumerate(order[d])} for d in range(2)]
            for i in range(NCH):
                b = i % NB
                cs = [order[0][i], order[1][i]]
                psA, psB, psC, psD = [None] * 2, [None] * 2, [None] * 2, [None] * 2
                for d in range(2):
                    c0 = cs[d] * 128
                    S.dma("sp", V(qk_in[b], d, 512).rearrange("p (j n) -> p j n", j=4),
                          self.QKT[:, c0:c0 + 128].rearrange("(j p) n -> p j n", p=128), writes=[Tqi[d][b]])
                    S.dma("sp", V(kv_in[b], d, 768), self.KV_tm[c0:c0 + 128, :], writes=[Tki[d][b]])
                    S.dma("sp", V(lg_in[b], d, 256), self.LG[c0:c0 + 128, d * 256:(d + 1) * 256], writes=[Tli[d][b]])
                for d in range(2):
                    psA[d] = self.psn()
                    p, Tp = psA[d]
                    lg = V(lg_in[b], d, 256)
                    self.mm(p[:, 0:128], lg[:, 0:128], tri[:, (2 * d) * 128:(2 * d + 1) * 128], True, True, [Tli[d][b], Tk], [Tp])
                    self.mm(p[:, 128:256], lg[:, 128:256], tri[:, (2 * d) * 128:(2 * d + 1) * 128], True, True, [Tli[d][b], Tk], [Tp])
                    self.mm(p[:, 256:512], tri[:, (2 * d + 1) * 128:(2 * d + 2) * 128], lg[:, 0:256], True, True, [Tli[d][b], Tk], [Tp])
                for d in range(2):
                    p, Tp = psA[d]
                    self.act(V(Eq[b], d, 256), p[:, 0:256], AF.Exp, [Tp], [TE[d][b]])
                    self.act(V(Ek[b], d, 256), p[:, 0:256], AF.Exp, [Tp], [TE[d][b]], scale=-1.0)
                    self.act(V(Er[b], d, 256), p[:, 256:512], AF.Exp, [Tp], [TE[d][b]])
                rd = [Tqi[0][b], Tqi[1][b], Tki[0][b], Tki[1][b], TE[0][b], TE[1][b]]
                self.stt(V3(qtl[b])[0:64, :, 0:256], V3(qk_in[b])[0:64, :, 0:256], 0.125, V3(Eq[b])[0:64, :, :], ALU.mult, ALU.mult,
                         rd, [Tq[0][b], Tq[1][b]])
                self.stt(V3(qtl[b])[64:128, :, 256:512], V3(qk_in[b])[64:128, :, 0:256], 0.125, V3(Eq[b])[64:128, :, :], ALU.mult, ALU.mult,
                         rd, [Tq[0][b], Tq[1][b]])
                self.tt("dve", V3(ktl[b])[:, :, :], V3(qk_in[b])[:, :, 256:512], V3(Ek[b])[:, :, :], ALU.mult, rd, [Tq[0][b], Tq[1][b]])
                self.tt("dve", V3(kht[b])[:, :, :], V3(kv_in[b])[:, :, 0:256], V3(Er[b])[:, :, :], ALU.mult, rd, [Tkh[0][b], Tkh[1][b]])
                for d in range(2):
                    psB[d] = self.psn()
                    p, Tp = psB[d]
                    for h in range(4):
                        half = h // 2
                        qo = (h % 2) * 256 + half * 128
                        self.mm(p[:, h * 128:(h + 1) * 128], V(ktl[b], d, 256)[:, half * 128:(half + 1) * 128],
                                V(qtl[b], d, 512)[:, qo:qo + 128], True, True, [Tq[d][b]], [Tp])
                    self.tt("dve", V(scm[b], d, 512), p[:, :], mask[:, d * 512:(d + 1) * 512], ALU.mult, [Tp, Tk], [Tsc[d][b]])
                for d in range(2):
                    psC[d] = self.psn()
                    p, Tp = psC[d]
                    c = cs[d]
                    for h in range(4):
                        half = h // 2
                        self.mm(p[:, h * 128:(h + 1) * 128], V(kv_in[b], d, 768)[:, 256 + h * 128:256 + (h + 1) * 128],
                                V(scm[b], d, 512)[:, h * 128:(h + 1) * 128], True, False, [Tki[d][b], Tsc[d][b]], [Tp])
                        qo = (h % 2) * 256 + half * 128
                        self.mm(p[:, h * 128:(h + 1) * 128], Sbf[d][:, h * 128:(h + 1) * 128],
                                V(qtl[b], d, 512)[:, qo:qo + 128], False, True, [TSbf[d], Tq[d][b]], [Tp])
                    first = step_of[d][c] < step_of[1 - d][c]
                    oview = obuf[:, :].rearrange("p (h n) -> p h n", h=4)[:, :, c * 128:(c + 1) * 128]
                    pview = p[:, :].rearrange("p (h n) -> p h n", h=4)
                    if first:
                        self.cp("act", oview, pview, [Tp], [Tob[c]])
                    else:
                        self.tt("dve", oview, oview, pview, ALU.add, [Tp], [Tob[c]])
                for d in range(2):
                    psD[d] = self.psn()
                    p, Tp = psD[d]
                    tl = 127 if d == 0 else 0
                    for half in range(2):
                        self.mm(p[:, half * 256:(half + 1) * 256], V(kht[b], d, 256)[:, half * 128:(half + 1) * 128],
                                V(kv_in[b], d, 768)[:, 256 + half * 256:256 + (half + 1) * 256], True, True, [Tkh[d][b], Tki[d][b]], [Tp])
                    for half in range(2):
                        self.stt(S32[d][:, half * 256:(half + 1) * 256], S32[d][:, half * 256:(half + 1) * 256],
                                 V(Eq[b], d, 256)[:, half * 128 + tl:half * 128 + tl + 1],
                                 p[:, half * 256:(half + 1) * 256], ALU.mult, ALU.add, [Tp, TE[d][b]], [TS32[d]])
                    self.cp("act", Sbf[d][:, :], S32[d][:, :], [TS32[d]], [TSbf[d]])
            sgt = [self.sb(st, [128, 4 * 512], BF16) for _ in range(2)]
            Tsg = TL(2)
            sq = self.sb(st, [128, 4 * 512], BF16)
            Tsq = TL(4)
            rs = [self.sb(st, [128, 512], F32) for _ in range(2)]
            Trs = TL(2)
            t1 = [self.sb(st, [128, 512], F32) for _ in range(2)]
            Tt1 = TL(2)
            yg = [self.sb(st, [128, 4 * 512], BF16) for _ in range(2)]
            Tyg = TL(2)
            if "OB" in self.dbg:
                S.dma("pool", self.OB[:, :].rearrange("(h p) n -> p h n", p=128),
                      obuf[:, :].rearrange("p (h n) -> p h n", h=4), reads=Tob)
            for ti, (c0, n) in enumerate(TILES):
                b = ti % 2
                Tobs = [Tob[c] for c in range(c0 // 128, (c0 + n) // 128)]
                S.dma("sp", sgt[b][:, :4 * n].rearrange("p (j n) -> p j n", j=4),
                      self.SGT[:, c0:c0 + n].rearrange("(j p) n -> p j n", p=128), writes=[Tsg[b]])
                for h in range(4):
                    self.act(sq[:, h * n:(h + 1) * n], obuf[:, h * NTOK + c0:h * NTOK + c0 + n], AF.Square, Tobs, [Tsq[h]])
                for h in range(4):
                    p, Tp = self.psn()
                    self.mm(p[:, :n], self.ones_bf[:], sq[:, h * n:(h + 1) * n], True, True, [Tsq[h], self.Tc], [Tp])
                    e = h % 2
                    self.rstd_from_ps(rs[e], Trs[e], p, Tp, n, 1.0 / 128)
                    self.stt(t1[e][:, :n], obuf[:, h * NTOK + c0:h * NTOK + c0 + n], gnw[:, 0:1], rs[e][:, :n], ALU.mult, ALU.mult,
                             Tobs + [Trs[e], Tk], [Tt1[e]])
                    self.tt("pool", yg[b][:, h * n:(h + 1) * n], t1[e][:, :n], sgt[b][:, h * n:(h + 1) * n], ALU.mult,
                            [Tt1[e], Tsg[b]], [Tyg[b]])
                S.dma("pool", self.YT[512:1024, c0:c0 + n].rearrange("(j p) n -> p j n", p=128),
                      yg[b][:, :4 * n].rearrange("p (j n) -> p j n", j=4), reads=[Tyg[b]])

    def phase_fourier(self, l, do_ctx):
        S, I = self.S, self.I
        with contextlib.ExitStack() as st:
            U = self.sb(st, [128, NCH * 512], BF16, "U")
            TU = T()
            for g0 in range(0, NCH, 17):
                S.dma("sp", U[:, g0 * 512:(g0 + 17) * 512].rearrange("p (t n) -> p t n", t=17),
                      self.U_tm[g0 * 128:(g0 + 17) * 128, :].rearrange("(t p) n -> p t n", p=128), writes=[TU])
            cs128 = self.sb(st, [128, 256], BF16)
            jsh = self.sb(st, [128, 256], BF16)
            alt = self.sb(st, [1, SEQ], BF16)
            Tcs = T()
            S.dma("sp", cs128[:], I["cs128"][:, :], writes=[Tcs])
            S.dma("sp", jsh[:], I["jsh"][:, :], writes=[Tcs])
            S.dma("sp", alt[:], I["alt"][:, :], writes=[Tcs])
            NW = 256
            HC = 16
            cl = [self.sb(st, [128, HC * NW], BF16, "cl") for _ in range(2)]
            sl = [self.sb(st, [128, HC * NW], BF16, "sl") for _ in range(2)]
            Tcl = TL(2)
            z = self.sb(st, [128, 8 * NW], BF16)
            Tz = TL(4)
            yf = [self.sb(st, [128, 4 * NW], BF16) for _ in range(2)]
            Tyf = TL(2)
            UE = self.sb(st, [128, HC * 512], BF16, "UE")
            UO = self.sb(st, [128, HC * 512], BF16, "UO")
            TUE = TL(HC)
            for tc in range(HC):
                p, Tp = self.psn()
                self.mm(p[:, :512], jsh[:, 0:128], U[:, (2 + 31 - tc) * 512:(2 + 32 - tc) * 512], True, tc == 0, [TU, Tcs], [Tp])
                if tc > 0:
                    self.mm(p[:, :512], jsh[:, 128:256], U[:, (2 + 32 - tc) * 512:(2 + 33 - tc) * 512], False, True, [TU, Tcs], [Tp])
                self.tt("dve", UE[:, tc * 512:(tc + 1) * 512], U[:, (2 + tc) * 512:(3 + tc) * 512], p[:, :512], ALU.add, [TU, Tp], [TUE[tc]])
                self.tt("dve", UO[:, tc * 512:(tc + 1) * 512], U[:, (2 + tc) * 512:(3 + tc) * 512], p[:, :512], ALU.subtract, [TU, Tp], [TUE[tc]])
            jobs = []
            if do_ctx:
                jobs.append(("c", 0))
            for nt in range(SEQ // NW):
                jobs.append(("x", nt))
            for ji, (kind, nt) in enumerate(jobs):
                b = ji % 2
                banks = [self.psn() for _ in range(4)]
                if kind == "c":
                    col0 = 0
                    S.dma("sp", cl[b][:, :2 * NW].rearrange("p (t n) -> p t n", t=2),
                          I["dftc256"][:, :].rearrange("(t p) n -> p t n", p=128), writes=[Tcl[b]])
                    S.dma("sp", sl[b][:, :2 * NW].rearrange("p (t n) -> p t n", t=2),
                          I["dfts256"][:, :].rearrange("(t p) n -> p t n", p=128), writes=[Tcl[b]])
                    for tc in range(2):
                        for g in range(4):
                            p, Tp = banks[g]
                            lhs = U[:, tc * 512 + g * 128:tc * 512 + (g + 1) * 128]
                            self.mm(p[:, 0:NW], lhs, cl[b][:, tc * NW:(tc + 1) * NW], tc == 0, tc == 1, [TU, Tcl[b]], [Tp])
                            self.mm(p[:, NW:2 * NW], lhs, sl[b][:, tc * NW:(tc + 1) * NW], False, tc == 1, [TU, Tcl[b]], [Tp])
                else:
                    col0 = CTX + nt * NW
                    S.dma("sp", cl[b][:, :].rearrange("p (t n) -> p t n", t=HC),
                          I["dftc"][0:HC * 128, nt * NW:(nt + 1) * NW].rearrange("(t p) n -> p t n", p=128), writes=[Tcl[b]])
                    S.dma("sp", sl[b][:, :].rearrange("p (t n) -> p t n", t=HC),
                          I["dfts"][0:HC * 128, nt * NW:(nt + 1) * NW].rearrange("(t p) n -> p t n", p=128), writes=[Tcl[b]])
                    for tc in range(HC):
                        for g in range(4):
                            p, Tp = banks[g]
                            self.mm(p[:, 0:NW], UE[:, tc * 512 + g * 128:tc * 512 + (g + 1) * 128], cl[b][:, tc * NW:(tc + 1) * NW],
                                    tc == 0, False, [TUE[tc], Tcl[b]], [Tp])
                            self.mm(p[:, NW:2 * NW], UO[:, tc * 512 + g * 128:tc * 512 + (g + 1) * 128], sl[b][:, tc * NW:(tc + 1) * NW],
                                    False, tc == HC - 1, [TUE[tc], Tcl[b]], [Tp])
                    for g in range(4):
                        p, Tp = banks[g]
                        self.mm(p[:, 0:NW], U[0:1, (2 + HC) * 512 + g * 128:(2 + HC) * 512 + (g + 1) * 128], alt[0:1, nt * NW:(nt + 1) * NW],
                                False, True, [TU, Tcs], [Tp])
                for g in range(4):
                    p, Tp = banks[g]
                    self.cp("act" if g % 2 == 0 else "dve", z[:, g * 2 * NW:(g + 1) * 2 * NW], p[:, :2 * NW], [Tp], [Tz[g]])
                pa, Tpa = self.psn()
                pb, Tpb = self.psn()
                for g in range(4):
                    p, Tp = (pa, Tpa) if g < 2 else (pb, Tpb)
                    o = (g % 2) * NW
                    self.mm(p[:, o:o + NW], cs128[:, 0:128], z[:, g * 2 * NW:g * 2 * NW + NW], g % 2 == 0, False, [Tz[g], Tcs], [Tp])
                    self.mm(p[:, o:o + NW], cs128[:, 128:256], z[:, g * 2 * NW + NW:(g + 1) * 2 * NW], False, True, [Tz[g], Tcs], [Tp])
                self.cp("act", yf[b][:, 0:2 * NW], pa[:, :2 * NW], [Tpa], [Tyf[b]])
                self.cp("dve", yf[b][:, 2 * NW:4 * NW], pb[:, :2 * NW], [Tpb], [Tyf[b]])
                S.dma("pool", self.YT[0:512, col0:col0 + NW].rearrange("(j p) n -> p j n", p=128),
                      yf[b][:, :].rearrange("p (j n) -> p j n", j=4), reads=[Tyf[b]])

    def phase_outproj(self, l, xsrc, do_ctx):
        S, I = self.S, self.I
        with contextlib.ExitStack() as st:
            wo = self.sb(st, [128, 8 * D], BF16, "wo")
            Two = T()
            for hh in range(2):
                S.dma("pool", wo[:, hh * 4096:(hh + 1) * 4096].rearrange("p (k n) -> p k n", k=4),
                      I[f"w_out{l}"][hh * 512:(hh + 1) * 512, :].rearrange("(k p) n -> p k n", p=128), writes=[Two])
            yc = [self.sb(st, [128, 8 * 512], BF16) for _ in range(2)]
            Tyc = TL(2)
            xt = [self.sb(st, [128, 8 * 512], F32) for _ in range(2)]
            Txt = TL(2)
            tiles = TILES if do_ctx else TILES[1:]
            for ti, (c0, n) in enumerate(tiles):
                a = 1 if c0 == 0 else 0
                b = ti % 2
                S.dma("sp", yc[b][:, :8 * n].rearrange("p (k n) -> p k n", k=8),
                      self.YT[:, c0:c0 + n].rearrange("(k p) n -> p k n", p=128), writes=[Tyc[b]])
                S.dma("sp", xt[b][:, :8 * n].rearrange("p (k n) -> p k n", k=8),
                      xsrc[:, c0:c0 + n].rearrange("(k p) n -> p k n", p=128), writes=[Txt[b]])
                for i in range(8):
                    p, Tp = self.psn()
                    for k in range(8):
                        self.mm(p[:, :n], wo[:, k * D + i * 128:k * D + (i + 1) * 128], yc[b][:, k * n:(k + 1) * n],
                                k == 0, k == 7, [Two, Tyc[b]], [Tp])
                    self.stt(xt[b][:, i * n:(i + 1) * n], p[:, :n], self.modt[l][:, a * 48 + 16 + i:a * 48 + 17 + i],
                             xt[b][:, i * n:(i + 1) * n], ALU.mult, ALU.add, [Tp, self.Tmod], [Txt[b]])
                S.dma("pool", self.XS[:, c0:c0 + n].rearrange("(k p) n -> p k n", p=128),
                      xt[b][:, :8 * n].rearrange("p (k n) -> p k n", k=8), reads=[Txt[b]])


    def phase_moe(self, l):
        S, I = self.S, self.I
        NBLK = SEQ // 128
        a = 0
        NG = EFF // 512
        G = 4
        with contextlib.ExitStack() as st0:
            rw = self.sb(st0, [128, 8 * NEXP], F32)
            su = self.sb(st0, [128, 128], F32)
            ones128 = self.sb(st0, [128, 128], F32)
            identb = self.sb(st0, [128, 128], BF16)
            slotstart = self.sb(st0, [128, NSLOT], F32)
            gp7 = self.sb(st0, [128, NG], F32)
            fnw = self.sb(st0, [128, 8], F32)
            Tf = T()
            S.dma("sp", rw[:, :].rearrange("p (k e) -> p k e", k=8), I["router_w"].rearrange("(k p) e -> p k e", p=128), writes=[Tf])
            S.dma("sp", su[:], I["su"][:, :], writes=[Tf])
            S.dma("sp", identb[:], I["identb"][:, :], writes=[Tf])
            S.dma("sp", slotstart[:], I["slotstart"][:, :], writes=[Tf])
            S.dma("sp", gp7[:], I["gp7"][:, :], writes=[Tf])
            S.dma("sp", fnw[:], I["fnw"][:, :], writes=[Tf])
            self.memset("pool", ones128[:], 1.0, [Tf])
            IND = [self.sb(st0, [128, NBLK * 8], F32) for _ in range(2)]
            TOPA = self.sb(st0, [128, NBLK * 8], F32)
            R8A = self.sb(st0, [128, NBLK * 8], F32)
            BP = self.sb(st0, [128, (NBLK + 1) * 8], F32)
            ecap = self.sb(st0, [128, 8], F32)
            dsi = self.sb(st0, [128, 2 * NBLK], I32)
            Tds = TL(NBLK)
            TR8 = TL(NBLK)
            xidx = self.sb(st0, [128, 4 * NSLOT], I32)
            Txi = T()
            GK = [self.sb(st0, [128, NBLK], F32) for _ in range(2)]
            TI = TL(NBLK)
            TBP, TGK = T(), T()
            S.dma("sp", ecap[:], I["ecap"][:, :], writes=[Tf])
            S.dma("sp", BP[:, 0:8], I["ecap"][:, :], writes=[TBP])
            desti = self.sb(st0, [128, 2 * NBLK], I32)
            Tdd = T()
            widx = self.sb(st0, [128, NG * NSLOT], I32)
            Twi = T()
            TXG = T()
            with contextlib.ExitStack() as st:
                h2tm = self.sb(st, [128, NBLK * D], BF16, "h2tm")
                Th2tm = TL(NBLK)
                xt = [self.sb(st, [128, 8 * 512], F32, "xt") for _ in range(2)]
                Txt = TL(2)
                sq = self.sb(st, [128, 8 * 512], BF16)
                Tsq = TL(8)
                rstd = self.sb(st, [128, 512], F32)
                Trs = T()
                tmpb = [self.sb(st, [128, 512], F32) for _ in range(2)]
                Ttmp = TL(2)
                hFk = [self.sb(st, [128, 512], F32) for _ in range(2)]
                ThF = TL(2)
                h2T = [self.sb(st, [128, 8 * 512], BF16) for _ in range(2)]
                Th2T = [TL(8) for _ in range(2)]
                ind = [self.sb(st, [128, 8], F32) for _ in range(4)]
                Tind = TL(4)
                t8 = [[self.sb(st, [128, 8], F32) for _ in range(2)] for _ in range(4)]
                dsf = [self.sb(st, [128, 2], F32) for _ in range(4)]
                Tt8 = TL(4)
                for ti, (c0, n) in enumerate(TILES[1:]):
                    b = ti % 2
                    S.dma("sp", xt[b][:, :].rearrange("p (k n) -> p k n", k=8),
                          self.XS[:, c0:c0 + n].rearrange("(k p) n -> p k n", p=128), writes=[Txt[b]])
                    for k in range(8):
                        self.act(sq[:, k * n:(k + 1) * n], xt[b][:, k * n:(k + 1) * n], AF.Square, [Txt[b]], [Tsq[k]])
                    p, Tp = self.psn()
                    for k in range(8):
                        self.mm(p[:, :n], self.ones_bf[:], sq[:, k * n:(k + 1) * n], k == 0, k == 7, [Tsq[k], self.Tc], [Tp])
                    self.rstd_from_ps(rstd, Trs, p, Tp, n, 1.0 / D)
                    pl, Tpl = self.psn()
                    for k in range(8):
                        tb_ = k % 2
                        self.stt(tmpb[tb_][:, :n], xt[b][:, k * n:(k + 1) * n], self.A2[l][:, a * 8 + k:a * 8 + k + 1], rstd[:, :n],
                                 ALU.mult, ALU.mult, [Txt[b], Trs, self.Tmod], [Ttmp[tb_]])
                        self.act(h2T[b][:, k * n:(k + 1) * n], tmpb[tb_][:, :n], AF.Identity, [Ttmp[tb_], self.Tmod], [Th2T[b][k]],
                                 bias=self.modt[l][:, a * 48 + 24 + k:a * 48 + 25 + k])
                        self.act(hFk[tb_][:, :n], tmpb[tb_][:, :n], AF.Identity, [Ttmp[tb_], self.Tmod], [ThF[tb_]],
                                 bias=self.modt[l][:, a * 48 + 24 + k:a * 48 + 25 + k])
                        for tb in range(4):
                            self.mm(pl[:, tb * 8:(tb + 1) * 8], hFk[tb_][:, tb * 128:(tb + 1) * 128], rw[:, k * 8:(k + 1) * 8],
                                    k == 0 and tb == 0, k == 7, [ThF[tb_], Tf], [Tpl])
                    for tb in range(4):
                        blk = ti * 4 + tb
                        pt, Tpt = self.psn()
                        ptb = pt[:, :].bitcast(BF16)
                        for k in range(8):
                            S.op("pe", lambda e, ptb=ptb, k=k, src=h2T[b][:, k * n + tb * 128:k * n + (tb + 1) * 128]:
                                 e.transpose(ptb[:, k * 128:(k + 1) * 128], src, identb[:, :]), reads=[Th2T[b][k], Tf], writes=[Tpt])
                        self.cp("act", h2tm[:, blk * D:(blk + 1) * D], ptb[:, 0:D], [Tpt], [Th2tm[blk]])
                        plb = pl[:, tb * 8:(tb + 1) * 8]
                        top8 = TOPA[:, blk * 8:(blk + 1) * 8]
                        i1 = IND[0][:, blk * 8:(blk + 1) * 8]
                        i2 = IND[1][:, blk * 8:(blk + 1) * 8]
                        S.op("dve", lambda e, top8=top8, plb=plb: e.max(out=top8, in_=plb), reads=[Tpl], writes=[TI[blk]])
                        self.ts("dve", i1, plb, top8[:, 0:1], None, ALU.is_equal, None, [Tpl, TI[blk]], [TI[blk]])
                        self.ts("dve", i2, plb, top8[:, 1:2], None, ALU.is_equal, None, [Tpl, TI[blk]], [TI[blk]])
                        self.tt("dve", ind[tb][:, :], i1, i2, ALU.add, [TI[blk]], [Tind[tb]])
                        pp, Tpp = self.psn()
                        self.mm(pp[:, 0:8], su[:, :], ind[tb][:, :], True, True, [Tind[tb], Tf], [Tpp])
                        self.mm(pp[:, 8:16], ones128[:, :], ind[tb][:, :], False, True, [Tind[tb], Tf], [Tpp])
                        r8 = R8A[:, blk * 8:(blk + 1) * 8]
                        self.tt("dve", r8, BP[:, blk * 8:(blk + 1) * 8], pp[:, 0:8], ALU.add, [Tpp, TBP], [TR8[blk]])
                        self.tt("dve", BP[:, (blk + 1) * 8:(blk + 2) * 8], BP[:, blk * 8:(blk + 1) * 8], pp[:, 8:16], ALU.add, [Tpp, TBP], [TBP])
                        for kk in range(2):
                            self.tt("dve", t8[tb][kk][:, :], IND[kk][:, blk * 8:(blk + 1) * 8], r8, ALU.mult, [TI[blk], TR8[blk]], [Tt8[tb]])
                            S.op("dve", lambda e, tb=tb, kk=kk: e.reduce_sum(out=dsf[tb][:, kk:kk + 1], in_=t8[tb][kk][:, :], axis=mybir.AxisListType.X),
                                 reads=[Tt8[tb]], writes=[Tt8[tb]])
                        self.cp("dve", dsi[:, blk * 2:blk * 2 + 2], dsf[tb][:, 0:2], [Tt8[tb]], [Tds[blk]])
                        for kk in range(2):
                            def sc1(e, q=blk * 2 + kk, blk=blk):
                                return e.indirect_dma_start(out=self.XG[:, :], out_offset=bass.IndirectOffsetOnAxis(ap=dsi[:, q:q + 1], axis=0),
                                                            in_=h2tm[:, blk * D:(blk + 1) * D], in_offset=None,
                                                            bounds_check=self.breg(e, NEXP * CAP - 1), oob_is_err=False)
                            S.dma_custom("pool", sc1, reads=[Tds[blk], Th2tm[blk]], writes=[T()])
                run = self.sb(st, [128, 8], F32)
                self.tt("dve", run[:, :], BP[:, NBLK * 8:(NBLK + 1) * 8], ecap[:, :], ALU.subtract, [TBP, Tf], [TBP])
                gd = self.sb(st, [128, NBLK], F32)
                Tg = T()
                self.tt("dve", gd[:, :], TOPA[:, 1:NBLK * 8:8], TOPA[:, 0:NBLK * 8:8], ALU.subtract, TI, [Tg])
                self.act(gd[:, :], gd[:, :], AF.Exp, [Tg], [Tg])
                self.ts("dve", gd[:, :], gd[:, :], 1.0, None, ALU.add, None, [Tg], [Tg])
                self.recip(GK[0][:, :], gd[:, :], [Tg], [TGK])
                self.ts("dve", GK[1][:, :], GK[0][:, :], -1.0, 1.0, ALU.mult, ALU.add, [TGK], [TGK])
                destf = self.sb(st, [128, 2 * NBLK], F32)
                Tm = [self.sb(st, [128, NBLK * 8], F32) for _ in range(2)]
                r32 = [self.sb(st, [128, NBLK], F32) for _ in range(2)]
                Tdf = TL(2)
                for kk in range(2):
                    dk = destf[:, kk * NBLK:(kk + 1) * NBLK]
                    self.tt("dve", Tm[kk][:, :], IND[kk][:, :], R8A[:, :], ALU.mult, TI + TR8, [Tdf[kk]])
                    S.op("dve", lambda e, kk=kk, dk=dk: e.reduce_sum(out=dk, in_=Tm[kk][:, :].rearrange("p (b e) -> p b e", e=8),
                                                                      axis=mybir.AxisListType.X), reads=[Tdf[kk]], writes=[Tdf[kk]])
                pad = self.sb(st, [128, 8], F32)
                base = self.sb(st, [128, 8], F32)
                ends = self.sb(st, [128, 8], F32)
                esf = self.sb(st, [128, NSLOT], F32)
                est = [self.sb(st, [128, NSLOT], F32) for _ in range(2)]
                widf = self.sb(st, [128, NG * NSLOT], F32)
                Tb = T()
                for e_ in range(8):
                    self.ts("dve", est[e_ % 2][:, :], slotstart[:, :], run[:, e_:e_ + 1], None, ALU.is_lt, None, [TBP, Tf, Tb], [Tb])
                    S.op("dve", lambda e, e_=e_: e.reduce_sum(out=pad[:, e_:e_ + 1], in_=est[e_ % 2][:, :], axis=mybir.AxisListType.X),
                         reads=[Tb], writes=[Tb])
                self.ts("dve", pad[:, :], pad[:, :], float(TS), None, ALU.mult, None, [Tb], [Tb])
                self.memset("dve", base[:, 0:1], 0.0, [Tb])
                for e_ in range(1, 8):
                    self.tt("dve", base[:, e_:e_ + 1], base[:, e_ - 1:e_], pad[:, e_ - 1:e_], ALU.add, [Tb], [Tb])
                self.tt("dve", ends[:, :], base[:, :], pad[:, :], ALU.add, [Tb], [Tb])
                bos = self.sb(st, [128, NSLOT], F32)
                bm = self.sb(st, [128, 8], F32)
                self.tt("dve", bm[:, :], base[:, :], ecap[:, :], ALU.subtract, [Tb, Tf], [Tb])
                self.memset("dve", esf[:, :], 0.0, [Tb])
                self.memset("dve", bos[:, :], 0.0, [Tb])
                for e_ in range(8):
                    self.ts("dve", est[e_ % 2][:, :], slotstart[:, :], ends[:, e_:e_ + 1], None, ALU.is_ge, None, [Tb, Tf], [Tb])
                    self.tt("dve", esf[:, :], esf[:, :], est[e_ % 2][:, :], ALU.add, [Tb], [Tb])
                    self.stt(bos[:, :], est[e_ % 2][:, :], pad[:, e_:e_ + 1], bos[:, :], ALU.mult, ALU.add, [Tb], [Tb])
                self.ts("dve", esf[:, :], esf[:, :], 7.0, None, ALU.min, None, [Tb], [Tb])
                for g in range(NG):
                    self.ts("dve", widf[:, g * NSLOT:(g + 1) * NSLOT], esf[:, :], float(NG * 128), gp7[:, g:g + 1], ALU.mult, ALU.add,
                            [Tb, Tf], [Tb])
                self.cp("dve", widx[:, :], widf[:, :], [Tb], [Twi])
                xoff = self.sb(st, [128, NSLOT], F32)
                xidf = self.sb(st, [128, 4 * NSLOT], F32)
                self.stt(xoff[:, :], esf[:, :], float(CAP), slotstart[:, :], ALU.mult, ALU.add, [Tb, Tf], [Tb])
                self.tt("dve", xoff[:, :], xoff[:, :], bos[:, :], ALU.subtract, [Tb], [Tb])
                for t_ in range(4):
                    self.ts("dve", xidf[:, t_ * NSLOT:(t_ + 1) * NSLOT], xoff[:, :], gp7[:, t_:t_ + 1], None, ALU.add, None, [Tb, Tf], [Tb])
                self.cp("dve", xidx[:, :], xidf[:, :], [Tb], [Txi])
                for e_ in range(8):
                    for kk in range(2):
                        dk = destf[:, kk * NBLK:(kk + 1) * NBLK]
                        self.stt(dk, IND[kk][:, e_:NBLK * 8:8], bm[:, e_:e_ + 1], dk, ALU.mult, ALU.add, TI + [Tb, Tdf[kk]], [Tdf[kk]])
                self.cp("dve", desti[:, :], destf[:, :], Tdf, [Tdd])
            S.barrier()
            with contextlib.ExitStack() as st:
                NWB = 3
                w13 = [self.sb(st, [128, 2 * 8 * 512], BF16, "w13") for _ in range(NWB)]
                w2 = [self.sb(st, [128, G * D], BF16, "w2") for _ in range(NWB)]
                Tw = [TL(3) for _ in range(NWB)]
                xg_tm = [self.sb(st, [128, 4 * D], BF16, "xgtm") for _ in range(2)]
                Txg = [TL(4) for _ in range(2)]
                xgT = [self.sb(st, [128, 8 * TS], BF16, "xgT") for _ in range(2)]
                TxgT = TL(2)
                actb = [self.sb(st, [128, G * TS], BF16, "actb") for _ in range(2)]
                Tab = [TL(G) for _ in range(2)]
                sil = [self.sb(st, [128, TS], F32) for _ in range(2)]
                Tsil = TL(2)
                acc = [self.sb(st, [128, 4 * D], F32, "acctm") for _ in range(2)]
                Tacc = [TL(4) for _ in range(2)]
                jobs = [(s_, gi) for s_ in range(NSLOT) for gi in range(NG)]
                NROW = NEXP * NG * 128

                def issue_w(ji):
                    s_, gi = jobs[ji]
                    wb = ji % NWB
                    ixap = widx[:, gi * NSLOT + s_:gi * NSLOT + s_ + 1]
                    for wi, (wname, dst) in enumerate((("mw1", w13[wb][:, 0:4096]), ("mw3", w13[wb][:, 4096:8192]), ("mw2", w2[wb][:, :]))):
                        def f(e, wname=wname, dst=dst, ixap=ixap):
                            return e.indirect_dma_start(out=dst, out_offset=None, in_=I[wname][:, :],
                                                        in_offset=bass.IndirectOffsetOnAxis(ap=ixap, axis=0),
                                                        bounds_check=self.breg(e, NROW - 1), oob_is_err=False)
                        S.dma_custom("pool", f, reads=[Twi], writes=[Tw[wb][wi]])

                def issue_x(s_):
                    xb = s_ % 2
                    for t_ in range(4):
                        def gx(e, s_=s_, t_=t_, dst=xg_tm[xb][:, t_ * D:(t_ + 1) * D]):
                            return e.indirect_dma_start(out=dst, out_offset=None, in_=self.XG[:, :],
                                                        in_offset=bass.IndirectOffsetOnAxis(ap=xidx[:, t_ * NSLOT + s_:t_ * NSLOT + s_ + 1], axis=0),
                                                        bounds_check=self.breg(e, NEXP * CAP - 1), oob_is_err=False)
                        S.dma_custom("pool", gx, reads=[Txi], writes=[Txg[xb][t_]])

                issue_w(0)
                issue_w(1)
                issue_x(0)
                TY = T()
                for ji, (s_, gi) in enumerate(jobs):
                    wb = ji % NWB
                    xb = s_ % 2
                    if ji + 2 < len(jobs):
                        issue_w(ji + 2)
                    if gi == 0:
                        if s_ + 1 < NSLOT:
                            issue_x(s_ + 1)
                        for tb in range(4):
                            pt, Tpt = self.psn()
                            ptb = pt[:, :].bitcast(BF16)
                            for k in range(8):
                                S.op("pe", lambda e, ptb=ptb, k=k, src=xg_tm[xb][:, tb * D + k * 128:tb * D + (k + 1) * 128]:
                                     e.transpose(ptb[:, k * 128:(k + 1) * 128], src, identb[:, :]), reads=[Txg[xb][tb], Tf], writes=[Tpt])
                            self.cp("act" if tb % 2 == 0 else "dve",
                                    xgT[xb][:, :].rearrange("p (k n) -> p k n", k=8)[:, :, tb * 128:(tb + 1) * 128],
                                    ptb[:, 0:D].rearrange("p (k n) -> p k n", k=8), [Tpt], [TxgT[xb]])
                    ab = ji % 2
                    for j in range(G):
                        pa, Tpa = self.psn()
                        pb, Tpb = self.psn()
                        for k in range(8):
                            self.mm(pa[:, :TS], w13[wb][:, k * 512 + j * 128:k * 512 + (j + 1) * 128], xgT[xb][:, k * TS:(k + 1) * TS],
                                    k == 0, k == 7, [Tw[wb][0], TxgT[xb]], [Tpa])
                        for k in range(8):
                            self.mm(pb[:, :TS], w13[wb][:, 4096 + k * 512 + j * 128:4096 + k * 512 + (j + 1) * 128], xgT[xb][:, k * TS:(k + 1) * TS],
                                    k == 0, k == 7, [Tw[wb][1], TxgT[xb]], [Tpb])
                        sb_ = j % 2
                        self.act(sil[sb_][:, :], pa[:, :TS], AF.Silu, [Tpa], [Tsil[sb_]])
                        self.tt("dve", actb[ab][:, j * TS:(j + 1) * TS], sil[sb_][:, :], pb[:, :TS], ALU.mult, [Tpb, Tsil[sb_]], [Tab[ab][j]])
                    for tb in range(4):
                        for hf in range(2):
                            po, Tpo = self.psn()
                            for j in range(G):
                                self.mm(po[:, :512], actb[ab][:, j * TS + tb * 128:j * TS + (tb + 1) * 128],
                                        w2[wb][:, j * D + hf * 512:j * D + (hf + 1) * 512], j == 0, j == G - 1, [Tw[wb][2], Tab[ab][j]], [Tpo])
                            dst = acc[xb][:, tb * D + hf * 512:tb * D + (hf + 1) * 512]
                            if gi == 0:
                                self.cp("act", dst, po[:, :512], [Tpo], [Tacc[xb][tb]])
                            else:
                                self.tt("dve", dst, dst, po[:, :512], ALU.add, [Tpo], [Tacc[xb][tb]])
                    if gi == NG - 1:
                        S.dma("sp", self.YS[s_ * TS:(s_ + 1) * TS, :].rearrange("(t p) n -> p t n", p=128),
                              acc[xb][:, :].rearrange("p (t n) -> p t n", t=4), reads=Tacc[xb], writes=[TY])
            S.barrier()
            with contextlib.ExitStack() as st:
                ym = [self.sb(st, [128, 4 * 2 * D], F32, "ym") for _ in range(2)]
                Tym = [[TL(2) for _ in range(4)] for _ in range(2)]
                ysum = [self.sb(st, [128, 4 * D], F32, "ysum") for _ in range(2)]
                Tys = [TL(4) for _ in range(2)]
                xt = [self.sb(st, [128, 8 * 512], F32, "xt") for _ in range(2)]
                Txt = [TL(8) for _ in range(2)]
                sq = self.sb(st, [128, 8 * 512], BF16)
                Tsq = TL(8)
                rstd = self.sb(st, [128, 512], F32)
                Trs = T()
                for ti, (c0, n) in enumerate(TILES[1:]):
                    b = ti % 2
                    t0 = c0 - CTX
                    for k in range(8):
                        S.dma("sp", xt[b][:, k * n:(k + 1) * n], self.XS[k * 128:(k + 1) * 128, c0:c0 + n], writes=[Txt[b][k]])
                    for tb in range(4):
                        blk = ti * 4 + tb
                        for kk in range(2):
                            def gy(e, q=kk * NBLK + blk, dst=ym[b][:, (tb * 2 + kk) * D:(tb * 2 + kk + 1) * D]):
                                return e.indirect_dma_start(out=dst, out_offset=None, in_=self.YS[:, :],
                                                            in_offset=bass.IndirectOffsetOnAxis(ap=desti[:, q:q + 1], axis=0),
                                                            bounds_check=self.breg(e, NSLOT * TS - 1), oob_is_err=False)
                            S.dma_custom("pool", gy, reads=[Tdd], writes=[Tym[b][tb][kk]])
                        self.ts("dve", ysum[b][:, tb * D:(tb + 1) * D], ym[b][:, tb * 2 * D:tb * 2 * D + D], GK[0][:, blk:blk + 1], None,
                                ALU.mult, None, [Tym[b][tb][0], TGK], [Tys[b][tb]])
                        self.stt(ysum[b][:, tb * D:(tb + 1) * D], ym[b][:, tb * 2 * D + D:(tb + 1) * 2 * D], GK[1][:, blk:blk + 1],
                                 ysum[b][:, tb * D:(tb + 1) * D], ALU.mult, ALU.add, [Tym[b][tb][1], TGK], [Tys[b][tb]])
                    for k in range(8):
                        p, Tp = self.psn()
                        for tb in range(4):
                            S.op("pe", lambda e, p=p, tb=tb, src=ysum[b][:, tb * D + k * 128:tb * D + (k + 1) * 128]:
                                 e.transpose(p[:, tb * 128:(tb + 1) * 128], src, self.ident[:, :]), reads=[Tys[b][tb], self.Tc], writes=[Tp])
                        self.stt(xt[b][:, k * n:(k + 1) * n], p[:, :n], self.modt[l][:, a * 48 + 40 + k:a * 48 + 41 + k],
                                 xt[b][:, k * n:(k + 1) * n], ALU.mult, ALU.add, [Tp, self.Tmod], [Txt[b][k]])
                    for k in range(8):
                        self.act(sq[:, k * n:(k + 1) * n], xt[b][:, k * n:(k + 1) * n], AF.Square, [Txt[b][k]], [Tsq[k]])
                    p, Tp = self.psn()
                    for k in range(8):
                        self.mm(p[:, :n], self.ones_bf[:], sq[:, k * n:(k + 1) * n], k == 0, k == 7, [Tsq[k], self.Tc], [Tp])
                    self.rstd_from_ps(rstd, Trs, p, Tp, n, 1.0 / D)
                    for k in range(8):
                        self.stt(xt[b][:, k * n:(k + 1) * n], xt[b][:, k * n:(k + 1) * n], fnw[:, k:k + 1], rstd[:, :n],
                                 ALU.mult, ALU.mult, [Trs, Tf], [Txt[b][k]])
                        S.dma("sp", self.out[k * 128:(k + 1) * 128, t0:t0 + n], xt[b][:, k * n:(k + 1) * n], reads=[Txt[b][k]])

    def phase_ffn(self, l, last):
        S, I = self.S, self.I
        moe = (l % 2 == 1)
        G = 2 if moe else 4
        if moe:
            experts = list(range(NEXP))
            nf = EFF // 128
            W1 = lambda e: I["moe_w1"][e]
            W3 = lambda e: I["moe_w3"][e]
            W2 = lambda e: I["moe_w2"][e]
        else:
            experts = [0]
            nf = DFF // 128
            W1 = lambda e: I["ffn_w1"]
            W3 = lambda e: I["ffn_w3"]
            W2 = lambda e: I["ffn_w2"]
        groups = [(g0, min(G, nf - g0)) for g0 in range(0, nf, G)]
        if last:
            passes = [TILES[1:5], TILES[5:9]]
        else:
            passes = [TILES[0:5], TILES[5:9]]
        TB = 2304
        with contextlib.ExitStack() as st:
            acc = self.sb(st, [128, 8 * TB], F32, "acc")
            h2 = self.sb(st, [128, 8 * TB], BF16, "h2")
            w13 = [self.sb(st, [128, 2 * 8 * G * 128], BF16, "w13") for _ in range(2)]
            w2 = [self.sb(st, [128, G * D], BF16, "w2") for _ in range(2)]
            Tw = TL(2)
            actb = [self.sb(st, [128, G * 512], BF16, "actb") for _ in range(2)]
            Tab = [TL(G) for _ in range(2)]
            sil = [self.sb(st, [128, 512], F32) for _ in range(2)]
            Tsil = TL(2)
            sq = self.sb(st, [128, 8 * 512], BF16)
            Tsq = TL(8)
            rstd = self.sb(st, [128, 512], F32)
            Trs = T()
            tmpb = [self.sb(st, [128, 512], F32) for _ in range(2)]
            Ttmp = TL(2)
            fnw = self.sb(st, [128, 8], F32)
            Tf = T()
            S.dma("sp", fnw[:], I["fnw"][:, :], writes=[Tf])
            if moe:
                rw = self.sb(st, [128, 8 * NEXP], F32)
                sel = self.sb(st, [8, 1024], F32)
                S.dma("sp", rw[:, :].rearrange("p (k e) -> p k e", k=8), I["router_w"].rearrange("(k p) e -> p k e", p=128), writes=[Tf])
                S.dma("sp", sel[:], I["sel"][:, :], writes=[Tf])
                hF = self.sb(st, [128, 8 * 512], F32, "hF")
                ThF = TL(8)
                lgt = self.sb(st, [128, 8], F32)
                top = self.sb(st, [128, 8], F32)
                gat = self.sb(st, [128, 4], F32)
                cmb = self.sb(st, [128, 8], F32)
                cm2 = self.sb(st, [128, 8], F32)
                Trt = T()
                combT = self.sb(st, [8, TB], F32, "combT")
                TcT = TL(5)
                cbc = [self.sb(st, [128, 512], F32) for _ in range(2)]
                Tcb = TL(2)
            wcount = 0
            for tiles in passes:
                offs = []
                o = 0
                for (c0, n) in tiles:
                    offs.append(o)
                    o += n
                TBn = o
                Tacc = [TL(8) for _ in tiles]
                Th2 = [TL(8) for _ in tiles]
                for si, (c0, n) in enumerate(tiles):
                    a = 1 if c0 == 0 else 0
                    o = offs[si]
                    for k in range(8):
                        S.dma("sp", acc[:, k * TB + o:k * TB + o + n], self.XS[k * 128:(k + 1) * 128, c0:c0 + n], writes=[Tacc[si][k]])
                    for k in range(8):
                        self.act(sq[:, k * n:(k + 1) * n], acc[:, k * TB + o:k * TB + o + n], AF.Square, [Tacc[si][k]], [Tsq[k]])
                    p, Tp = self.psn()
                    for k in range(8):
                        self.mm(p[:, :n], self.ones_bf[:], sq[:, k * n:(k + 1) * n], k == 0, k == 7, [Tsq[k], self.Tc], [Tp])
                    self.rstd_from_ps(rstd, Trs, p, Tp, n, 1.0 / D)
                    for k in range(8):
                        tb = k % 2
                        self.stt(tmpb[tb][:, :n], acc[:, k * TB + o:k * TB + o + n], self.A2[l][:, a * 8 + k:a * 8 + k + 1], rstd[:, :n],
                                 ALU.mult, ALU.mult, [Tacc[si][k], Trs, self.Tmod], [Ttmp[tb]])
                        self.act(h2[:, k * TB + o:k * TB + o + n], tmpb[tb][:, :n], AF.Identity, [Ttmp[tb], self.Tmod], [Th2[si][k]],
                                 bias=self.modt[l][:, a * 48 + 24 + k:a * 48 + 25 + k])
                        if moe:
                            self.ts("pool", hF[:, k * n:(k + 1) * n], tmpb[tb][:, :n], self.modt[l][:, a * 48 + 24 + k:a * 48 + 25 + k], None,
                                    ALU.add, None, [Ttmp[tb], self.Tmod], [ThF[k]])
                    if moe:
                        for tb in range(n // 128):
                            p, Tp = self.psn()
                            for k in range(8):
                                self.mm(p[:, 0:8], hF[:, k * n + tb * 128:k * n + (tb + 1) * 128], rw[:, k * 8:(k + 1) * 8],
                                        k == 0, k == 7, [ThF[k], Tf], [Tp])
                            self.cp("dve", lgt[:, :], p[:, 0:8], [Tp], [Trt])
                            S.op("dve", lambda e: e.max(out=top[:, :], in_=lgt[:, :]), reads=[Trt], writes=[Trt])
                            self.tt("dve", gat[:, 0:1], top[:, 1:2], top[:, 0:1], ALU.subtract, [Trt], [Trt])
                            self.act(gat[:, 1:2], gat[:, 0:1], AF.Exp, [Trt], [Trt])
                            self.ts("dve", gat[:, 1:2], gat[:, 1:2], 1.0, None, ALU.add, None, [Trt], [Trt])
                            self.recip(gat[:, 2:3], gat[:, 1:2], [Trt], [Trt])
                            self.ts("dve", gat[:, 3:4], gat[:, 2:3], -1.0, 1.0, ALU.mult, ALU.add, [Trt], [Trt])
                            self.ts("dve", cmb[:, :], lgt[:, :], top[:, 0:1], gat[:, 2:3], ALU.is_equal, ALU.mult, [Trt], [Trt])
                            self.ts("dve", cm2[:, :], lgt[:, :], top[:, 1:2], gat[:, 3:4], ALU.is_equal, ALU.mult, [Trt], [Trt])
                            self.tt("dve", cmb[:, :], cmb[:, :], cm2[:, :], ALU.add, [Trt], [Trt])
                            p2, Tp2 = self.psn()
                            S.op("pe", lambda e, p2=p2: e.transpose(p2[0:8, 0:128], cmb[:, :], self.ident[:, :]), reads=[Trt, self.Tc], writes=[Tp2])
                            self.cp("act", combT[:, o + tb * 128:o + (tb + 1) * 128], p2[0:8, 0:128], [Tp2], [TcT[si]])
                for e in experts:
                    for (g0, gn) in groups:
                        wb = wcount % 2
                        wcount += 1
                        for (ww, off) in ((W1(e), 0), (W3(e), 8 * G * 128)):
                            for hh in range(2):
                                S.dma("pool", w13[wb][:, off + hh * 4 * G * 128:off + hh * 4 * G * 128 + 4 * gn * 128].rearrange("p (k n) -> p k n", k=4),
                                      ww[hh * 512:(hh + 1) * 512, g0 * 128:(g0 + gn) * 128].rearrange("(k p) n -> p k n", p=128), writes=[Tw[wb]])
                        S.dma("pool", w2[wb][:, :gn * D].rearrange("p (j n) -> p j n", j=gn),
                              W2(e)[g0 * 128:(g0 + gn) * 128, :].rearrange("(j p) n -> p j n", p=128), writes=[Tw[wb]])
                        for si, (c0, n) in enumerate(tiles):
                            a = 1 if c0 == 0 else 0
                            o = offs[si]
                            ab = si % 2
                            if moe:
                                cb = si % 2
                                pc, Tpc = self.psn()
                                self.mm(pc[:, :n], sel[:, e * 128:(e + 1) * 128], combT[:, o:o + n], True, True, [Tf, TcT[si]], [Tpc])
                                self.cp("act", cbc[cb][:, :n], pc[:, :n], [Tpc], [Tcb[cb]])
                            for j in range(gn):
                                pa, Tpa = self.psn()
                                pb, Tpb = self.psn()
                                wrow = (lambda k, off: w13[wb][:, off + (k // 4) * 4 * G * 128 + (k % 4) * gn * 128 + j * 128:
                                                               off + (k // 4) * 4 * G * 128 + (k % 4) * gn * 128 + (j + 1) * 128])
                                for k in range(8):
                                    self.mm(pa[:, :n], wrow(k, 0), h2[:, k * TB + o:k * TB + o + n], k == 0, k == 7, [Tw[wb], Th2[si][k]], [Tpa])
                                for k in range(8):
                                    self.mm(pb[:, :n], wrow(k, 8 * G * 128), h2[:, k * TB + o:k * TB + o + n], k == 0, k == 7, [Tw[wb], Th2[si][k]], [Tpb])
                                sb_ = j % 2
                                self.act(sil[sb_][:, :n], pa[:, :n], AF.Silu, [Tpa], [Tsil[sb_]])
                                if moe:
                                    self.tt("dve", sil[sb_][:, :n], sil[sb_][:, :n], pb[:, :n], ALU.mult, [Tpb, Tsil[sb_]], [Tsil[sb_]])
                                    self.tt("pool", actb[ab][:, j * 512:j * 512 + n], sil[sb_][:, :n], cbc[cb][:, :n], ALU.mult,
                                            [Tsil[sb_], Tcb[cb]], [Tab[ab][j]])
                                else:
                                    self.tt("dve", actb[ab][:, j * 512:j * 512 + n], sil[sb_][:, :n], pb[:, :n], ALU.mult,
                                            [Tpb, Tsil[sb_]], [Tab[ab][j]])
                            for i in range(8):
                                po, Tpo = self.psn()
                                for j in range(gn):
                                    self.mm(po[:, :n], w2[wb][:, j * D + i * 128:j * D + (i + 1) * 128], actb[ab][:, j * 512:j * 512 + n],
                                            j == 0, j == gn - 1, [Tw[wb], Tab[ab][j]], [Tpo])
                                self.stt(acc[:, i * TB + o:i * TB + o + n], po[:, :n], self.modt[l][:, a * 48 + 40 + i:a * 48 + 41 + i],
                                         acc[:, i * TB + o:i * TB + o + n], ALU.mult, ALU.add, [Tpo, self.Tmod], [Tacc[si][i]])
                for si, (c0, n) in enumerate(tiles):
                    o = offs[si]
                    if not last:
                        for k in range(8):
                            S.dma("pool", self.XS[k * 128:(k + 1) * 128, c0:c0 + n], acc[:, k * TB + o:k * TB + o + n], reads=[Tacc[si][k]])
                    else:
                        for k in range(8):
                            self.act(sq[:, k * n:(k + 1) * n], acc[:, k * TB + o:k * TB + o + n], AF.Square, [Tacc[si][k]], [Tsq[k]])
                        p, Tp = self.psn()
                        for k in range(8):
                            self.mm(p[:, :n], self.ones_bf[:], sq[:, k * n:(k + 1) * n], k == 0, k == 7, [Tsq[k], self.Tc], [Tp])
                        self.rstd_from_ps(rstd, Trs, p, Tp, n, 1.0 / D)
                        for k in range(8):
                            self.stt(acc[:, k * TB + o:k * TB + o + n], acc[:, k * TB + o:k * TB + o + n], fnw[:, k:k + 1], rstd[:, :n],
                                     ALU.mult, ALU.mult, [Trs, Tf], [Tacc[si][k]])
                            S.dma("pool", self.out[k * 128:(k + 1) * 128, c0 - CTX:c0 - CTX + n], acc[:, k * TB + o:k * TB + o + n],
                                  reads=[Tacc[si][k]])
                S.barrier()


_CONST = {}


def _consts():
    if _CONST:
        return _CONST
    bf = ml_dtypes.bfloat16
    t = np.arange(SEQ, dtype=np.int64)
    ang = 2.0 * np.pi * ((t[:, None] * t[None, :]) % SEQ).astype(np.float64) / SEQ
    _CONST["dftc"] = (np.cos(ang) / 64.0).astype(np.float32).astype(bf)
    _CONST["dfts"] = (np.sin(ang) / 64.0).astype(np.float32).astype(bf)
    t = np.arange(CTX, dtype=np.int64)
    ang = 2.0 * np.pi * ((t[:, None] * t[None, :]) % CTX).astype(np.float64) / CTX
    _CONST["dftc256"] = (np.cos(ang) / 16.0).astype(np.float32).astype(bf)
    _CONST["dfts256"] = (np.sin(ang) / 16.0).astype(np.float32).astype(bf)
    t = np.arange(128, dtype=np.int64)
    ang = 2.0 * np.pi * ((t[:, None] * t[None, :]) % 128).astype(np.float64) / 128
    s = 1.0 / np.sqrt(128.0)
    _CONST["cs128"] = np.concatenate([np.cos(ang) * s, -np.sin(ang) * s], axis=1).astype(np.float32).astype(bf)
    s_, t_ = np.meshgrid(np.arange(128), np.arange(128), indexing="ij")
    sc = -1.0 / 16.0
    tri = np.concatenate([(s_ <= t_), (s_ > t_), (s_ >= t_), (s_ < t_)], axis=1).astype(np.float32) * sc
    _CONST["tri"] = np.ascontiguousarray(tri)
    mf = (s_ <= t_).astype(np.float32)
    mb = (s_ >= t_).astype(np.float32)
    _CONST["mask"] = np.concatenate([mf] * 4 + [mb] * 4, axis=1).astype(bf)
    _CONST["ident"] = np.eye(128, dtype=np.float32)
    sel = np.zeros((8, 1024), np.float32)
    for e in range(8):
        sel[e, e * 128:(e + 1) * 128] = 1.0
    _CONST["sel"] = sel
    _CONST["su"] = (s_ < t_).astype(np.float32)
    _CONST["identb"] = np.eye(128, dtype=np.float32).astype(bf)
    _CONST["slotstart"] = np.ascontiguousarray(np.broadcast_to((np.arange(NSLOT, dtype=np.float32) * TS)[None, :], (128, NSLOT)))
    jsh = np.zeros((128, 256), np.float32)
    for p_ in range(1, 128):
        jsh[128 - p_, p_] = 1.0
    jsh[0, 128] = 1.0
    _CONST["jsh"] = jsh.astype(bf)
    _CONST["alt"] = (((-1.0) ** np.arange(SEQ)) / 64.0).astype(np.float32).astype(bf)[None, :]
    _CONST["ecap"] = np.ascontiguousarray(np.broadcast_to((np.arange(8, dtype=np.float32) * CAP)[None, :], (128, 8)))
    _CONST["gp7"] = np.ascontiguousarray((np.arange(7)[None, :] * 128 + np.arange(128)[:, None]).astype(np.float32))
    return _CONST


def _pcol(v, nchunk):
    return np.ascontiguousarray(np.asarray(v, np.float32).reshape(nchunk, 128).T)


def make_in_maps(inputs, cores):
    C = _consts()
    f = lambda a: np.ascontiguousarray(np.asarray(a, np.float32))
    shared = dict(C)
    for l in range(DEPTH):
        shared[f"w_mod{l}"] = f(inputs["w_mod"][l])
        shared[f"b_mod{l}"] = _pcol(inputs["b_mod"][l], 48)
        shared[f"nmw{l}"] = _pcol(inputs["norm_mix_w"][l], 8)
        shared[f"nfw{l}"] = _pcol(inputs["norm_ffn_w"][l], 8)
        shared[f"w_in{l}"] = f(inputs["w_in"][l])
        shared[f"wg{l}"] = np.ascontiguousarray(np.concatenate([f(inputs["w_gate_f"][l]), f(inputs["w_gate_b"][l])], axis=1))
        shared[f"bg{l}"] = np.ascontiguousarray(np.concatenate([f(inputs["b_gate_f"][l]), f(inputs["b_gate_b"][l])])[None, :])
        shared[f"gnw{l}"] = f(inputs["gla_norm_w"][l]).reshape(128, 1)
        shared[f"w_out{l}"] = f(inputs["w_out"][l])
    shared["ffn_w1"] = f(inputs["ffn_w1"][0])
    shared["ffn_w3"] = f(inputs["ffn_w3"][0])
    shared["ffn_w2"] = f(inputs["ffn_w2"][0])
    shared["router_w"] = f(inputs["router_w"][0])
    for nm, src in (("mw1", "moe_w1"), ("mw3", "moe_w3")):
        w = np.asarray(inputs[src][0], np.float32).reshape(NEXP, 8, 128, 7, 512)
        shared[nm] = np.ascontiguousarray(w.transpose(0, 3, 2, 1, 4)).reshape(NEXP * 7 * 128, 8 * 512)
    w = np.asarray(inputs["moe_w2"][0], np.float32).reshape(NEXP, 7, 4, 128, D)
    shared["mw2"] = np.ascontiguousarray(w.transpose(0, 1, 3, 2, 4)).reshape(NEXP * 7 * 128, 4 * D)
    shared["fnw"] = _pcol(inputs["final_norm_w"], 8)
    x = np.asarray(inputs["x"], np.float32)
    ctx = np.asarray(inputs["ctx"], np.float32)
    c = np.asarray(inputs["c"], np.float32)
    cc = np.asarray(inputs["c_ctx"], np.float32)
    maps = []
    for b in cores:
        m = dict(shared)
        m["xt"] = np.ascontiguousarray(np.concatenate([ctx[b], x[b]], axis=0).T)
        m["cvec"] = np.ascontiguousarray(np.concatenate([_pcol(c[b], 8), _pcol(cc, 8)], axis=1))
        maps.append(m)
    return maps


_NC = {}


def kernel(**inputs):
    if "nc" not in _NC:
        _NC["nc"] = Builder().build()
    nc = _NC["nc"]
    maps = make_in_maps(inputs, list(range(8)))
    res = run_bass_kernel_spmd(nc, maps, core_ids=list(range(8)))
    out = np.stack([np.ascontiguousarray(r["out"].T) for r in res.results], axis=0)
    return out.astype(np.float32)
```

```python
import contextlib
import os
import numpy as np
import ml_dtypes
import concourse.bass as bass
import concourse.mybir as mybir
from concourse.bass_utils import run_bass_kernel_spmd

F32 = mybir.dt.float32
BF16 = mybir.dt.bfloat16
AF = mybir.ActivationFunctionType
ALU = mybir.AluOpType

D = 1024
KD = 8
SEQ = 4096
CTX = 256
NTOK = SEQ + CTX
DEPTH = 2
IN_W = 2080
DFF = 2816
EFF = 3584
NEXP = 8
EPS = 1e-6
TILES = [(0, 256)] + [(256 + 512 * i, 512) for i in range(8)]
NCH = NTOK // 128
TS = 512
NSLOT = (2 * SEQ + NEXP * (TS - 1)) // TS
I32 = mybir.dt.int32
CAP = SEQ


class T:
    __slots__ = ("w", "r")

    def __init__(self):
        self.w = {}
        self.r = {}


def TL(n):
    return [T() for _ in range(n)]


class Sched:
    ENG = ("pe", "act", "dve", "pool", "sp")

    def __init__(self, nc, stack, ring=20):
        self.nc = nc
        self.streams = {e: [] for e in self.ENG}
        self.semobj = {}
        for e in ("pe", "act", "dve", "pool"):
            self.semobj[e] = stack.enter_context(nc.semaphore("s_" + e))
        self.cnt = {e: 0 for e in ("pe", "act", "dve", "pool")}
        self.seen = {e: {} for e in self.ENG}
        self.ring = {}
        self.ring_n = {}
        self.ring_pos = {}
        for q, n in (("sp", ring), ("pool", ring)):
            keys = []
            for i in range(n):
                k = f"d_{q}{i}"
                self.semobj[k] = stack.enter_context(nc.semaphore(k))
                keys.append(k)
            self.ring[q] = keys
            self.ring_n[q] = {k: 0 for k in keys}
            self.ring_pos[q] = 0

    def _deps(self, reads, writes):
        deps = {}
        for t in reads:
            for k, v in t.w.items():
                if deps.get(k, 0) < v:
                    deps[k] = v
        for t in writes:
            for k, v in t.w.items():
                if deps.get(k, 0) < v:
                    deps[k] = v
            for k, v in t.r.items():
                if deps.get(k, 0) < v:
                    deps[k] = v
        return deps

    def _waits(self, eng, deps, skip_own=None):
        waits = []
        seen = self.seen[eng]
        for k, v in deps.items():
            if k == skip_own:
                continue
            if seen.get(k, 0) < v:
                seen[k] = v
                waits.append((self.semobj[k], v))
        return waits

    def _mark(self, reads, writes, key, val):
        for t in reads:
            if t.r.get(key, 0) < val:
                t.r[key] = val
        for t in writes:
            t.w = {key: val}
            t.r = {}

    def op(self, eng, fn, reads=(), writes=()):
        deps = self._deps(reads, writes)
        waits = self._waits(eng, deps, skip_own=("pe" if eng == "pe" else None))
        self.cnt[eng] += 1
        val = self.cnt[eng]
        so = self.semobj[eng]

        def emit(e, waits=waits, fn=fn, so=so):
            for s, v in waits:
                e.wait_ge(s, v)
            fn(e).then_inc(so, 1)
        self.streams[eng].append(emit)
        self._mark(reads, writes, eng, val)

    def dma(self, q, out, in_, reads=(), writes=(), **kw):
        deps = self._deps(reads, writes)
        keys = self.ring[q]
        key = keys[self.ring_pos[q] % len(keys)]
        self.ring_pos[q] += 1
        prev = self.ring_n[q][key]
        if prev > 0 and deps.get(key, 0) < prev * 16:
            deps[key] = prev * 16
        waits = self._waits(q, deps)
        self.ring_n[q][key] = prev + 1
        val = (prev + 1) * 16
        so = self.semobj[key]

        def emit(e, waits=waits, so=so, out=out, in_=in_, kw=kw):
            for s, v in waits:
                e.wait_ge(s, v)
            e.dma_start(out=out, in_=in_, **kw).then_inc(so, 16)
        self.streams[q].append(emit)
        self._mark(reads, writes, key, val)

    def dma_custom(self, q, fn, reads=(), writes=()):
        deps = self._deps(reads, writes)
        keys = self.ring[q]
        key = keys[self.ring_pos[q] % len(keys)]
        self.ring_pos[q] += 1
        prev = self.ring_n[q][key]
        if prev > 0 and deps.get(key, 0) < prev * 16:
            deps[key] = prev * 16
        waits = self._waits(q, deps)
        self.ring_n[q][key] = prev + 1
        val = (prev + 1) * 16
        so = self.semobj[key]

        def emit(e, waits=waits, so=so, fn=fn):
            for s, v in waits:
                e.wait_ge(s, v)
            fn(e).then_inc(so, 16)
        self.streams[q].append(emit)
        self._mark(reads, writes, key, val)

    def barrier(self):
        allv = {}
        for e in ("pe", "act", "dve", "pool"):
            if self.cnt[e] > 0:
                allv[e] = self.cnt[e]
        for q in self.ring:
            for k, n in self.ring_n[q].items():
                if n > 0:
                    allv[k] = n * 16
        for eng in self.ENG:
            waits = self._waits(eng, dict(allv))

            def emit(e, waits=waits):
                for s, v in waits:
                    e.wait_ge(s, v)
            self.streams[eng].append(emit)

    def emit_all(self):
        with self.nc.Block() as block:
            @block.tensor
            def _(e):
                for f in self.streams["pe"]:
                    f(e)

            @block.scalar
            def _(e):
                for f in self.streams["act"]:
                    f(e)

            @block.vector
            def _(e):
                for f in self.streams["dve"]:
                    f(e)

            @block.gpsimd
            def _(e):
                for f in self.streams["pool"]:
                    f(e)

            @block.sync
            def _(e):
                for f in self.streams["sp"]:
                    f(e)


class Builder:
    def __init__(self, dbg=None, stop_after=None):
        self.dbg = dbg or set()
        self.stop_after = stop_after
        self.nc = bass.Bass("TRN2", target_bir_lowering=False)
        self.uid = 0
        self._bregs = {}

    def breg(self, e, bound):
        if bound not in self._bregs:
            self._bregs[bound] = e.to_reg(bound)
        return self._bregs[bound]

    def sb(self, st, shape, dt, name=None):
        self.uid += 1
        return st.enter_context(self.nc.sbuf_tensor(f"{name or 't'}_{self.uid}", list(shape), dt))

    def din(self, name, shape, dt=F32):
        return self.nc.dram_tensor(name, list(shape), dt, kind="ExternalInput").ap()

    def dscr(self, name, shape, dt):
        kind = "ExternalOutput" if name in self.dbg else "Internal"
        return self.nc.dram_tensor(name, list(shape), dt, kind=kind).ap()

    def psn(self):
        i = self.ps_i % 8
        self.ps_i += 1
        return self.ps[i], self.Tps[i]

    def mm(self, out, lhsT, rhs, start, stop, r, w):
        self.S.op("pe", lambda e: e.matmul(out, lhsT, rhs, start=start, stop=stop, skip_group_check=True), reads=r, writes=w)

    def act(self, out, in_, func, r, w, bias=None, scale=None):
        kw = {}
        if bias is not None:
            kw["bias"] = bias
        if scale is not None:
            kw["scale"] = scale
        self.S.op("act", lambda e: e.activation(out=out, in_=in_, func=func, **kw), reads=r, writes=w)

    def tt(self, eng, out, in0, in1, op, r, w):
        self.S.op(eng, lambda e: e.tensor_tensor(out=out, in0=in0, in1=in1, op=op), reads=r, writes=w)

    def ts(self, eng, out, in0, s1, s2, op0, op1, r, w):
        if s2 is None:
            self.S.op(eng, lambda e: e.tensor_scalar(out=out, in0=in0, scalar1=s1, scalar2=None, op0=op0), reads=r, writes=w)
        else:
            self.S.op(eng, lambda e: e.tensor_scalar(out=out, in0=in0, scalar1=s1, scalar2=s2, op0=op0, op1=op1), reads=r, writes=w)

    def stt(self, out, in0, scalar, in1, op0, op1, r, w):
        self.S.op("dve", lambda e: e.scalar_tensor_tensor(out=out, in0=in0, scalar=scalar, in1=in1, op0=op0, op1=op1), reads=r, writes=w)

    def cp(self, eng, out, in_, r, w):
        if eng == "act":
            self.S.op("act", lambda e: e.activation(out=out, in_=in_, func=AF.Copy), reads=r, writes=w)
        else:
            self.S.op(eng, lambda e: e.tensor_copy(out=out, in_=in_), reads=r, writes=w)

    def recip(self, out, in_, r, w):
        self.S.op("dve", lambda e: e.reciprocal(out=out, in_=in_), reads=r, writes=w)

    def memset(self, eng, ap, val, w):
        self.S.op(eng, lambda e: e.memset(ap, val), writes=w)

    def rstd_from_ps(self, rstd, Trs, ps, Tp, n, inv_n):
        self.act(rstd[:, :n], ps[:, :n], AF.Sqrt, [Tp, self.Tc], [Trs], bias=self.eps_c[:, 0:1], scale=inv_n)
        self.recip(rstd[:, :n], rstd[:, :n], [Trs], [Trs])

    def build(self):
        nc = self.nc
        I = {}
        I["xt"] = self.din("xt", [D, NTOK])
        I["cvec"] = self.din("cvec", [128, 16])
        for l in range(DEPTH):
            I[f"w_mod{l}"] = self.din(f"w_mod{l}", [D, 6 * D])
            I[f"b_mod{l}"] = self.din(f"b_mod{l}", [128, 48])
            I[f"nmw{l}"] = self.din(f"nmw{l}", [128, 8])
            I[f"nfw{l}"] = self.din(f"nfw{l}", [128, 8])
            I[f"w_in{l}"] = self.din(f"w_in{l}", [D, IN_W])
            I[f"wg{l}"] = self.din(f"wg{l}", [16, 512])
            I[f"bg{l}"] = self.din(f"bg{l}", [1, 512])
            I[f"gnw{l}"] = self.din(f"gnw{l}", [128, 1])
            I[f"w_out{l}"] = self.din(f"w_out{l}", [D, D])
        I["ffn_w1"] = self.din("ffn_w1", [D, DFF])
        I["ffn_w3"] = self.din("ffn_w3", [D, DFF])
        I["ffn_w2"] = self.din("ffn_w2", [DFF, D])
        I["router_w"] = self.din("router_w", [D, NEXP])
        I["mw1"] = self.din("mw1", [NEXP * 7 * 128, 8 * 512])
        I["mw3"] = self.din("mw3", [NEXP * 7 * 128, 8 * 512])
        I["mw2"] = self.din("mw2", [NEXP * 7 * 128, 4 * D])
        I["fnw"] = self.din("fnw", [128, 8])
        I["dftc"] = self.din("dftc", [SEQ, SEQ], BF16)
        I["dfts"] = self.din("dfts", [SEQ, SEQ], BF16)
        I["dftc256"] = self.din("dftc256", [CTX, CTX], BF16)
        I["dfts256"] = self.din("dfts256", [CTX, CTX], BF16)
        I["cs128"] = self.din("cs128", [128, 256], BF16)
        I["tri"] = self.din("tri", [128, 512])
        I["mask"] = self.din("mask", [128, 1024], BF16)
        I["ident"] = self.din("ident", [128, 128])
        I["sel"] = self.din("sel", [8, 1024])
        I["su"] = self.din("su", [128, 128])
        I["identb"] = self.din("identb", [128, 128], BF16)
        I["slotstart"] = self.din("slotstart", [128, NSLOT])
        I["gp7"] = self.din("gp7", [128, 7])
        I["ecap"] = self.din("ecap", [128, 8])
        I["jsh"] = self.din("jsh", [128, 256], BF16)
        I["alt"] = self.din("alt", [1, SEQ], BF16)
        self.I = I
        out = nc.dram_tensor("out", [D, SEQ], F32, kind="ExternalOutput").ap()
        self.out = out
        self.XS = self.dscr("XS", [D, NTOK], F32)
        self.U_tm = self.dscr("U_tm", [NTOK, 512], BF16)
        self.QKT = self.dscr("QKT", [512, NTOK], F32)
        self.KV_tm = self.dscr("KV_tm", [NTOK, 768], BF16)
        self.SGT = self.dscr("SGT", [512, NTOK], BF16)
        self.LG = self.dscr("LG", [NTOK, 512], F32)
        self.YT = self.dscr("YT", [D, NTOK], BF16)
        self.OB = self.dscr("OB", [512, NTOK], F32)
        self.XG = self.dscr("XG", [NEXP * CAP, D], BF16)
        self.YS = self.dscr("YS", [NSLOT * TS, D], F32)

        with contextlib.ExitStack() as st:
            self.S = S = Sched(nc, st)
            self.ps = [st.enter_context(nc.psum_tensor(f"ps{i}", [128, 512], F32)) for i in range(8)]
            self.Tps = TL(8)
            self.ps_i = 0
            self.Tc = Tc = T()
            self.ones_bf = self.sb(st, [128, 128], BF16, "ones")
            self.ones_f = self.sb(st, [1, 128], F32, "onesf")
            self.eps_c = self.sb(st, [128, 1], F32, "eps")
            self.ident = self.sb(st, [128, 128], F32, "ident")
            self.modt = [self.sb(st, [128, 96], F32, f"modt{l}") for l in range(DEPTH)]
            self.A1 = [self.sb(st, [128, 16], F32, f"A1{l}") for l in range(DEPTH)]
            self.A2 = [self.sb(st, [128, 16], F32, f"A2{l}") for l in range(DEPTH)]
            self.Tmod = T()
            self.memset("pool", self.ones_bf[:], 1.0, [Tc])
            self.memset("pool", self.ones_f[:], 1.0, [Tc])
            self.memset("pool", self.eps_c[:], EPS, [Tc])
            S.dma("sp", self.ident[:], I["ident"][:, :], writes=[Tc])
            self.mod_init(st)
            with contextlib.ExitStack() as stm:
                wm0 = [self.sb(stm, [128, 8 * 1024], BF16, "wm") for _ in range(2)]
                Twm0 = TL(2)
                for m in range(2):
                    self.mod_block(0, m, wm0[m], Twm0[m], self.Tmod)
            S.barrier()
            pending = [(0, m) for m in range(2, 6)] + [(1, m) for m in range(6)]
            self.Tmod2 = T()
            for l in range(DEPTH):
                last = l == DEPTH - 1
                xsrc = I["xt"] if l == 0 else self.XS
                self.phase_proj(l, xsrc, pending if l == 0 else [])
                S.barrier()
                if self.stop_after == f"proj{l}":
                    break
                self.phase_gla(l)
                S.barrier()
                if self.stop_after == f"gla{l}":
                    break
                self.phase_fourier(l, do_ctx=not last)
                S.barrier()
                if self.stop_after == f"four{l}":
                    break
                self.phase_outproj(l, xsrc, do_ctx=not last)
                S.barrier()
                if self.stop_after == f"outp{l}":
                    break
                if l % 2 == 1:
                    self.phase_moe(l)
                else:
                    self.phase_ffn(l, last)
                S.barrier()
                if self.stop_after == f"ffn{l}":
                    break
            S.barrier()
            S.emit_all()
        return nc

    def mod_init(self, st):
        S, I = self.S, self.I
        cv = self.sb(st, [128, 16], F32)
        self.m_cs = self.sb(st, [128, 16], BF16)
        self.m_bm = [self.sb(st, [128, 48], F32) for _ in range(DEPTH)]
        self.m_nw = {}
        self.m_tmp = self.sb(st, [128, 16], F32)
        self.Tmc, self.Tmtmp = T(), T()
        Tcv = T()
        S.dma("sp", cv[:], I["cvec"][:, :], writes=[Tcv])
        self.act(self.m_cs[:], cv[:], AF.Silu, [Tcv], [self.Tmc])
        for l in range(DEPTH):
            S.dma("sp", self.m_bm[l][:], I[f"b_mod{l}"][:, :], writes=[self.Tmc])
            for nm in (f"nmw{l}", f"nfw{l}"):
                self.m_nw[nm] = self.sb(st, [128, 8], F32)
                S.dma("sp", self.m_nw[nm][:], I[nm][:, :], writes=[self.Tmc])

    def mod_block(self, l, m, wm, Twm, Tm):
        self.mod_load(l, m, wm, Twm)
        self.mod_compute(l, m, wm, Twm, Tm)

    def mod_load(self, l, m, wm, Twm):
        S, I = self.S, self.I
        for hh in range(2):
            S.dma("pool", wm[:, hh * 4096:(hh + 1) * 4096].rearrange("p (k n) -> p k n", k=4),
                  I[f"w_mod{l}"][hh * 512:(hh + 1) * 512, m * 1024:(m + 1) * 1024].rearrange("(k p) n -> p k n", p=128),
                  writes=[Twm])

    def mod_compute(self, l, m, wm, Twm, Tm):
        S, I = self.S, self.I
        p, Tp = self.psn()
        for j in range(8):
            for k in range(8):
                self.mm(p[:, j:j + 9:8], wm[:, k * 1024 + j * 128:k * 1024 + (j + 1) * 128],
                        self.m_cs[:, k:k + 9:8], j == 0 and k == 0, k == 7, [Twm, self.Tmc], [Tp])
        for a in range(2):
            self.tt("dve", self.modt[l][:, a * 48 + m * 8:a * 48 + (m + 1) * 8], p[:, a * 8:(a + 1) * 8],
                    self.m_bm[l][:, m * 8:(m + 1) * 8], ALU.add, [Tp, self.Tmc], [Tm])
        if m in (1, 4):
            nm, dst, off = (f"nmw{l}", self.A1[l], 8) if m == 1 else (f"nfw{l}", self.A2[l], 32)
            tmp = self.m_tmp
            for a in range(2):
                self.ts("dve", tmp[:, a * 8:(a + 1) * 8], self.modt[l][:, a * 48 + off:a * 48 + off + 8], 1.0, None,
                        ALU.add, None, [Tm], [self.Tmtmp])
                self.tt("dve", dst[:, a * 8:(a + 1) * 8], tmp[:, a * 8:(a + 1) * 8], self.m_nw[nm][:], ALU.mult,
                        [self.Tmtmp, self.Tmc], [Tm])

    def norm_mod(self, xt, Txt, n, A, Bv, a, hT, ThT, sq, Tsq, rstd, Trs, tmpb, Ttmp, hF=None, ThF=None, xstride=None):
        xs = xstride or n
        for k in range(8):
            self.act(sq[:, k * n:(k + 1) * n], xt[:, k * xs:k * xs + n], AF.Square, [Txt], [Tsq[k]])
        p, Tp = self.psn()
        for k in range(8):
            self.mm(p[:, :n], self.ones_bf[:], sq[:, k * n:(k + 1) * n], k == 0, k == 7, [Tsq[k], self.Tc], [Tp])
        self.rstd_from_ps(rstd, Trs, p, Tp, n, 1.0 / D)
        for k in range(8):
            tb = k % len(tmpb)
            self.stt(tmpb[tb][:, :n], xt[:, k * xs:k * xs + n], A[:, a * 8 + k:a * 8 + k + 1], rstd[:, :n],
                     ALU.mult, ALU.mult, [Txt, Trs, self.Tmod], [Ttmp[tb]])
            self.act(hT[:, k * n:(k + 1) * n], tmpb[tb][:, :n], AF.Identity, [Ttmp[tb], self.Tmod], [ThT[k]],
                     bias=Bv[:, a * 48 + k:a * 48 + k + 1])
            if hF is not None:
                self.ts("pool", hF[:, k * n:(k + 1) * n], tmpb[tb][:, :n], Bv[:, a * 48 + k:a * 48 + k + 1], None,
                        ALU.add, None, [Ttmp[tb], self.Tmod], [ThF[k]])

    def phase_proj(self, l, xsrc, pending=()):
        S, I = self.S, self.I
        pending = list(pending)
        with contextlib.ExitStack() as st:
            if pending:
                wmp = [self.sb(st, [128, 8 * 1024], BF16, "wmp") for _ in range(2)]
                Twmp = TL(2)
                njob = 0
            win = self.sb(st, [128, 8 * IN_W], BF16, "win")
            Twin2 = [TL(2) for _ in range(8)]
            for hf, (c0, c1) in enumerate(((0, 1040), (1040, 2080))):
                for k in range(8):
                    S.dma("pool", win[:, k * IN_W + c0:k * IN_W + c1], I[f"w_in{l}"][k * 128:(k + 1) * 128, c0:c1], writes=[Twin2[k][hf]])

            def Tw(k, col, w):
                if col + w <= 1040:
                    return [Twin2[k][0]]
                if col >= 1040:
                    return [Twin2[k][1]]
                return Twin2[k]
            if pending:
                self.mod_load(*pending[0], wmp[0], Twmp[0])
                njob = 1
            wg = self.sb(st, [16, 512], F32)
            bg = self.sb(st, [1, 512], F32)
            Twg = T()
            S.dma("sp", wg[:], I[f"wg{l}"][:, :], writes=[Twg])
            S.dma("sp", bg[:], I[f"bg{l}"][:, :], writes=[Twg])
            xt = [self.sb(st, [128, 8 * 512], F32, "xt") for _ in range(2)]
            Txt = TL(2)
            sq = self.sb(st, [128, 8 * 512], BF16, "sq")
            Tsq = TL(8)
            rstd = self.sb(st, [128, 512], F32)
            Trs = T()
            tmpb = [self.sb(st, [128, 512], F32) for _ in range(3)]
            Ttmp = TL(3)
            hT = [self.sb(st, [128, 8 * 512], BF16, "hT") for _ in range(2)]
            ThT = [TL(8) for _ in range(2)]
            u_st = [self.sb(st, [128, 4 * 512], BF16) for _ in range(2)]
            Tu = TL(2)
            qk_st = [self.sb(st, [128, 4 * 512], F32) for _ in range(2)]
            Tqk = TL(2)
            kv_st = [self.sb(st, [128, 4 * 768], BF16) for _ in range(2)]
            Tkv = TL(2)
            sg_st = [self.sb(st, [128, 4 * 512], BF16) for _ in range(2)]
            Tsg = TL(2)
            lfb = [self.sb(st, [16, 2 * 512], F32) for _ in range(2)]
            Tlfb = TL(2)
            lg_st = [self.sb(st, [128, 4 * 512], F32) for _ in range(2)]
            Tlg = TL(2)
            etmp = [self.sb(st, [128, 512], F32) for _ in range(2)]
            Tet = TL(2)
            for ti, (c0, n) in enumerate(TILES):
                a = 1 if ti == 0 else 0
                b = ti % 2
                nb = n // 128
                S.dma("sp", xt[b][:, :8 * n].rearrange("p (k n) -> p k n", k=8),
                      xsrc[:, c0:c0 + n].rearrange("(k p) n -> p k n", p=128), writes=[Txt[b]])
                self.norm_mod(xt[b], Txt[b], n, self.A1[l], self.modt[l], a, hT[b], ThT[b], sq, Tsq, rstd, Trs, tmpb, Ttmp)
                h = hT[b]
                Th = ThT[b]
                for tb in range(nb):
                    p, Tp = self.psn()
                    for k in range(8):
                        self.mm(p[:, :512], h[:, k * n + tb * 128:k * n + (tb + 1) * 128], win[:, k * IN_W:k * IN_W + 512],
                                k == 0, k == 7, [Th[k]] + Tw(k, 0, 512), [Tp])
                    self.cp("dve" if tb % 2 == 0 else "act", u_st[b][:, tb * 512:(tb + 1) * 512], p[:, :512], [Tp], [Tu[b]])
                S.dma("pool", self.U_tm[c0:c0 + n, :].rearrange("(t p) n -> p t n", p=128),
                      u_st[b][:, :nb * 512].rearrange("p (t n) -> p t n", t=nb), reads=[Tu[b]])
                for j in range(4):
                    p, Tp = self.psn()
                    col = 512 + j * 128
                    for k in range(8):
                        self.mm(p[:, :n], win[:, k * IN_W + col:k * IN_W + col + 128], h[:, k * n:(k + 1) * n],
                                k == 0, k == 7, [Th[k]] + Tw(k, col, 128), [Tp])
                    self.cp("act" if j % 2 == 0 else "dve", qk_st[b][:, j * n:(j + 1) * n], p[:, :n], [Tp], [Tqk[b]])
                S.dma("pool", self.QKT[:, c0:c0 + n].rearrange("(j p) n -> p j n", p=128),
                      qk_st[b][:, :4 * n].rearrange("p (j n) -> p j n", j=4), reads=[Tqk[b]])
                for tb in range(nb):
                    p, Tp = self.psn()
                    for k in range(8):
                        self.mm(p[:, :256], h[:, k * n + tb * 128:k * n + (tb + 1) * 128], win[:, k * IN_W + 768:k * IN_W + 1024],
                                k == 0, k == 7, [Th[k]] + Tw(k, 768, 256), [Tp])
                    self.cp("dve", kv_st[b][:, tb * 768:tb * 768 + 256], p[:, :256], [Tp], [Tkv[b]])
                    p, Tp = self.psn()
                    for k in range(8):
                        self.mm(p[:, :512], h[:, k * n + tb * 128:k * n + (tb + 1) * 128], win[:, k * IN_W + 1024:k * IN_W + 1536],
                                k == 0, k == 7, [Th[k]] + Tw(k, 1024, 512), [Tp])
                    self.cp("act", kv_st[b][:, tb * 768 + 256:(tb + 1) * 768], p[:, :512], [Tp], [Tkv[b]])
                S.dma("pool", self.KV_tm[c0:c0 + n, :].rearrange("(t p) n -> p t n", p=128),
                      kv_st[b][:, :nb * 768].rearrange("p (t n) -> p t n", t=nb), reads=[Tkv[b]])
                for j in range(4):
                    p, Tp = self.psn()
                    col = 1536 + j * 128
                    for k in range(8):
                        self.mm(p[:, :n], win[:, k * IN_W + col:k * IN_W + col + 128], h[:, k * n:(k + 1) * n],
                                k == 0, k == 7, [Th[k]] + Tw(k, col, 128), [Tp])
                    self.act(sg_st[b][:, j * n:(j + 1) * n], p[:, :n], AF.Silu, [Tp], [Tsg[b]])
                S.dma("pool", self.SGT[:, c0:c0 + n].rearrange("(j p) n -> p j n", p=128),
                      sg_st[b][:, :4 * n].rearrange("p (j n) -> p j n", j=4), reads=[Tsg[b]])
                for dd in range(2):
                    p, Tp = self.psn()
                    col = 2048 + dd * 16
                    for k in range(8):
                        self.mm(p[0:16, :n], win[:, k * IN_W + col:k * IN_W + col + 16], h[:, k * n:(k + 1) * n],
                                k == 0, k == 7, [Th[k]] + Tw(k, col, 16), [Tp])
                    self.cp("dve", lfb[b][:, dd * 512:dd * 512 + n], p[0:16, :n], [Tp], [Tlfb[b]])
                for tb in range(nb):
                    for dd in range(2):
                        p, Tp = self.psn()
                        self.mm(p[:, :256], lfb[b][:, dd * 512 + tb * 128:dd * 512 + (tb + 1) * 128], wg[:, dd * 256:(dd + 1) * 256],
                                True, False, [Tlfb[b], Twg], [Tp])
                        self.mm(p[:, :256], self.ones_f[:, :], bg[:, dd * 256:(dd + 1) * 256], False, True, [self.Tc, Twg], [Tp])
                        e = (tb * 2 + dd) % 2
                        self.act(etmp[e][:, :256], p[:, :256], AF.Exp, [Tp], [Tet[e]], scale=-1.0)
                        self.act(lg_st[b][:, tb * 512 + dd * 256:tb * 512 + (dd + 1) * 256], etmp[e][:, :256], AF.Ln,
                                 [Tet[e]], [Tlg[b]], bias=1.0)
                S.dma("pool", self.LG[c0:c0 + n, :].rearrange("(t p) n -> p t n", p=128),
                      lg_st[b][:, :nb * 512].rearrange("p (t n) -> p t n", t=nb), reads=[Tlg[b]])
                if pending:
                    if njob > 0:
                        self.mod_compute(*pending[njob - 1], wmp[(njob - 1) % 2], Twmp[(njob - 1) % 2], self.Tmod2)
                    if njob < len(pending):
                        self.mod_load(*pending[njob], wmp[njob % 2], Twmp[njob % 2])
                        njob += 1
            while pending and njob <= len(pending):
                self.mod_compute(*pending[njob - 1], wmp[(njob - 1) % 2], Twmp[(njob - 1) % 2], self.Tmod2)
                if njob < len(pending):
                    self.mod_load(*pending[njob], wmp[njob % 2], Twmp[njob % 2])
                njob += 1

    def phase_gla(self, l):
        S, I = self.S, self.I
        with contextlib.ExitStack() as st:
            tri = self.sb(st, [128, 512], F32, "tri")
            mask = self.sb(st, [128, 1024], BF16, "mask")
            gnw = self.sb(st, [128, 1], F32)
            Tk = T()
            S.dma("sp", tri[:], I["tri"][:, :], writes=[Tk])
            S.dma("sp", mask[:], I["mask"][:, :], writes=[Tk])
            S.dma("sp", gnw[:], I[f"gnw{l}"][:, :], writes=[Tk])
            obuf = self.sb(st, [128, 4 * NTOK], F32, "obuf")
            Tob = TL(NCH)
            S32 = [self.sb(st, [128, 512], F32) for _ in range(2)]
            Sbf = [self.sb(st, [128, 512], BF16) for _ in range(2)]
            TS32 = TL(2)
            TSbf = TL(2)
            for d in range(2):
                self.memset("pool", S32[d][:], 0.0, [TS32[d]])
                self.memset("pool", Sbf[d][:], 0.0, [TSbf[d]])
            NB = 2
            mk2 = lambda W, dt: [self.sb(st, [128, 2 * W], dt) for _ in range(NB)]
            V = lambda t, d, W: t[:, d * W:(d + 1) * W]
            V3 = lambda t: t[:, :].rearrange("p (d w) -> p d w", d=2)
            qk_in, kv_in, lg_in = mk2(512, F32), mk2(768, BF16), mk2(256, F32)
            Tqi, Tki, Tli = [TL(NB) for _ in range(2)], [TL(NB) for _ in range(2)], [TL(NB) for _ in range(2)]
            Eq, Ek, Er = mk2(256, F32), mk2(256, F32), mk2(256, F32)
            TE = [TL(NB) for _ in range(2)]
            qtl, ktl, kht, scm = mk2(512, BF16), mk2(256, BF16), mk2(256, BF16), mk2(512, BF16)
            for bb in range(NB):
                self.memset("pool", qtl[bb][:, :], 0.0, [])
            Tq = [TL(NB) for _ in range(2)]
            Tkh = [TL(NB) for _ in range(2)]
            Tsc = [TL(NB) for _ in range(2)]
            order = [list(range(NCH)), [1, 0] + list(range(NCH - 1, 1, -1))]
            step_of = [{c: i for i, c in enumerate(order[d])} for d in range(2)]
            for i in range(NCH):
                b = i % NB
                cs = [order[0][i], order[1][i]]
                psA, psB, psC, psD = [None] * 2, [None] * 2, [None] * 2, [None] * 2
                for d in range(2):
                    c0 = cs[d] * 128
                    S.dma("sp", V(qk_in[b], d, 512).rearrange("p (j n) -> p j n", j=4),
                          self.QKT[:, c0:c0 + 128].rearrange("(j p) n -> p j n", p=128), writes=[Tqi[d][b]])
                    S.dma("sp", V(kv_in[b], d, 768), self.KV_tm[c0:c0 + 128, :], writes=[Tki[d][b]])
                    S.dma("sp", V(lg_in[b], d, 256), self.LG[c0:c0 + 128, d * 256:(d + 1) * 256], writes=[Tli[d][b]])
                for d in range(2):
                    psA[d] = self.psn()
                    p, Tp = psA[d]
                    lg = V(lg_in[b], d, 256)
                    self.mm(p[:, 0:128], lg[:, 0:128], tri[:, (2 * d) * 128:(2 * d + 1) * 128], True, True, [Tli[d][b], Tk], [Tp])
                    self.mm(p[:, 128:256], lg[:, 128:256], tri[:, (2 * d) * 128:(2 * d + 1) * 128], True, True, [Tli[d][b], Tk], [Tp])
                    self.mm(p[:, 256:512], tri[:, (2 * d + 1) * 128:(2 * d + 2) * 128], lg[:, 0:256], True, True, [Tli[d][b], Tk], [Tp])
                for d in range(2):
                    p, Tp = psA[d]
                    self.act(V(Eq[b], d, 256), p[:, 0:256], AF.Exp, [Tp], [TE[d][b]])
                    self.act(V(Ek[b], d, 256), p[:, 0:256], AF.Exp, [Tp], [TE[d][b]], scale=-1.0)
                    self.act(V(Er[b], d, 256), p[:, 256:512], AF.Exp, [Tp], [TE[d][b]])
                rd = [Tqi[0][b], Tqi[1][b], Tki[0][b], Tki[1][b], TE[0][b], TE[1][b]]
                self.stt(V3(qtl[b])[0:64, :, 0:256], V3(qk_in[b])[0:64, :, 0:256], 0.125, V3(Eq[b])[0:64, :, :], ALU.mult, ALU.mult,
                         rd, [Tq[0][b], Tq[1][b]])
                self.stt(V3(qtl[b])[64:128, :, 256:512], V3(qk_in[b])[64:128, :, 0:256], 0.125, V3(Eq[b])[64:128, :, :], ALU.mult, ALU.mult,
                         rd, [Tq[0][b], Tq[1][b]])
                self.tt("dve", V3(ktl[b])[:, :, :], V3(qk_in[b])[:, :, 256:512], V3(Ek[b])[:, :, :], ALU.mult, rd, [Tq[0][b], Tq[1][b]])
                self.tt("dve", V3(kht[b])[:, :, :], V3(kv_in[b])[:, :, 0:256], V3(Er[b])[:, :, :], ALU.mult, rd, [Tkh[0][b], Tkh[1][b]])
                for d in range(2):
                    psB[d] = self.psn()
                    p, Tp = psB[d]
                    for h in range(4):
                        half = h // 2
                        qo = (h % 2) * 256 + half * 128
                        self.mm(p[:, h * 128:(h + 1) * 128], V(ktl[b], d, 256)[:, half * 128:(half + 1) * 128],
                                V(qtl[b], d, 512)[:, qo:qo + 128], True, True, [Tq[d][b]], [Tp])
                    self.tt("dve", V(scm[b], d, 512), p[:, :], mask[:, d * 512:(d + 1) * 512], ALU.mult, [Tp, Tk], [Tsc[d][b]])
                for d in range(2):
                    psC[d] = self.psn()
                    p, Tp = psC[d]
                    c = cs[d]
                    for h in range(4):
                        half = h // 2
                        self.mm(p[:, h * 128:(h + 1) * 128], V(kv_in[b], d, 768)[:, 256 + h * 128:256 + (h + 1) * 128],
                                V(scm[b], d, 512)[:, h * 128:(h + 1) * 128], True, False, [Tki[d][b], Tsc[d][b]], [Tp])
                        qo = (h % 2) * 256 + half * 128
                        self.mm(p[:, h * 128:(h + 1) * 128], Sbf[d][:, h * 128:(h + 1) * 128],
                                V(qtl[b], d, 512)[:, qo:qo + 128], False, True, [TSbf[d], Tq[d][b]], [Tp])
                    first = step_of[d][c] < step_of[1 - d][c]
                    oview = obuf[:, :].rearrange("p (h n) -> p h n", h=4)[:, :, c * 128:(c + 1) * 128]
                    pview = p[:, :].rearrange("p (h n) -> p h n", h=4)
                    if first:
                        self.cp("act", oview, pview, [Tp], [Tob[c]])
                    else:
                        self.tt("dve", oview, oview, pview, ALU.add, [Tp], [Tob[c]])
                for d in range(2):
                    psD[d] = self.psn()
                    p, Tp = psD[d]
                    tl = 127 if d == 0 else 0
                    for half in range(2):
                        self.mm(p[:, half * 256:(half + 1) * 256], V(kht[b], d, 256)[:, half * 128:(half + 1) * 128],
                                V(kv_in[b], d, 768)[:, 256 + half * 256:256 + (half + 1) * 256], True, True, [Tkh[d][b], Tki[d][b]], [Tp])
                    for half in range(2):
                        self.stt(S32[d][:, half * 256:(half + 1) * 256], S32[d][:, half * 256:(half + 1) * 256],
                                 V(Eq[b], d, 256)[:, half * 128 + tl:half * 128 + tl + 1],
                                 p[:, half * 256:(half + 1) * 256], ALU.mult, ALU.add, [Tp, TE[d][b]], [TS32[d]])
                    self.cp("act", Sbf[d][:, :], S32[d][:, :], [TS32[d]], [TSbf[d]])
            sgt = [self.sb(st, [128, 4 * 512], BF16) for _ in range(2)]
            Tsg = TL(2)
            sq = self.sb(st, [128, 4 * 512], BF16)
            Tsq = TL(4)
            rs = [self.sb(st, [128, 512], F32) for _ in range(2)]
            Trs = TL(2)
            t1 = [self.sb(st, [128, 512], F32) for _ in range(2)]
            Tt1 = TL(2)
            yg = [self.sb(st, [128, 4 * 512], BF16) for _ in range(2)]
            Tyg = TL(2)
            if "OB" in self.dbg:
                S.dma("pool", self.OB[:, :].rearrange("(h p) n -> p h n", p=128),
                      obuf[:, :].rearrange("p (h n) -> p h n", h=4), reads=Tob)
            for ti, (c0, n) in enumerate(TILES):
                b = ti % 2
                Tobs = [Tob[c] for c in range(c0 // 128, (c0 + n) // 128)]
                S.dma("sp", sgt[b][:, :4 * n].rearrange("p (j n) -> p j n", j=4),
                      self.SGT[:, c0:c0 + n].rearrange("(j p) n -> p j n", p=128), writes=[Tsg[b]])
                for h in range(4):
                    self.act(sq[:, h * n:(h + 1) * n], obuf[:, h * NTOK + c0:h * NTOK + c0 + n], AF.Square, Tobs, [Tsq[h]])
                for h in range(4):
                    p, Tp = self.psn()
                    self.mm(p[:, :n], self.ones_bf[:], sq[:, h * n:(h + 1) * n], True, True, [Tsq[h], self.Tc], [Tp])
                    e = h % 2
                    self.rstd_from_ps(rs[e], Trs[e], p, Tp, n, 1.0 / 128)
                    self.stt(t1[e][:, :n], obuf[:, h * NTOK + c0:h * NTOK + c0 + n], gnw[:, 0:1], rs[e][:, :n], ALU.mult, ALU.mult,
                             Tobs + [Trs[e], Tk], [Tt1[e]])
                    self.tt("pool", yg[b][:, h * n:(h + 1) * n], t1[e][:, :n], sgt[b][:, h * n:(h + 1) * n], ALU.mult,
                            [Tt1[e], Tsg[b]], [Tyg[b]])
                S.dma("pool", self.YT[512:1024, c0:c0 + n].rearrange("(j p) n -> p j n", p=128),
                      yg[b][:, :4 * n].rearrange("p (j n) -> p j n", j=4), reads=[Tyg[b]])

    def phase_fourier(self, l, do_ctx):
        S, I = self.S, self.I
        with contextlib.ExitStack() as st:
            U = self.sb(st, [128, NCH * 512], BF16, "U")
            TU = T()
            for g0 in range(0, NCH, 17):
                S.dma("sp", U[:, g0 * 512:(g0 + 17) * 512].rearrange("p (t n) -> p t n", t=17),
                      self.U_tm[g0 * 128:(g0 + 17) * 128, :].rearrange("(t p) n -> p t n", p=128), writes=[TU])
            cs128 = self.sb(st, [128, 256], BF16)
            jsh = self.sb(st, [128, 256], BF16)
            alt = self.sb(st, [1, SEQ], BF16)
            Tcs = T()
            S.dma("sp", cs128[:], I["cs128"][:, :], writes=[Tcs])
            S.dma("sp", jsh[:], I["jsh"][:, :], writes=[Tcs])
            S.dma("sp", alt[:], I["alt"][:, :], writes=[Tcs])
            NW = 256
            HC = 16
            cl = [self.sb(st, [128, HC * NW], BF16, "cl") for _ in range(2)]
            sl = [self.sb(st, [128, HC * NW], BF16, "sl") for _ in range(2)]
            Tcl = TL(2)
            z = self.sb(st, [128, 8 * NW], BF16)
            Tz = TL(4)
            yf = [self.sb(st, [128, 4 * NW], BF16) for _ in range(2)]
            Tyf = TL(2)
            UE = self.sb(st, [128, HC * 512], BF16, "UE")
            UO = self.sb(st, [128, HC * 512], BF16, "UO")
            TUE = TL(HC)
            for tc in range(HC):
                p, Tp = self.psn()
                self.mm(p[:, :512], jsh[:, 0:128], U[:, (2 + 31 - tc) * 512:(2 + 32 - tc) * 512], True, tc == 0, [TU, Tcs], [Tp])
                if tc > 0:
                    self.mm(p[:, :512], jsh[:, 128:256], U[:, (2 + 32 - tc) * 512:(2 + 33 - tc) * 512], False, True, [TU, Tcs], [Tp])
                self.tt("dve", UE[:, tc * 512:(tc + 1) * 512], U[:, (2 + tc) * 512:(3 + tc) * 512], p[:, :512], ALU.add, [TU, Tp], [TUE[tc]])
                self.tt("dve", UO[:, tc * 512:(tc + 1) * 512], U[:, (2 + tc) * 512:(3 + tc) * 512], p[:, :512], ALU.subtract, [TU, Tp], [TUE[tc]])
            jobs = []
            if do_ctx:
                jobs.append(("c", 0))
            for nt in range(SEQ // NW):
                jobs.append(("x", nt))
            for ji, (kind, nt) in enumerate(jobs):
                b = ji % 2
                banks = [self.psn() for _ in range(4)]
                if kind == "c":
                    col0 = 0
                    S.dma("sp", cl[b][:, :2 * NW].rearrange("p (t n) -> p t n", t=2),
                          I["dftc256"][:, :].rearrange("(t p) n -> p t n", p=128), writes=[Tcl[b]])
                    S.dma("sp", sl[b][:, :2 * NW].rearrange("p (t n) -> p t n", t=2),
                          I["dfts256"][:, :].rearrange("(t p) n -> p t n", p=128), writes=[Tcl[b]])
                    for tc in range(2):
                        for g in range(4):
                            p, Tp = banks[g]
                            lhs = U[:, tc * 512 + g * 128:tc * 512 + (g + 1) * 128]
                            self.mm(p[:, 0:NW], lhs, cl[b][:, tc * NW:(tc + 1) * NW], tc == 0, tc == 1, [TU, Tcl[b]], [Tp])
                            self.mm(p[:, NW:2 * NW], lhs, sl[b][:, tc * NW:(tc + 1) * NW], False, tc == 1, [TU, Tcl[b]], [Tp])
                else:
                    col0 = CTX + nt * NW
                    S.dma("sp", cl[b][:, :].rearrange("p (t n) -> p t n", t=HC),
                          I["dftc"][0:HC * 128, nt * NW:(nt + 1) * NW].rearrange("(t p) n -> p t n", p=128), writes=[Tcl[b]])
                    S.dma("sp", sl[b][:, :].rearrange("p (t n) -> p t n", t=HC),
                          I["dfts"][0:HC * 128, nt * NW:(nt + 1) * NW].rearrange("(t p) n -> p t n", p=128), writes=[Tcl[b]])
                    for tc in range(HC):
                        for g in range(4):
                            p, Tp = banks[g]
                            self.mm(p[:, 0:NW], UE[:, tc * 512 + g * 128:tc * 512 + (g + 1) * 128], cl[b][:, tc * NW:(tc + 1) * NW],
                                    tc == 0, False, [TUE[tc], Tcl[b]], [Tp])
                            self.mm(p[:, NW:2 * NW], UO[:, tc * 512 + g * 128:tc * 512 + (g + 1) * 128], sl[b][:, tc * NW:(tc + 1) * NW],
                                    False, tc == HC - 1, [TUE[tc], Tcl[b]], [Tp])
                    for g in range(4):
                        p, Tp = banks[g]
                        self.mm(p[:, 0:NW], U[0:1, (2 + HC) * 512 + g * 128:(2 + HC) * 512 + (g + 1) * 128], alt[0:1, nt * NW:(nt + 1) * NW],
                                False, True, [TU, Tcs], [Tp])
                for g in range(4):
                    p, Tp = banks[g]
                    self.cp("act" if g % 2 == 0 else "dve", z[:, g * 2 * NW:(g + 1) * 2 * NW], p[:, :2 * NW], [Tp], [Tz[g]])
                pa, Tpa = self.psn()
                pb, Tpb = self.psn()
                for g in range(4):
                    p, Tp = (pa, Tpa) if g < 2 else (pb, Tpb)
                    o = (g % 2) * NW
                    self.mm(p[:, o:o + NW], cs128[:, 0:128], z[:, g * 2 * NW:g * 2 * NW + NW], g % 2 == 0, False, [Tz[g], Tcs], [Tp])
                    self.mm(p[:, o:o + NW], cs128[:, 128:256], z[:, g * 2 * NW + NW:(g + 1) * 2 * NW], False, True, [Tz[g], Tcs], [Tp])
                self.cp("act", yf[b][:, 0:2 * NW], pa[:, :2 * NW], [Tpa], [Tyf[b]])
                self.cp("dve", yf[b][:, 2 * NW:4 * NW], pb[:, :2 * NW], [Tpb], [Tyf[b]])
                S.dma("pool", self.YT[0:512, col0:col0 + NW].rearrange("(j p) n -> p j n", p=128),
                      yf[b][:, :].rearrange("p (j n) -> p j n", j=4), reads=[Tyf[b]])

    def phase_outproj(self, l, xsrc, do_ctx):
        S, I = self.S, self.I
        with contextlib.ExitStack() as st:
            wo = self.sb(st, [128, 8 * D], BF16, "wo")
            Two = T()
            for hh in range(2):
                S.dma("pool", wo[:, hh * 4096:(hh + 1) * 4096].rearrange("p (k n) -> p k n", k=4),
                      I[f"w_out{l}"][hh * 512:(hh + 1) * 512, :].rearrange("(k p) n -> p k n", p=128), writes=[Two])
            yc = [self.sb(st, [128, 8 * 512], BF16) for _ in range(2)]
            Tyc = TL(2)
            xt = [self.sb(st, [128, 8 * 512], F32) for _ in range(2)]
            Txt = TL(2)
            tiles = TILES if do_ctx else TILES[1:]
            for ti, (c0, n) in enumerate(tiles):
                a = 1 if c0 == 0 else 0
                b = ti % 2
                S.dma("sp", yc[b][:, :8 * n].rearrange("p (k n) -> p k n", k=8),
                      self.YT[:, c0:c0 + n].rearrange("(k p) n -> p k n", p=128), writes=[Tyc[b]])
                S.dma("sp", xt[b][:, :8 * n].rearrange("p (k n) -> p k n", k=8),
                      xsrc[:, c0:c0 + n].rearrange("(k p) n -> p k n", p=128), writes=[Txt[b]])
                for i in range(8):
                    p, Tp = self.psn()
                    for k in range(8):
                        self.mm(p[:, :n], wo[:, k * D + i * 128:k * D + (i + 1) * 128], yc[b][:, k * n:(k + 1) * n],
                                k == 0, k == 7, [Two, Tyc[b]], [Tp])
                    self.stt(xt[b][:, i * n:(i + 1) * n], p[:, :n], self.modt[l][:, a * 48 + 16 + i:a * 48 + 17 + i],
                             xt[b][:, i * n:(i + 1) * n], ALU.mult, ALU.add, [Tp, self.Tmod], [Txt[b]])
                S.dma("pool", self.XS[:, c0:c0 + n].rearrange("(k p) n -> p k n", p=128),
                      xt[b][:, :8 * n].rearrange("p (k n) -> p k n", k=8), reads=[Txt[b]])


    def phase_moe(self, l):
        S, I = self.S, self.I
        NBLK = SEQ // 128
        a = 0
        NG = EFF // 512
        G = 4
        with contextlib.ExitStack() as st0:
            rw = self.sb(st0, [128, 8 * NEXP], F32)
            su = self.sb(st0, [128, 128], F32)
            ones128 = self.sb(st0, [128, 128], F32)
            identb = self.sb(st0, [128, 128], BF16)
            slotstart = self.sb(st0, [128, NSLOT], F32)
            gp7 = self.sb(st0, [128, NG], F32)
            fnw = self.sb(st0, [128, 8], F32)
            Tf = T()
            S.dma("sp", rw[:, :].rearrange("p (k e) -> p k e", k=8), I["router_w"].rearrange("(k p) e -> p k e", p=128), writes=[Tf])
            S.dma("sp", su[:], I["su"][:, :], writes=[Tf])
            S.dma("sp", identb[:], I["identb"][:, :], writes=[Tf])
            S.dma("sp", slotstart[:], I["slotstart"][:, :], writes=[Tf])
            S.dma("sp", gp7[:], I["gp7"][:, :], writes=[Tf])
            S.dma("sp", fnw[:], I["fnw"][:, :], writes=[Tf])
            self.memset("pool", ones128[:], 1.0, [Tf])
            IND = [self.sb(st0, [128, NBLK * 8], F32) for _ in range(2)]
            TOPA = self.sb(st0, [128, NBLK * 8], F32)
            R8A = self.sb(st0, [128, NBLK * 8], F32)
            BP = self.sb(st0, [128, (NBLK + 1) * 8], F32)
            ecap = self.sb(st0, [128, 8], F32)
            dsi = self.sb(st0, [128, 2 * NBLK], I32)
            Tds = TL(NBLK)
            TR8 = TL(NBLK)
            xidx = self.sb(st0, [128, 4 * NSLOT], I32)
            Txi = T()
            GK = [self.sb(st0, [128, NBLK], F32) for _ in range(2)]
            TI = TL(NBLK)
            TBP, TGK = T(), T()
            S.dma("sp", ecap[:], I["ecap"][:, :], writes=[Tf])
            S.dma("sp", BP[:, 0:8], I["ecap"][:, :], writes=[TBP])
            desti = self.sb(st0, [128, 2 * NBLK], I32)
            Tdd = T()
            widx = self.sb(st0, [128, NG * NSLOT], I32)
            Twi = T()
            TXG = T()
            with contextlib.ExitStack() as st:
                h2tm = self.sb(st, [128, NBLK * D], BF16, "h2tm")
                Th2tm = TL(NBLK)
                xt = [self.sb(st, [128, 8 * 512], F32, "xt") for _ in range(2)]
                Txt = TL(2)
                sq = self.sb(st, [128, 8 * 512], BF16)
                Tsq = TL(8)
                rstd = self.sb(st, [128, 512], F32)
                Trs = T()
                tmpb = [self.sb(st, [128, 512], F32) for _ in range(2)]
                Ttmp = TL(2)
                hFk = [self.sb(st, [128, 512], F32) for _ in range(2)]
                ThF = TL(2)
                h2T = [self.sb(st, [128, 8 * 512], BF16) for _ in range(2)]
                Th2T = [TL(8) for _ in range(2)]
                ind = [self.sb(st, [128, 8], F32) for _ in range(4)]
                Tind = TL(4)
                t8 = [[self.sb(st, [128, 8], F32) for _ in range(2)] for _ in range(4)]
                dsf = [self.sb(st, [128, 2], F32) for _ in range(4)]
                Tt8 = TL(4)
                for ti, (c0, n) in enumerate(TILES[1:]):
                    b = ti % 2
                    S.dma("sp", xt[b][:, :].rearrange("p (k n) -> p k n", k=8),
                          self.XS[:, c0:c0 + n].rearrange("(k p) n -> p k n", p=128), writes=[Txt[b]])
                    for k in range(8):
                        self.act(sq[:, k * n:(k + 1) * n], xt[b][:, k * n:(k + 1) * n], AF.Square, [Txt[b]], [Tsq[k]])
                    p, Tp = self.psn()
                    for k in range(8):
                        self.mm(p[:, :n], self.ones_bf[:], sq[:, k * n:(k + 1) * n], k == 0, k == 7, [Tsq[k], self.Tc], [Tp])
                    self.rstd_from_ps(rstd, Trs, p, Tp, n, 1.0 / D)
                    pl, Tpl = self.psn()
                    for k in range(8):
                        tb_ = k % 2
                        self.stt(tmpb[tb_][:, :n], xt[b][:, k * n:(k + 1) * n], self.A2[l][:, a * 8 + k:a * 8 + k + 1], rstd[:, :n],
                                 ALU.mult, ALU.mult, [Txt[b], Trs, self.Tmod], [Ttmp[tb_]])
                        self.act(h2T[b][:, k * n:(k + 1) * n], tmpb[tb_][:, :n], AF.Identity, [Ttmp[tb_], self.Tmod], [Th2T[b][k]],
                                 bias=self.modt[l][:, a * 48 + 24 + k:a * 48 + 25 + k])
                        self.act(hFk[tb_][:, :n], tmpb[tb_][:, :n], AF.Identity, [Ttmp[tb_], self.Tmod], [ThF[tb_]],
                                 bias=self.modt[l][:, a * 48 + 24 + k:a * 48 + 25 + k])
                        for tb in range(4):
                            self.mm(pl[:, tb * 8:(tb + 1) * 8], hFk[tb_][:, tb * 128:(tb + 1) * 128], rw[:, k * 8:(k + 1) * 8],
                                    k == 0 and tb == 0, k == 7, [ThF[tb_], Tf], [Tpl])
                    for tb in range(4):
                        blk = ti * 4 + tb
                        pt, Tpt = self.psn()
                        ptb = pt[:, :].bitcast(BF16)
                        for k in range(8):
                            S.op("pe", lambda e, ptb=ptb, k=k, src=h2T[b][:, k * n + tb * 128:k * n + (tb + 1) * 128]:
                                 e.transpose(ptb[:, k * 128:(k + 1) * 128], src, identb[:, :]), reads=[Th2T[b][k], Tf], writes=[Tpt])
                        self.cp("act", h2tm[:, blk * D:(blk + 1) * D], ptb[:, 0:D], [Tpt], [Th2tm[blk]])
                        plb = pl[:, tb * 8:(tb + 1) * 8]
                        top8 = TOPA[:, blk * 8:(blk + 1) * 8]
                        i1 = IND[0][:, blk * 8:(blk + 1) * 8]
                        i2 = IND[1][:, blk * 8:(blk + 1) * 8]
                        S.op("dve", lambda e, top8=top8, plb=plb: e.max(out=top8, in_=plb), reads=[Tpl], writes=[TI[blk]])
                        self.ts("dve", i1, plb, top8[:, 0:1], None, ALU.is_equal, None, [Tpl, TI[blk]], [TI[blk]])
                        self.ts("dve", i2, plb, top8[:, 1:2], None, ALU.is_equal, None, [Tpl, TI[blk]], [TI[blk]])
                        self.tt("dve", ind[tb][:, :], i1, i2, ALU.add, [TI[blk]], [Tind[tb]])
                        pp, Tpp = self.psn()
                        self.mm(pp[:, 0:8], su[:, :], ind[tb][:, :], True, True, [Tind[tb], Tf], [Tpp])
                        self.mm(pp[:, 8:16], ones128[:, :], ind[tb][:, :], False, True, [Tind[tb], Tf], [Tpp])
                        r8 = R8A[:, blk * 8:(blk + 1) * 8]
                        self.tt("dve", r8, BP[:, blk * 8:(blk + 1) * 8], pp[:, 0:8], ALU.add, [Tpp, TBP], [TR8[blk]])
                        self.tt("dve", BP[:, (blk + 1) * 8:(blk + 2) * 8], BP[:, blk * 8:(blk + 1) * 8], pp[:, 8:16], ALU.add, [Tpp, TBP], [TBP])
                        for kk in range(2):
                            self.tt("dve", t8[tb][kk][:, :], IND[kk][:, blk * 8:(blk + 1) * 8], r8, ALU.mult, [TI[blk], TR8[blk]], [Tt8[tb]])
                            S.op("dve", lambda e, tb=tb, kk=kk: e.reduce_sum(out=dsf[tb][:, kk:kk + 1], in_=t8[tb][kk][:, :], axis=mybir.AxisListType.X),
                                 reads=[Tt8[tb]], writes=[Tt8[tb]])
                        self.cp("dve", dsi[:, blk * 2:blk * 2 + 2], dsf[tb][:, 0:2], [Tt8[tb]], [Tds[blk]])
                        for kk in range(2):
                            def sc1(e, q=blk * 2 + kk, blk=blk):
                                return e.indirect_dma_start(out=self.XG[:, :], out_offset=bass.IndirectOffsetOnAxis(ap=dsi[:, q:q + 1], axis=0),
                                                            in_=h2tm[:, blk * D:(blk + 1) * D], in_offset=None,
                                                            bounds_check=self.breg(e, NEXP * CAP - 1), oob_is_err=False)
                            S.dma_custom("pool", sc1, reads=[Tds[blk], Th2tm[blk]], writes=[T()])
                run = self.sb(st, [128, 8], F32)
                self.tt("dve", run[:, :], BP[:, NBLK * 8:(NBLK + 1) * 8], ecap[:, :], ALU.subtract, [TBP, Tf], [TBP])
                gd = self.sb(st, [128, NBLK], F32)
                Tg = T()
                self.tt("dve", gd[:, :], TOPA[:, 1:NBLK * 8:8], TOPA[:, 0:NBLK * 8:8], ALU.subtract, TI, [Tg])
                self.act(gd[:, :], gd[:, :], AF.Exp, [Tg], [Tg])
                self.ts("dve", gd[:, :], gd[:, :], 1.0, None, ALU.add, None, [Tg], [Tg])
                self.recip(GK[0][:, :], gd[:, :], [Tg], [TGK])
                self.ts("dve", GK[1][:, :], GK[0][:, :], -1.0, 1.0, ALU.mult, ALU.add, [TGK], [TGK])
                destf = self.sb(st, [128, 2 * NBLK], F32)
                Tm = [self.sb(st, [128, NBLK * 8], F32) for _ in range(2)]
                r32 = [self.sb(st, [128, NBLK], F32) for _ in range(2)]
                Tdf = TL(2)
                for kk in range(2):
                    dk = destf[:, kk * NBLK:(kk + 1) * NBLK]
                    self.tt("dve", Tm[kk][:, :], IND[kk][:, :], R8A[:, :], ALU.mult, TI + TR8, [Tdf[kk]])
                    S.op("dve", lambda e, kk=kk, dk=dk: e.reduce_sum(out=dk, in_=Tm[kk][:, :].rearrange("p (b e) -> p b e", e=8),
                                                                      axis=mybir.AxisListType.X), reads=[Tdf[kk]], writes=[Tdf[kk]])
                pad = self.sb(st, [128, 8], F32)
                base = self.sb(st, [128, 8], F32)
                ends = self.sb(st, [128, 8], F32)
                esf = self.sb(st, [128, NSLOT], F32)
                est = [self.sb(st, [128, NSLOT], F32) for _ in range(2)]
                widf = self.sb(st, [128, NG * NSLOT], F32)
                Tb = T()
                for e_ in range(8):
                    self.ts("dve", est[e_ % 2][:, :], slotstart[:, :], run[:, e_:e_ + 1], None, ALU.is_lt, None, [TBP, Tf, Tb], [Tb])
                    S.op("dve", lambda e, e_=e_: e.reduce_sum(out=pad[:, e_:e_ + 1], in_=est[e_ % 2][:, :], axis=mybir.AxisListType.X),
                         reads=[Tb], writes=[Tb])
                self.ts("dve", pad[:, :], pad[:, :], float(TS), None, ALU.mult, None, [Tb], [Tb])
                self.memset("dve", base[:, 0:1], 0.0, [Tb])
                for e_ in range(1, 8):
                    self.tt("dve", base[:, e_:e_ + 1], base[:, e_ - 1:e_], pad[:, e_ - 1:e_], ALU.add, [Tb], [Tb])
                self.tt("dve", ends[:, :], base[:, :], pad[:, :], ALU.add, [Tb], [Tb])
                bos = self.sb(st, [128, NSLOT], F32)
                bm = self.sb(st, [128, 8], F32)
                self.tt("dve", bm[:, :], base[:, :], ecap[:, :], ALU.subtract, [Tb, Tf], [Tb])
                self.memset("dve", esf[:, :], 0.0, [Tb])
                self.memset("dve", bos[:, :], 0.0, [Tb])
                for e_ in range(8):
                    self.ts("dve", est[e_ % 2][:, :], slotstart[:, :], ends[:, e_:e_ + 1], None, ALU.is_ge, None, [Tb, Tf], [Tb])
                    self.tt("dve", esf[:, :], esf[:, :], est[e_ % 2][:, :], ALU.add, [Tb], [Tb])
                    self.stt(bos[:, :], est[e_ % 2][:, :], pad[:, e_:e_ + 1], bos[:, :], ALU.mult, ALU.add, [Tb], [Tb])
                self.ts("dve", esf[:, :], esf[:, :], 7.0, None, ALU.min, None, [Tb], [Tb])
                for g in range(NG):
                    self.ts("dve", widf[:, g * NSLOT:(g + 1) * NSLOT], esf[:, :], float(NG * 128), gp7[:, g:g + 1], ALU.mult, ALU.add,
                            [Tb, Tf], [Tb])
                self.cp("dve", widx[:, :], widf[:, :], [Tb], [Twi])
                xoff = self.sb(st, [128, NSLOT], F32)
                xidf = self.sb(st, [128, 4 * NSLOT], F32)
                self.stt(xoff[:, :], esf[:, :], float(CAP), slotstart[:, :], ALU.mult, ALU.add, [Tb, Tf], [Tb])
                self.tt("dve", xoff[:, :], xoff[:, :], bos[:, :], ALU.subtract, [Tb], [Tb])
                for t_ in range(4):
                    self.ts("dve", xidf[:, t_ * NSLOT:(t_ + 1) * NSLOT], xoff[:, :], gp7[:, t_:t_ + 1], None, ALU.add, None, [Tb, Tf], [Tb])
                self.cp("dve", xidx[:, :], xidf[:, :], [Tb], [Txi])
                for e_ in range(8):
                    for kk in range(2):
                        dk = destf[:, kk * NBLK:(kk + 1) * NBLK]
                        self.stt(dk, IND[kk][:, e_:NBLK * 8:8], bm[:, e_:e_ + 1], dk, ALU.mult, ALU.add, TI + [Tb, Tdf[kk]], [Tdf[kk]])
                self.cp("dve", desti[:, :], destf[:, :], Tdf, [Tdd])
            S.barrier()
            with contextlib.ExitStack() as st:
                NWB = 3
                w13 = [self.sb(st, [128, 2 * 8 * 512], BF16, "w13") for _ in range(NWB)]
                w2 = [self.sb(st, [128, G * D], BF16, "w2") for _ in range(NWB)]
                Tw = [TL(3) for _ in range(NWB)]
                xg_tm = [self.sb(st, [128, 4 * D], BF16, "xgtm") for _ in range(2)]
                Txg = [TL(4) for _ in range(2)]
                xgT = [self.sb(st, [128, 8 * TS], BF16, "xgT") for _ in range(2)]
                TxgT = TL(2)
                actb = [self.sb(st, [128, G * TS], BF16, "actb") for _ in range(2)]
                Tab = [TL(G) for _ in range(2)]
                sil = [self.sb(st, [128, TS], F32) for _ in range(2)]
                Tsil = TL(2)
                acc = [self.sb(st, [128, 4 * D], F32, "acctm") for _ in range(2)]
                Tacc = [TL(4) for _ in range(2)]
                jobs = [(s_, gi) for s_ in range(NSLOT) for gi in range(NG)]
                NROW = NEXP * NG * 128

                def issue_w(ji):
                    s_, gi = jobs[ji]
                    wb = ji % NWB
                    ixap = widx[:, gi * NSLOT + s_:gi * NSLOT + s_ + 1]
                    for wi, (wname, dst) in enumerate((("mw1", w13[wb][:, 0:4096]), ("mw3", w13[wb][:, 4096:8192]), ("mw2", w2[wb][:, :]))):
                        def f(e, wname=wname, dst=dst, ixap=ixap):
                            return e.indirect_dma_start(out=dst, out_offset=None, in_=I[wname][:, :],
                                                        in_offset=bass.IndirectOffsetOnAxis(ap=ixap, axis=0),
                                                        bounds_check=self.breg(e, NROW - 1), oob_is_err=False)
                        S.dma_custom("pool", f, reads=[Twi], writes=[Tw[wb][wi]])

                def issue_x(s_):
                    xb = s_ % 2
                    for t_ in range(4):
                        def gx(e, s_=s_, t_=t_, dst=xg_tm[xb][:, t_ * D:(t_ + 1) * D]):
                            return e.indirect_dma_start(out=dst, out_offset=None, in_=self.XG[:, :],
                                                        in_offset=bass.IndirectOffsetOnAxis(ap=xidx[:, t_ * NSLOT + s_:t_ * NSLOT + s_ + 1], axis=0),
                                                        bounds_check=self.breg(e, NEXP * CAP - 1), oob_is_err=False)
                        S.dma_custom("pool", gx, reads=[Txi], writes=[Txg[xb][t_]])

                issue_w(0)
                issue_w(1)
                issue_x(0)
                TY = T()
                for ji, (s_, gi) in enumerate(jobs):
                    wb = ji % NWB
                    xb = s_ % 2
                    if ji + 2 < len(jobs):
                        issue_w(ji + 2)
                    if gi == 0:
                        if s_ + 1 < NSLOT:
                            issue_x(s_ + 1)
                        for tb in range(4):
                            pt, Tpt = self.psn()
                            ptb = pt[:, :].bitcast(BF16)
                            for k in range(8):
                                S.op("pe", lambda e, ptb=ptb, k=k, src=xg_tm[xb][:, tb * D + k * 128:tb * D + (k + 1) * 128]:
                                     e.transpose(ptb[:, k * 128:(k + 1) * 128], src, identb[:, :]), reads=[Txg[xb][tb], Tf], writes=[Tpt])
                            self.cp("act" if tb % 2 == 0 else "dve",
                                    xgT[xb][:, :].rearrange("p (k n) -> p k n", k=8)[:, :, tb * 128:(tb + 1) * 128],
                                    ptb[:, 0:D].rearrange("p (k n) -> p k n", k=8), [Tpt], [TxgT[xb]])
                    ab = ji % 2
                    for j in range(G):
                        pa, Tpa = self.psn()
                        pb, Tpb = self.psn()
                        for k in range(8):
                            self.mm(pa[:, :TS], w13[wb][:, k * 512 + j * 128:k * 512 + (j + 1) * 128], xgT[xb][:, k * TS:(k + 1) * TS],
                                    k == 0, k == 7, [Tw[wb][0], TxgT[xb]], [Tpa])
                        for k in range(8):
                            self.mm(pb[:, :TS], w13[wb][:, 4096 + k * 512 + j * 128:4096 + k * 512 + (j + 1) * 128], xgT[xb][:, k * TS:(k + 1) * TS],
                                    k == 0, k == 7, [Tw[wb][1], TxgT[xb]], [Tpb])
                        sb_ = j % 2
                        self.act(sil[sb_][:, :], pa[:, :TS], AF.Silu, [Tpa], [Tsil[sb_]])
                        self.tt("dve", actb[ab][:, j * TS:(j + 1) * TS], sil[sb_][:, :], pb[:, :TS], ALU.mult, [Tpb, Tsil[sb_]], [Tab[ab][j]])
                    for tb in range(4):
                        for hf in range(2):
                            po, Tpo = self.psn()
                            for j in range(G):
                                self.mm(po[:, :512], actb[ab][:, j * TS + tb * 128:j * TS + (tb + 1) * 128],
                                        w2[wb][:, j * D + hf * 512:j * D + (hf + 1) * 512], j == 0, j == G - 1, [Tw[wb][2], Tab[ab][j]], [Tpo])
                            dst = acc[xb][:, tb * D + hf * 512:tb * D + (hf + 1) * 512]
                            if gi == 0:
                                self.cp("act", dst, po[:, :512], [Tpo], [Tacc[xb][tb]])
                            else:
                                self.tt("dve", dst, dst, po[:, :512], ALU.add, [Tpo], [Tacc[xb][tb]])
                    if gi == NG - 1:
                        S.dma("sp", self.YS[s_ * TS:(s_ + 1) * TS, :].rearrange("(t p) n -> p t n", p=128),
                              acc[xb][:, :].rearrange("p (t n) -> p t n", t=4), reads=Tacc[xb], writes=[TY])
            S.barrier()
            with contextlib.ExitStack() as st:
                ym = [self.sb(st, [128, 4 * 2 * D], F32, "ym") for _ in range(2)]
                Tym = [[TL(2) for _ in range(4)] for _ in range(2)]
                ysum = [self.sb(st, [128, 4 * D], F32, "ysum") for _ in range(2)]
                Tys = [TL(4) for _ in range(2)]
                xt = [self.sb(st, [128, 8 * 512], F32, "xt") for _ in range(2)]
                Txt = [TL(8) for _ in range(2)]
                sq = self.sb(st, [128, 8 * 512], BF16)
                Tsq = TL(8)
                rstd = self.sb(st, [128, 512], F32)
                Trs = T()
                for ti, (c0, n) in enumerate(TILES[1:]):
                    b = ti % 2
                    t0 = c0 - CTX
                    for k in range(8):
                        S.dma("sp", xt[b][:, k * n:(k + 1) * n], self.XS[k * 128:(k + 1) * 128, c0:c0 + n], writes=[Txt[b][k]])
                    for tb in range(4):
                        blk = ti * 4 + tb
                        for kk in range(2):
                            def gy(e, q=kk * NBLK + blk, dst=ym[b][:, (tb * 2 + kk) * D:(tb * 2 + kk + 1) * D]):
                                return e.indirect_dma_start(out=dst, out_offset=None, in_=self.YS[:, :],
                                                            in_offset=bass.IndirectOffsetOnAxis(ap=desti[:, q:q + 1], axis=0),
                                                            bounds_check=self.breg(e, NSLOT * TS - 1), oob_is_err=False)
                            S.dma_custom("pool", gy, reads=[Tdd], writes=[Tym[b][tb][kk]])
                        self.act(ysum[b][:, tb * D:(tb + 1) * D], ym[b][:, tb * 2 * D:tb * 2 * D + D], AF.Copy, [Tym[b][tb][0], TGK], [Tys[b][tb]],
                                 scale=GK[0][:, blk:blk + 1])
                        self.stt(ysum[b][:, tb * D:(tb + 1) * D], ym[b][:, tb * 2 * D + D:(tb + 1) * 2 * D], GK[1][:, blk:blk + 1],
                                 ysum[b][:, tb * D:(tb + 1) * D], ALU.mult, ALU.add, [Tym[b][tb][1], TGK], [Tys[b][tb]])
                    for k in range(8):
                        p, Tp = self.psn()
                        for tb in range(4):
                            S.op("pe", lambda e, p=p, tb=tb, src=ysum[b][:, tb * D + k * 128:tb * D + (k + 1) * 128]:
                                 e.transpose(p[:, tb * 128:(tb + 1) * 128], src, self.ident[:, :]), reads=[Tys[b][tb], self.Tc], writes=[Tp])
                        self.stt(xt[b][:, k * n:(k + 1) * n], p[:, :n], self.modt[l][:, a * 48 + 40 + k:a * 48 + 41 + k],
                                 xt[b][:, k * n:(k + 1) * n], ALU.mult, ALU.add, [Tp, self.Tmod], [Txt[b][k]])
                    for k in range(8):
                        self.act(sq[:, k * n:(k + 1) * n], xt[b][:, k * n:(k + 1) * n], AF.Square, [Txt[b][k]], [Tsq[k]])
                    p, Tp = self.psn()
                    for k in range(8):
                        self.mm(p[:, :n], self.ones_bf[:], sq[:, k * n:(k + 1) * n], k == 0, k == 7, [Tsq[k], self.Tc], [Tp])
                    self.rstd_from_ps(rstd, Trs, p, Tp, n, 1.0 / D)
                    for k in range(8):
                        self.stt(xt[b][:, k * n:(k + 1) * n], xt[b][:, k * n:(k + 1) * n], fnw[:, k:k + 1], rstd[:, :n],
                                 ALU.mult, ALU.mult, [Trs, Tf], [Txt[b][k]])
                        S.dma("sp", self.out[k * 128:(k + 1) * 128, t0:t0 + n], xt[b][:, k * n:(k + 1) * n], reads=[Txt[b][k]])

    def phase_ffn(self, l, last):
        S, I = self.S, self.I
        moe = (l % 2 == 1)
        G = 2 if moe else 4
        if moe:
            experts = list(range(NEXP))
            nf = EFF // 128
            W1 = lambda e: I["moe_w1"][e]
            W3 = lambda e: I["moe_w3"][e]
            W2 = lambda e: I["moe_w2"][e]
        else:
            experts = [0]
            nf = DFF // 128
            W1 = lambda e: I["ffn_w1"]
            W3 = lambda e: I["ffn_w3"]
            W2 = lambda e: I["ffn_w2"]
        groups = [(g0, min(G, nf - g0)) for g0 in range(0, nf, G)]
        if last:
            passes = [TILES[1:5], TILES[5:9]]
        else:
            passes = [TILES[0:5], TILES[5:9]]
        TB = 2304
        with contextlib.ExitStack() as st:
            acc = self.sb(st, [128, 8 * TB], F32, "acc")
            h2 = self.sb(st, [128, 8 * TB], BF16, "h2")
            w13 = [self.sb(st, [128, 2 * 8 * G * 128], BF16, "w13") for _ in range(2)]
            w2 = [self.sb(st, [128, G * D], BF16, "w2") for _ in range(2)]
            Tw = TL(2)
            actb = [self.sb(st, [128, G * 512], BF16, "actb") for _ in range(2)]
            Tab = [TL(G) for _ in range(2)]
            sil = [self.sb(st, [128, 512], F32) for _ in range(2)]
            Tsil = TL(2)
            sq = self.sb(st, [128, 8 * 512], BF16)
            Tsq = TL(8)
            rstd = self.sb(st, [128, 512], F32)
            Trs = T()
            tmpb = [self.sb(st, [128, 512], F32) for _ in range(2)]
            Ttmp = TL(2)
            fnw = self.sb(st, [128, 8], F32)
            Tf = T()
            S.dma("sp", fnw[:], I["fnw"][:, :], writes=[Tf])
            if moe:
                rw = self.sb(st, [128, 8 * NEXP], F32)
                sel = self.sb(st, [8, 1024], F32)
                S.dma("sp", rw[:, :].rearrange("p (k e) -> p k e", k=8), I["router_w"].rearrange("(k p) e -> p k e", p=128), writes=[Tf])
                S.dma("sp", sel[:], I["sel"][:, :], writes=[Tf])
                hF = self.sb(st, [128, 8 * 512], F32, "hF")
                ThF = TL(8)
                lgt = self.sb(st, [128, 8], F32)
                top = self.sb(st, [128, 8], F32)
                gat = self.sb(st, [128, 4], F32)
                cmb = self.sb(st, [128, 8], F32)
                cm2 = self.sb(st, [128, 8], F32)
                Trt = T()
                combT = self.sb(st, [8, TB], F32, "combT")
                TcT = TL(5)
                cbc = [self.sb(st, [128, 512], F32) for _ in range(2)]
                Tcb = TL(2)
            wcount = 0
            for tiles in passes:
                offs = []
                o = 0
                for (c0, n) in tiles:
                    offs.append(o)
                    o += n
                TBn = o
                Tacc = [TL(8) for _ in tiles]
                Th2 = [TL(8) for _ in tiles]
                for si, (c0, n) in enumerate(tiles):
                    a = 1 if c0 == 0 else 0
                    o = offs[si]
                    for k in range(8):
                        S.dma("sp", acc[:, k * TB + o:k * TB + o + n], self.XS[k * 128:(k + 1) * 128, c0:c0 + n], writes=[Tacc[si][k]])
                    for k in range(8):
                        self.act(sq[:, k * n:(k + 1) * n], acc[:, k * TB + o:k * TB + o + n], AF.Square, [Tacc[si][k]], [Tsq[k]])
                    p, Tp = self.psn()
                    for k in range(8):
                        self.mm(p[:, :n], self.ones_bf[:], sq[:, k * n:(k + 1) * n], k == 0, k == 7, [Tsq[k], self.Tc], [Tp])
                    self.rstd_from_ps(rstd, Trs, p, Tp, n, 1.0 / D)
                    for k in range(8):
                        tb = k % 2
                        self.stt(tmpb[tb][:, :n], acc[:, k * TB + o:k * TB + o + n], self.A2[l][:, a * 8 + k:a * 8 + k + 1], rstd[:, :n],
                                 ALU.mult, ALU.mult, [Tacc[si][k], Trs, self.Tmod], [Ttmp[tb]])
                        self.act(h2[:, k * TB + o:k * TB + o + n], tmpb[tb][:, :n], AF.Identity, [Ttmp[tb], self.Tmod], [Th2[si][k]],
                                 bias=self.modt[l][:, a * 48 + 24 + k:a * 48 + 25 + k])
                        if moe:
                            self.ts("pool", hF[:, k * n:(k + 1) * n], tmpb[tb][:, :n], self.modt[l][:, a * 48 + 24 + k:a * 48 + 25 + k], None,
                                    ALU.add, None, [Ttmp[tb], self.Tmod], [ThF[k]])
                    if moe:
                        for tb in range(n // 128):
                            p, Tp = self.psn()
                            for k in range(8):
                                self.mm(p[:, 0:8], hF[:, k * n + tb * 128:k * n + (tb + 1) * 128], rw[:, k * 8:(k + 1) * 8],
                                        k == 0, k == 7, [ThF[k], Tf], [Tp])
                            self.cp("dve", lgt[:, :], p[:, 0:8], [Tp], [Trt])
                            S.op("dve", lambda e: e.max(out=top[:, :], in_=lgt[:, :]), reads=[Trt], writes=[Trt])
                            self.tt("dve", gat[:, 0:1], top[:, 1:2], top[:, 0:1], ALU.subtract, [Trt], [Trt])
                            self.act(gat[:, 1:2], gat[:, 0:1], AF.Exp, [Trt], [Trt])
                            self.ts("dve", gat[:, 1:2], gat[:, 1:2], 1.0, None, ALU.add, None, [Trt], [Trt])
                            self.recip(gat[:, 2:3], gat[:, 1:2], [Trt], [Trt])
                            self.ts("dve", gat[:, 3:4], gat[:, 2:3], -1.0, 1.0, ALU.mult, ALU.add, [Trt], [Trt])
                            self.ts("dve", cmb[:, :], lgt[:, :], top[:, 0:1], gat[:, 2:3], ALU.is_equal, ALU.mult, [Trt], [Trt])
                            self.ts("dve", cm2[:, :], lgt[:, :], top[:, 1:2], gat[:, 3:4], ALU.is_equal, ALU.mult, [Trt], [Trt])
                            self.tt("dve", cmb[:, :], cmb[:, :], cm2[:, :], ALU.add, [Trt], [Trt])
                            p2, Tp2 = self.psn()
                            S.op("pe", lambda e, p2=p2: e.transpose(p2[0:8, 0:128], cmb[:, :], self.ident[:, :]), reads=[Trt, self.Tc], writes=[Tp2])
                            self.cp("act", combT[:, o + tb * 128:o + (tb + 1) * 128], p2[0:8, 0:128], [Tp2], [TcT[si]])
                for e in experts:
                    for (g0, gn) in groups:
                        wb = wcount % 2
                        wcount += 1
                        for (ww, off) in ((W1(e), 0), (W3(e), 8 * G * 128)):
                            for hh in range(2):
                                S.dma("pool", w13[wb][:, off + hh * 4 * G * 128:off + hh * 4 * G * 128 + 4 * gn * 128].rearrange("p (k n) -> p k n", k=4),
                                      ww[hh * 512:(hh + 1) * 512, g0 * 128:(g0 + gn) * 128].rearrange("(k p) n -> p k n", p=128), writes=[Tw[wb]])
                        S.dma("pool", w2[wb][:, :gn * D].rearrange("p (j n) -> p j n", j=gn),
                              W2(e)[g0 * 128:(g0 + gn) * 128, :].rearrange("(j p) n -> p j n", p=128), writes=[Tw[wb]])
                        for si, (c0, n) in enumerate(tiles):
                            a = 1 if c0 == 0 else 0
                            o = offs[si]
                            ab = si % 2
                            if moe:
                                cb = si % 2
                                pc, Tpc = self.psn()
                                self.mm(pc[:, :n], sel[:, e * 128:(e + 1) * 128], combT[:, o:o + n], True, True, [Tf, TcT[si]], [Tpc])
                                self.cp("act", cbc[cb][:, :n], pc[:, :n], [Tpc], [Tcb[cb]])
                            for j in range(gn):
                                pa, Tpa = self.psn()
                                pb, Tpb = self.psn()
                                wrow = (lambda k, off: w13[wb][:, off + (k // 4) * 4 * G * 128 + (k % 4) * gn * 128 + j * 128:
                                                               off + (k // 4) * 4 * G * 128 + (k % 4) * gn * 128 + (j + 1) * 128])
                                for k in range(8):
                                    self.mm(pa[:, :n], wrow(k, 0), h2[:, k * TB + o:k * TB + o + n], k == 0, k == 7, [Tw[wb], Th2[si][k]], [Tpa])
                                for k in range(8):
                                    self.mm(pb[:, :n], wrow(k, 8 * G * 128), h2[:, k * TB + o:k * TB + o + n], k == 0, k == 7, [Tw[wb], Th2[si][k]], [Tpb])
                                sb_ = j % 2
                                self.act(sil[sb_][:, :n], pa[:, :n], AF.Silu, [Tpa], [Tsil[sb_]])
                                if moe:
                                    self.tt("dve", sil[sb_][:, :n], sil[sb_][:, :n], pb[:, :n], ALU.mult, [Tpb, Tsil[sb_]], [Tsil[sb_]])
                                    self.tt("pool", actb[ab][:, j * 512:j * 512 + n], sil[sb_][:, :n], cbc[cb][:, :n], ALU.mult,
                                            [Tsil[sb_], Tcb[cb]], [Tab[ab][j]])
                                else:
                                    self.tt("dve", actb[ab][:, j * 512:j * 512 + n], sil[sb_][:, :n], pb[:, :n], ALU.mult,
                                            [Tpb, Tsil[sb_]], [Tab[ab][j]])
                            for i in range(8):
                                po, Tpo = self.psn()
                                for j in range(gn):
                                    self.mm(po[:, :n], w2[wb][:, j * D + i * 128:j * D + (i + 1) * 128], actb[ab][:, j * 512:j * 512 + n],
                                            j == 0, j == gn - 1, [Tw[wb], Tab[ab][j]], [Tpo])
                                self.stt(acc[:, i * TB + o:i * TB + o + n], po[:, :n], self.modt[l][:, a * 48 + 40 + i:a * 48 + 41 + i],
                                         acc[:, i * TB + o:i * TB + o + n], ALU.mult, ALU.add, [Tpo, self.Tmod], [Tacc[si][i]])
                for si, (c0, n) in enumerate(tiles):
                    o = offs[si]
                    if not last:
                        for k in range(8):
                            S.dma("pool", self.XS[k * 128:(k + 1) * 128, c0:c0 + n], acc[:, k * TB + o:k * TB + o + n], reads=[Tacc[si][k]])
                    else:
                        for k in range(8):
                            self.act(sq[:, k * n:(k + 1) * n], acc[:, k * TB + o:k * TB + o + n], AF.Square, [Tacc[si][k]], [Tsq[k]])
                        p, Tp = self.psn()
                        for k in range(8):
                            self.mm(p[:, :n], self.ones_bf[:], sq[:, k * n:(k + 1) * n], k == 0, k == 7, [Tsq[k], self.Tc], [Tp])
                        self.rstd_from_ps(rstd, Trs, p, Tp, n, 1.0 / D)
                        for k in range(8):
                            self.stt(acc[:, k * TB + o:k * TB + o + n], acc[:, k * TB + o:k * TB + o + n], fnw[:, k:k + 1], rstd[:, :n],
                                     ALU.mult, ALU.mult, [Trs, Tf], [Tacc[si][k]])
                            S.dma("pool", self.out[k * 128:(k + 1) * 128, c0 - CTX:c0 - CTX + n], acc[:, k * TB + o:k * TB + o + n],
                                  reads=[Tacc[si][k]])
                S.barrier()


_CONST = {}


def _consts():
    if _CONST:
        return _CONST
    bf = ml_dtypes.bfloat16
    t = np.arange(SEQ, dtype=np.int64)
    ang = 2.0 * np.pi * ((t[:, None] * t[None, :]) % SEQ).astype(np.float64) / SEQ
    _CONST["dftc"] = (np.cos(ang) / 64.0).astype(np.float32).astype(bf)
    _CONST["dfts"] = (np.sin(ang) / 64.0).astype(np.float32).astype(bf)
    t = np.arange(CTX, dtype=np.int64)
    ang = 2.0 * np.pi * ((t[:, None] * t[None, :]) % CTX).astype(np.float64) / CTX
    _CONST["dftc256"] = (np.cos(ang) / 16.0).astype(np.float32).astype(bf)
    _CONST["dfts256"] = (np.sin(ang) / 16.0).astype(np.float32).astype(bf)
    t = np.arange(128, dtype=np.int64)
    ang = 2.0 * np.pi * ((t[:, None] * t[None, :]) % 128).astype(np.float64) / 128
    s = 1.0 / np.sqrt(128.0)
    _CONST["cs128"] = np.concatenate([np.cos(ang) * s, -np.sin(ang) * s], axis=1).astype(np.float32).astype(bf)
    s_, t_ = np.meshgrid(np.arange(128), np.arange(128), indexing="ij")
    sc = -1.0 / 16.0
    tri = np.concatenate([(s_ <= t_), (s_ > t_), (s_ >= t_), (s_ < t_)], axis=1).astype(np.float32) * sc
    _CONST["tri"] = np.ascontiguousarray(tri)
    mf = (s_ <= t_).astype(np.float32)
    mb = (s_ >= t_).astype(np.float32)
    _CONST["mask"] = np.concatenate([mf] * 4 + [mb] * 4, axis=1).astype(bf)
    _CONST["ident"] = np.eye(128, dtype=np.float32)
    sel = np.zeros((8, 1024), np.float32)
    for e in range(8):
        sel[e, e * 128:(e + 1) * 128] = 1.0
    _CONST["sel"] = sel
    _CONST["su"] = (s_ < t_).astype(np.float32)
    _CONST["identb"] = np.eye(128, dtype=np.float32).astype(bf)
    _CONST["slotstart"] = np.ascontiguousarray(np.broadcast_to((np.arange(NSLOT, dtype=np.float32) * TS)[None, :], (128, NSLOT)))
    jsh = np.zeros((128, 256), np.float32)
    for p_ in range(1, 128):
        jsh[128 - p_, p_] = 1.0
    jsh[0, 128] = 1.0
    _CONST["jsh"] = jsh.astype(bf)
    _CONST["alt"] = (((-1.0) ** np.arange(SEQ)) / 64.0).astype(np.float32).astype(bf)[None, :]
    _CONST["ecap"] = np.ascontiguousarray(np.broadcast_to((np.arange(8, dtype=np.float32) * CAP)[None, :], (128, 8)))
    _CONST["gp7"] = np.ascontiguousarray((np.arange(7)[None, :] * 128 + np.arange(128)[:, None]).astype(np.float32))
    return _CONST


def _pcol(v, nchunk):
    return np.ascontiguousarray(np.asarray(v, np.float32).reshape(nchunk, 128).T)


def make_in_maps(inputs, cores):
    C = _consts()
    f = lambda a: np.ascontiguousarray(np.asarray(a, np.float32))
    shared = dict(C)
    for l in range(DEPTH):
        shared[f"w_mod{l}"] = f(inputs["w_mod"][l])
        shared[f"b_mod{l}"] = _pcol(inputs["b_mod"][l], 48)
        shared[f"nmw{l}"] = _pcol(inputs["norm_mix_w"][l], 8)
        shared[f"nfw{l}"] = _pcol(inputs["norm_ffn_w"][l], 8)
        shared[f"w_in{l}"] = f(inputs["w_in"][l])
        shared[f"wg{l}"] = np.ascontiguousarray(np.concatenate([f(inputs["w_gate_f"][l]), f(inputs["w_gate_b"][l])], axis=1))
        shared[f"bg{l}"] = np.ascontiguousarray(np.concatenate([f(inputs["b_gate_f"][l]), f(inputs["b_gate_b"][l])])[None, :])
        shared[f"gnw{l}"] = f(inputs["gla_norm_w"][l]).reshape(128, 1)
        shared[f"w_out{l}"] = f(inputs["w_out"][l])
    shared["ffn_w1"] = f(inputs["ffn_w1"][0])
    shared["ffn_w3"] = f(inputs["ffn_w3"][0])
    shared["ffn_w2"] = f(inputs["ffn_w2"][0])
    shared["router_w"] = f(inputs["router_w"][0])
    for nm, src in (("mw1", "moe_w1"), ("mw3", "moe_w3")):
        w = np.asarray(inputs[src][0], np.float32).reshape(NEXP, 8, 128, 7, 512)
        shared[nm] = np.ascontiguousarray(w.transpose(0, 3, 2, 1, 4)).reshape(NEXP * 7 * 128, 8 * 512)
    w = np.asarray(inputs["moe_w2"][0], np.float32).reshape(NEXP, 7, 4, 128, D)
    shared["mw2"] = np.ascontiguousarray(w.transpose(0, 1, 3, 2, 4)).reshape(NEXP * 7 * 128, 4 * D)
    shared["fnw"] = _pcol(inputs["final_norm_w"], 8)
    x = np.asarray(inputs["x"], np.float32)
    ctx = np.asarray(inputs["ctx"], np.float32)
    c = np.asarray(inputs["c"], np.float32)
    cc = np.asarray(inputs["c_ctx"], np.float32)
    maps = []
    for b in cores:
        m = dict(shared)
        m["xt"] = np.ascontiguousarray(np.concatenate([ctx[b], x[b]], axis=0).T)
        m["cvec"] = np.ascontiguousarray(np.concatenate([_pcol(c[b], 8), _pcol(cc, 8)], axis=1))
        maps.append(m)
    return maps


_NC = {}


def kernel(**inputs):
    if "nc" not in _NC:
        _NC["nc"] = Builder().build()
    nc = _NC["nc"]
    maps = make_in_maps(inputs, list(range(8)))
    res = run_bass_kernel_spmd(nc, maps, core_ids=list(range(8)))
    out = np.stack([np.ascontiguousarray(r["out"].T) for r in res.results], axis=0)
    return out.astype(np.float32)
```
